# Optimizing a Trainium2 kernel written in Bass

```python
import math
import jax, jax.numpy as jnp
from jax import lax
import numpy as np

D_MODEL = 1024
BATCH = 2
SEQ = 8192
DEPTH = 1

CHUNK = 64
Q_BLOCK = 128
ATT_WIDTH = D_MODEL // 2
ATT_HEAD_DIM = 64
ATT_HEADS = ATT_WIDTH // (2 * ATT_HEAD_DIM)
LRU_WIDTH = D_MODEL - ATT_WIDTH
LRU_BLOCKS = 8
LRU_BLOCK_DIM = LRU_WIDTH // LRU_BLOCKS
LRU_C = 8.0
CONV_WIDTH = 4
IN_COLS = 3 * ATT_WIDTH + 2 * LRU_WIDTH
N_EXPERTS = 32
TOP_K = 4
D_EXPERT = D_MODEL
SWIGLU_LIMIT = 7.0
SWIGLU_ALPHA = 1.702
EXPERT_BLOCK = 256
ALPHA = (2.0 * DEPTH) ** 0.25
BETA = (8.0 * DEPTH) ** -0.25
LN_EPS = 1e-5
SUBLN_EPS = 1e-5

kernel_name = "hybrid_diffattn_rglru_moe_deepnorm"


def layer_norm(x, g, b):
    xf = x.astype(jnp.float32)
    mu = jnp.mean(xf, axis=-1, keepdims=True)
    var = jnp.mean(jnp.square(xf - mu), axis=-1, keepdims=True)
    y = (xf - mu) * lax.rsqrt(var + LN_EPS) * g.astype(jnp.float32) + b.astype(jnp.float32)
    return y.astype(x.dtype)


def alibi_slopes(n):
    return jnp.asarray([2.0 ** (-8.0 * (i + 1) / n) for i in range(n)], dtype=jnp.float32)


def diff_attention(q, k, v, lam, subln_g, lam_init):
    B, S = q.shape[0], q.shape[1]
    nb = S // Q_BLOCK
    f32 = jnp.float32
    qf = (q.astype(f32) * (ATT_HEAD_DIM ** -0.5)).reshape(
        B, nb, Q_BLOCK, ATT_HEADS, 2, ATT_HEAD_DIM).transpose(1, 0, 3, 4, 2, 5)
    kf = k.astype(f32).transpose(0, 2, 3, 1, 4)
    vf = v.astype(f32).transpose(0, 2, 1, 3)
    slopes = alibi_slopes(ATT_HEADS)
    kpos = jnp.arange(S)

    def block(args):
        qb, bi = args
        qpos = bi * Q_BLOCK + jnp.arange(Q_BLOCK)
        s = jnp.einsum('bhmqd,bhmkd->bhmqk', qb, kf)
        dist = jnp.abs(qpos[:, None] - kpos[None, :]).astype(f32)
        bias = -slopes[:, None, None] * dist
        allowed = (kpos[None, :] // CHUNK) <= (qpos[:, None] // CHUNK)
        s = jnp.where(allowed, s + bias[None, :, None], -jnp.inf)
        p = jax.nn.softmax(s, axis=-1)
        a = p[:, :, 0] - lam * p[:, :, 1]
        return jnp.einsum('bhqk,bhkd->bhqd', a, vf)

    o = lax.map(block, (qf, jnp.arange(nb)))
    o = o.transpose(1, 0, 3, 2, 4).reshape(B, S, ATT_HEADS, 2 * ATT_HEAD_DIM)
    o = o * lax.rsqrt(jnp.mean(jnp.square(o), axis=-1, keepdims=True) + SUBLN_EPS)
    o = o * subln_g.astype(f32) * (1.0 - lam_init)
    return o.reshape(B, S, ATT_WIDTH)


def rg_lru_branch(xb, gb, conv_w, conv_b, w_a, b_a, w_x, b_x, lru_lambda):
    B, S = xb.shape[0], xb.shape[1]
    f32 = jnp.float32
    xc = lax.conv_general_dilated(
        xb, conv_w.astype(xb.dtype)[:, None, :], window_strides=(1,),
        padding=[(CONV_WIDTH - 1, 0)], dimension_numbers=('NWC', 'WIO', 'NWC'),
        feature_group_count=LRU_WIDTH)
    xc = xc.astype(f32) + conv_b.astype(f32)
    xh = xc.reshape(B, S, LRU_BLOCKS, LRU_BLOCK_DIM)
    r = jax.nn.sigmoid(jnp.einsum('bsnc,ncd->bsnd', xh, w_a.astype(f32)) + b_a.astype(f32)).reshape(B, S, LRU_WIDTH)
    i = jax.nn.sigmoid(jnp.einsum('bsnc,ncd->bsnd', xh, w_x.astype(f32)) + b_x.astype(f32)).reshape(B, S, LRU_WIDTH)
    log_a = -LRU_C * r * jax.nn.softplus(-lru_lambda.astype(f32))
    a = jnp.exp(log_a)
    u = jnp.sqrt(-jnp.expm1(2.0 * log_a)) * (i * xc)

    def combine(left, right):
        a1, b1 = left
        a2, b2 = right
        return a1 * a2, a2 * b1 + b2

    _, h = lax.associative_scan(combine, (a, u), axis=1)
    return (h * jax.nn.gelu(gb.astype(f32))).astype(xb.dtype)


def clamped_swiglu(h):
    x_glu = jnp.minimum(h[..., ::2], SWIGLU_LIMIT)
    x_lin = jnp.clip(h[..., 1::2], -SWIGLU_LIMIT, SWIGLU_LIMIT)
    return x_glu * jax.nn.sigmoid(SWIGLU_ALPHA * x_glu) * (x_lin + 1.0)


def moe(x, w_router, b_router, w_gu, b_gu, w_down, b_down):
    B, S, D = x.shape
    T = B * S
    A = T * TOP_K
    xt = x.reshape(T, D)
    logits = (xt @ w_router).astype(jnp.float32) + b_router.astype(jnp.float32)
    top_v, top_e = lax.top_k(logits, TOP_K)
    gates = jax.nn.softmax(top_v, axis=-1)
    flat_e = top_e.reshape(A)
    flat_tok = jnp.repeat(jnp.arange(T, dtype=jnp.int32), TOP_K)
    flat_g = gates.reshape(A)
    order = jnp.argsort(flat_e)
    sorted_e = flat_e[order]
    counts = jnp.bincount(flat_e, length=N_EXPERTS)
    starts = jnp.cumsum(counts) - counts
    padded = (counts + EXPERT_BLOCK - 1) // EXPERT_BLOCK * EXPERT_BLOCK
    pends = jnp.cumsum(padded)
    pstarts = pends - padded
    dest = pstarts[sorted_e] + (jnp.arange(A) - starts[sorted_e])
    num_blocks = (A + N_EXPERTS * (EXPERT_BLOCK - 1) + EXPERT_BLOCK - 1) // EXPERT_BLOCK
    R = num_blocks * EXPERT_BLOCK
    row_tok = jnp.zeros((R,), jnp.int32).at[dest].set(flat_tok[order])
    row_gate = jnp.zeros((R,), jnp.float32).at[dest].set(flat_g[order])
    block_e = jnp.minimum(
        jnp.searchsorted(pends, jnp.arange(num_blocks) * EXPERT_BLOCK, side='right'), N_EXPERTS - 1)

    def expert_block(args):
        tok, g, e = args
        xb = xt[tok]
        h = xb @ w_gu[e] + b_gu[e]
        y = clamped_swiglu(h) @ w_down[e] + b_down[e]
        return y * g[:, None].astype(y.dtype)

    ys = lax.map(expert_block, (row_tok.reshape(num_blocks, EXPERT_BLOCK),
                                row_gate.reshape(num_blocks, EXPERT_BLOCK), block_e))
    out = jax.ops.segment_sum(ys.reshape(R, D), row_tok, num_segments=T)
    return out.reshape(B, S, D).astype(x.dtype)


def setup_inputs(seed: int = 0) -> dict:
    key = jax.random.key(seed)
    ks = jax.random.split(key, 32)
    f32 = jnp.float32
    L = DEPTH
    nrm = lambda k, s, sc: jax.random.normal(k, s, f32) * sc
    x = jax.random.normal(ks[0], (BATCH, SEQ, D_MODEL), f32)
    ln0_g = 1.0 + nrm(ks[1], (D_MODEL,), 0.02)
    ln0_b = nrm(ks[2], (D_MODEL,), 0.02)
    w_in = nrm(ks[3], (L, D_MODEL, IN_COLS), D_MODEL ** -0.5)
    col_scale = jnp.ones((IN_COLS,), f32).at[2 * ATT_WIDTH:3 * ATT_WIDTH].set(BETA)
    w_in = w_in * col_scale
    conv_w = nrm(ks[4], (L, CONV_WIDTH, LRU_WIDTH), CONV_WIDTH ** -0.5)
    conv_b = nrm(ks[5], (L, LRU_WIDTH), 0.01)
    w_rg_a = nrm(ks[6], (L, LRU_BLOCKS, LRU_BLOCK_DIM, LRU_BLOCK_DIM), LRU_BLOCK_DIM ** -0.5)
    b_rg_a = nrm(ks[7], (L, LRU_BLOCKS, LRU_BLOCK_DIM), 0.01)
    w_rg_x = nrm(ks[8], (L, LRU_BLOCKS, LRU_BLOCK_DIM, LRU_BLOCK_DIM), LRU_BLOCK_DIM ** -0.5)
    b_rg_x = nrm(ks[9], (L, LRU_BLOCKS, LRU_BLOCK_DIM), 0.01)
    u = jax.random.uniform(ks[10], (L, LRU_WIDTH), f32, 0.9, 0.999)
    s = u ** (1.0 / LRU_C)
    lru_lambda = jnp.log(s) - jnp.log1p(-s)
    lam_q1 = nrm(ks[11], (L, ATT_HEAD_DIM), 0.1)
    lam_k1 = nrm(ks[12], (L, ATT_HEAD_DIM), 0.1)
    lam_q2 = nrm(ks[13], (L, ATT_HEAD_DIM), 0.1)
    lam_k2 = nrm(ks[14], (L, ATT_HEAD_DIM), 0.1)
    subln_g = 1.0 + nrm(ks[15], (L, 2 * ATT_HEAD_DIM), 0.02)
    w_out = nrm(ks[16], (L, D_MODEL, D_MODEL), BETA * D_MODEL ** -0.5)
    ln1_g = 1.0 + nrm(ks[17], (L, D_MODEL), 0.02)
    ln1_b = nrm(ks[18], (L, D_MODEL), 0.02)
    w_router = nrm(ks[19], (L, D_MODEL, N_EXPERTS), D_MODEL ** -0.5)
    b_router = nrm(ks[20], (L, N_EXPERTS), 0.01)
    w_gu = nrm(ks[21], (L, N_EXPERTS, D_MODEL, 2 * D_EXPERT), BETA * D_MODEL ** -0.5)
    b_gu = nrm(ks[22], (L, N_EXPERTS, 2 * D_EXPERT), 0.01)
    w_down = nrm(ks[23], (L, N_EXPERTS, D_EXPERT, D_MODEL), BETA * D_EXPERT ** -0.5)
    b_down = nrm(ks[24], (L, N_EXPERTS, D_MODEL), 0.01)
    ln2_g = 1.0 + nrm(ks[25], (L, D_MODEL), 0.02)
    ln2_b = nrm(ks[26], (L, D_MODEL), 0.02)
    return {"x": x, "ln0_g": ln0_g, "ln0_b": ln0_b, "w_in": w_in,
            "conv_w": conv_w, "conv_b": conv_b, "w_rg_a": w_rg_a, "b_rg_a": b_rg_a,
            "w_rg_x": w_rg_x, "b_rg_x": b_rg_x, "lru_lambda": lru_lambda,
            "lam_q1": lam_q1, "lam_k1": lam_k1, "lam_q2": lam_q2, "lam_k2": lam_k2,
            "subln_g": subln_g, "w_out": w_out, "ln1_g": ln1_g, "ln1_b": ln1_b,
            "w_router": w_router, "b_router": b_router, "w_gu": w_gu, "b_gu": b_gu,
            "w_down": w_down, "b_down": b_down, "ln2_g": ln2_g, "ln2_b": ln2_b}


def reference(x, ln0_g, ln0_b, w_in, conv_w, conv_b, w_rg_a, b_rg_a, w_rg_x, b_rg_x,
              lru_lambda, lam_q1, lam_k1, lam_q2, lam_k2, subln_g, w_out, ln1_g, ln1_b,
              w_router, b_router, w_gu, b_gu, w_down, b_down, ln2_g, ln2_b):
    B, S, _ = x.shape
    f32 = jnp.float32
    x = layer_norm(x, ln0_g, ln0_b)
    for l in range(DEPTH):
        lam_init = 0.8 - 0.6 * math.exp(-0.3 * l)
        proj = x @ w_in[l]
        q, k, v, xl, gl = jnp.split(
            proj, [ATT_WIDTH, 2 * ATT_WIDTH, 3 * ATT_WIDTH, 3 * ATT_WIDTH + LRU_WIDTH], axis=-1)
        q = q.reshape(B, S, ATT_HEADS, 2, ATT_HEAD_DIM)
        k = k.reshape(B, S, ATT_HEADS, 2, ATT_HEAD_DIM)
        v = v.reshape(B, S, ATT_HEADS, 2 * ATT_HEAD_DIM)
        lam = (jnp.exp(jnp.sum(lam_q1[l].astype(f32) * lam_k1[l].astype(f32)))
               - jnp.exp(jnp.sum(lam_q2[l].astype(f32) * lam_k2[l].astype(f32))) + lam_init)
        att = diff_attention(q, k, v, lam, subln_g[l], lam_init).astype(x.dtype)
        rec = rg_lru_branch(xl, gl, conv_w[l], conv_b[l], w_rg_a[l], b_rg_a[l],
                            w_rg_x[l], b_rg_x[l], lru_lambda[l])
        mixed = jnp.concatenate([att, rec], axis=-1) @ w_out[l]
        x = layer_norm(ALPHA * x + mixed, ln1_g[l], ln1_b[l])
        ffn = moe(x, w_router[l], b_router[l], w_gu[l], b_gu[l], w_down[l], b_down[l])
        x = layer_norm(ALPHA * x + ffn, ln2_g[l], ln2_b[l])
    return x
```

```python
import math
from contextlib import ExitStack

import numpy as np
import ml_dtypes
import concourse.bass as bass
import concourse.mybir as mybir
from concourse.bass_utils import run_bass_kernel_spmd

F32 = mybir.dt.float32
BF16 = mybir.dt.bfloat16
U32 = mybir.dt.uint32
AF = mybir.ActivationFunctionType
ALU = mybir.AluOpType

NCORES = 8
D = 1024
SEQ = 8192
NBLK = SEQ // 128
OWN = 2048
NOB = OWN // 128
NE = 32
CAP = 384
DEPTH = 1
ALPHA = (2.0 * DEPTH) ** 0.25
LN_EPS = 1e-5
LAM_INIT = 0.8 - 0.6 * math.exp(-0.3 * 0)
SLOPES = [2.0 ** (-8.0 * (i + 1) / 4) for i in range(4)]
NEG = -30000.0
NCF = 128 + 128 + NE + 4 + 4


class T:
    __slots__ = ("name", "w", "r", "dsem", "dcnt")

    def __init__(self, name):
        self.name = name
        self.w = None
        self.r = {}
        self.dsem = None
        self.dcnt = 0


ENGS = ("sync", "pe", "act", "dve", "pool")


SEM_STACK = [None]


class Sched:
    def __init__(self, nc, es, tag):
        self.nc = nc
        self.es = SEM_STACK[0]
        self.tag = tag
        self.q = {e: [] for e in ENGS}
        self.sem = {}
        self.cnt = {}
        for e in ("pe", "act", "dve", "pool"):
            self.sem[e] = self.es.enter_context(nc.semaphore(f"{tag}_{e}"))
            self.cnt[e] = 0
        self.seen = {e: {} for e in ENGS}
        self.semobj = {e: self.sem[e] for e in self.sem}
        self.dma_tiles = []
        self.nsem = 4

    def _waits(self, eng, reads, writes, strict=False):
        deps = {}

        def add(m, same_ok):
            if m is None:
                return
            k, v = m
            if k == eng and not (same_ok or strict):
                return
            if deps.get(k, 0) < v:
                deps[k] = v

        for t in reads:
            add(t.w, eng != "pe")
        for t in writes:
            add(t.w, False)
            for k, v in t.r.items():
                add((k, v), False)
        out = []
        for k, v in deps.items():
            if self.seen[eng].get(k, 0) >= v:
                continue
            self.seen[eng][k] = v
            out.append((self.semobj[k], v))
        return out

    def _mark(self, mark, reads, writes):
        k, v = mark
        for t in reads:
            if t.r.get(k, 0) < v:
                t.r[k] = v
        for t in writes:
            t.w = mark
            t.r = {}

    def op(self, eng, fn, reads=(), writes=()):
        waits = self._waits(eng, reads, writes)
        self.cnt[eng] += 1
        mark = (eng, self.cnt[eng])
        self.q[eng].append((waits, fn, (self.sem[eng], 1)))
        self._mark(mark, reads, writes)

    def dma(self, queue, fn, reads, writes, sem_tile=None):
        st = sem_tile if sem_tile is not None else writes[0]
        if st.dsem is None:
            st.dsem = self.es.enter_context(self.nc.semaphore(f"{self.tag}_d{self.nsem}"))
            self.nsem += 1
            self.semobj[id(st)] = st.dsem
            self.dma_tiles.append(st)
        waits = self._waits(queue, reads, writes, strict=True)
        st.dcnt += 16
        mark = (id(st), st.dcnt)
        self.q[queue].append((waits, fn, (st.dsem, 16)))
        if queue == "pool":
            pass
        self._mark(mark, reads, writes)

    def emit(self, block):
        final = [(t.dsem, t.dcnt) for t in self.dma_tiles]
        self.q["sync"].append((final, None, None))
        table = (("sync", block.sync), ("pe", block.tensor), ("act", block.scalar),
                 ("dve", block.vector), ("pool", block.gpsimd))
        for name, deco in table:
            ops = self.q[name]

            def body(eng, ops=ops):
                for waits, fn, inc in ops:
                    for s, v in waits:
                        eng.wait_ge(s, v)
                    if fn is not None:
                        ins = fn(eng)
                        if inc is not None:
                            ins.then_inc(inc[0], inc[1])

            deco(body)


class Ring:
    def __init__(self, bufs):
        self.bufs = bufs
        self.i = 0

    def next(self):
        b = self.bufs[self.i % len(self.bufs)]
        self.i += 1
        return b


class Ctx:
    pass


def _sb(nc, es, name, shape, dt):
    return es.enter_context(nc.sbuf_tensor(name, shape, dt))


def _ps(nc, es, name, shape, dt):
    return es.enter_context(nc.psum_tensor(name, shape, dt))


def _breg(e, cache):
    if "r" not in cache:
        cache["r"] = e.to_reg(NE * CAP - 1)
    return cache["r"]


def _bcast_rows(handle, nrows, ncols, offset=0):
    return bass.AP(handle, offset, [[0, nrows], [1, ncols]])


def build_program(debug=False, upto=5, nexp=NE, p4mask=31, only=None):
    nc = bass.Bass("TRN2", target_bir_lowering=False)
    c = Ctx()
    c.nc = nc
    dk = "ExternalOutput" if debug else "Internal"

    def din(name, shape, dt=F32):
        return nc.dram_tensor(name, list(shape), dt, kind="ExternalInput")

    c.x_full = din("x_full", [SEQ, D])
    c.x_own = din("x_own", [OWN, D])
    c.w_in = din("w_in", [D, 2560])
    c.w_out = din("w_out", [D, D])
    c.w_router = din("w_router", [D, NE])
    ne_w = nexp if upto >= 4 else 1
    c.nexp = nexp
    c.p4mask = p4mask
    c.w_gu = din("w_gu", [ne_w, D, 2048])
    c.w_down = din("w_down", [ne_w, D, D])
    c.b_down = din("b_down", [NE, D])
    c.rows = din("rows", [8, D])
    c.colp = din("colp", [128, 64])
    c.w_rg = din("w_rg", [2, 8, 64, 64])
    c.lamv = din("lamv", [4, 64])
    c.b_router = din("b_router", [1, NE])
    c.bgu = din("bgu", [128, NE * 16])
    c.kbt = din("kbt", [128, 4 * 64])
    c.dq = din("dq", [128, 4 * 512])
    c.mfull = din("mfull", [128, 16 * 128])
    c.cst_bf = din("cst_bf", [128, 4 * 128], BF16)
    c.cst_f = din("cst_f", [128, NCF])
    c.out = nc.dram_tensor("out", [OWN, D], F32, kind="ExternalOutput")
    c.k_scr = nc.dram_tensor("k_scr", [4, 128, SEQ], BF16, kind=dk)
    c.v_scr = nc.dram_tensor("v_scr", [4, 128, NBLK, 128], BF16, kind=dk)
    c.q_scr = nc.dram_tensor("q_scr", [4, 128, 2, OWN], BF16, kind=dk)
    c.xg_scr = nc.dram_tensor("xg_scr", [NE * CAP, D], BF16, kind=dk)
    c.y_scr = nc.dram_tensor("y_scr", [NE * CAP, D], F32, kind=dk)
    c.x1_scr = nc.dram_tensor("x1_scr", [OWN, D], F32, kind=dk)
    c.dbg = nc.dram_tensor("dbg", [128, 4 * OWN * 2], BF16, kind=dk)
    c.dbg_gts = nc.dram_tensor("dbg_gts", [128, NOB * 4], F32, kind=dk)
    c.dbg_dst = nc.dram_tensor("dbg_dst", [128, NOB * 4], U32, kind=dk)

    with ExitStack() as es:
        SEM_STACK[0] = es
        c.cbf = _sb(nc, es, "cbf", [128, 4 * 128], BF16)
        c.cf = _sb(nc, es, "cf", [128, NCF], F32)
        c.colp_sb = _sb(nc, es, "colp_sb", [128, 64], F32)
        c.gts = _sb(nc, es, "gts", [128, NOB, 4], F32)
        c.dst = _sb(nc, es, "dst", [128, NOB, 4], U32)
        c.ident_bf = c.cbf[:, 0:128]
        c.ones_bf = c.cbf[:, 128:256]
        c.ltri_bf = c.cbf[:, 256:384]
        c.row0_bf = c.cbf[:, 384:512]
        c.ident_f = c.cf[:, 0:128]
        c.ones_f = c.cf[:, 128:256]
        c.ecap = c.cf[:, 256:256 + NE]
        c.sel = c.cf[:, 256 + NE:256 + NE + 4]
        c.epsc = c.cf[:, 292:293]
        c.onec = c.cf[:, 293:294]

        if only is not None:
            S0 = Sched(nc, es, "p0")
            S0.dma("sync", lambda e: e.dma_start(out=c.cbf[:], in_=c.cst_bf[:, :]), [], [T("a")])
            S0.dma("sync", lambda e: e.dma_start(out=c.cf[:], in_=c.cst_f[:, :]), [], [T("b")])
            with nc.Block() as blk:
                S0.emit(blk)
            {4: phase4, 5: phase5}[only](c)
            return nc
        with ExitStack() as es2:
            c.recT = _sb(nc, es2, "recT", [128, 4, OWN], BF16)
            phase1(c)
            c.attT = _sb(nc, es2, "attT", [128, 4, OWN], BF16)
            if upto >= 2:
                phase2(c)
            if upto >= 3:
                phase3(c)
            if debug:
                phase_dbg(c)
        if upto >= 4:
            phase4(c)
        if upto >= 5:
            phase5(c)
    return nc


def phase_dbg(c):
    nc = c.nc
    with ExitStack() as es:
        S = Sched(nc, es, "pd")
        td = T("dbg")
        S.dma("sync", lambda e: e.dma_start(out=c.dbg[:, 0:4 * OWN], in_=c.recT[:].rearrange("p a b -> p (a b)")), [], [td])
        S.dma("sync", lambda e: e.dma_start(out=c.dbg[:, 4 * OWN:8 * OWN], in_=c.attT[:].rearrange("p a b -> p (a b)")), [], [td])
        S.dma("sync", lambda e: e.dma_start(out=c.dbg_gts[:, :], in_=c.gts[:].rearrange("p a b -> p (a b)")), [], [td])
        S.dma("sync", lambda e: e.dma_start(out=c.dbg_dst[:, :], in_=c.dst[:].rearrange("p a b -> p (a b)")), [], [td])
        with nc.Block() as blk:
            S.emit(blk)


def phase1(c):
    nc = c.nc
    with ExitStack() as es:
        S = Sched(nc, es, "p1")
        sb = lambda n, s, d: _sb(nc, es, n, s, d)
        c.Tk_scr, c.Tv_scr, c.Tq_scr = T("k_scr"), T("v_scr"), T("q_scr")
        win = sb("win", [128, 8, 2560], BF16)
        Twin = [T(f"win{k}") for k in range(8)]
        wst = Ring([(sb(f"wst{i}", [128, 1280], F32), T(f"wst{i}")) for i in range(2)])
        xs = Ring([(sb(f"xs{i}", [128, D], F32), T(f"xs{i}")) for i in range(3)])
        stt = sb("stt", [128, 4, 12], F32); Tstt = T("stt")
        mv = sb("mv", [128, 4, 2], F32); Tmv = T("mv")
        rstd = sb("rstd", [128, 4], F32); Trstd = T("rstd")
        nmr = sb("nmr", [128, 4], F32); Tnmr = T("nmr")
        xn = sb("xn", [128, 4, D], BF16); Txn = [T(f"xn{b}") for b in range(4)]
        xT = sb("xT", [128, 8, 512], BF16); TxT = [T(f"xT{k}") for k in range(8)]
        kst = sb("kst", [128, 4, 512], BF16); Tkst = T("kst")
        vst = sb("vst", [128, 4, 512], BF16); Tvst = T("vst")
        qst = sb("qst", [128, 4, 2, 512], BF16); Tqst = T("qst")
        xlb = [(sb(f"xlb{i}", [128, 4, 515], F32), T(f"xlb{i}")) for i in range(2)]
        xc = sb("xc", [128, 4, 512], F32); Txc = T("xc")
        xcb = sb("xcb", [128, 4, 512], BF16); Txcb = T("xcb")
        rg = sb("rg", [128, 4, 512], F32); Trg = T("rg")
        ig = sb("ig", [128, 4, 512], F32); Tig = T("ig")
        aa = sb("aa", [128, 4, 512], F32); Taa = T("aa")
        sq = sb("sq", [128, 4, 512], F32); Tsq = T("sq")
        hb = [(sb(f"hb{i}", [128, 4, 512], F32), T(f"hb{i}")) for i in range(2)]
        hsel = sb("hsel", [128, 4, 128], F32); Thsel = T("hsel")
        ge = sb("ge", [128, 512], F32); Tge = T("ge")
        bdst = sb("bdst", [128, 2, 4, 128], F32); Tbdst = T("bdst")
        bd = sb("bd", [128, 2, 4, 128], BF16); Tbd = T("bd")
        lrp = sb("lrp", [128, 12], F32); Tlrp = T("lrp")
        Tcst = T("cst"); Tcolp = T("colp"); TrecT = T("recT")
        pst = [(_ps(nc, es, f"pst{i}", [128, 1024], BF16), T(f"pst{i}")) for i in range(2)]
        psr = Ring([(_ps(nc, es, f"psm{i}", [128, 512], F32), T(f"psm{i}")) for i in range(6)])
        colp = c.colp_sb

        S.dma("sync", lambda e: e.dma_start(out=c.cbf[:], in_=c.cst_bf[:, :]), [], [Tcst])
        S.dma("sync", lambda e: e.dma_start(out=c.cf[:], in_=c.cst_f[:, :]), [], [Tcst])
        S.dma("sync", lambda e: e.dma_start(out=colp[:], in_=c.colp[:, :]), [], [Tcolp])
        Tparts = [T(f"bdp{i}") for i in range(16)]
        S.op("pool", lambda e: e.memset(bdst[:].rearrange("p a b c -> p (a b c)"), 0.0), [], [Tbdst] + Tparts)
        for m in range(2):
            for ch in range(4):
                for u in range(2):
                    S.dma("sync", lambda e, m=m, ch=ch, u=u: e.dma_start(
                        out=bdst[u * 64:(u + 1) * 64, m, ch, u * 64:(u + 1) * 64],
                        in_=c.w_rg[m, 2 * ch + u, :, :]), [], [Tparts[m * 8 + ch * 2 + u]])
        S.op("act", lambda e: e.copy(out=bd[:].rearrange("p a b c -> p (a b c)"),
                                     in_=bdst[:].rearrange("p a b c -> p (a b c)")), [Tbdst] + Tparts, [Tbd])
        S.op("act", lambda e: e.activation(out=lrp[:, 8:12], in_=colp[:, 44:48], func=AF.Exp, scale=-1.0),
             [Tcolp], [Tlrp])
        S.op("act", lambda e: e.activation(out=lrp[:, 8:12], in_=lrp[:, 8:12], func=AF.Ln, bias=1.0),
             [Tlrp], [Tlrp])
        S.op("dve", lambda e: e.tensor_scalar(out=lrp[:, 0:4], in0=lrp[:, 8:12], scalar1=-8.0, scalar2=None,
                                              op0=ALU.mult), [Tlrp], [Tlrp])
        S.op("dve", lambda e: e.tensor_scalar(out=lrp[:, 4:8], in0=lrp[:, 8:12], scalar1=-16.0, scalar2=None,
                                              op0=ALU.mult), [Tlrp], [Tlrp])
        S.op("pool", lambda e: e.memset(qst[:].rearrange("p a b c -> p (a b c)"), 0.0), [], [Tqst])
        S.op("pool", lambda e: e.memset(xlb[0][0][:, :, 0:3], 0.0), [], [xlb[0][1]])
        cast_engs = ("act", "pool")
        for kc in range(8):
            for hf in range(2):
                st, Tst = wst.next()
                S.dma("sync", lambda e, st=st, kc=kc, hf=hf: e.dma_start(
                    out=st[:], in_=c.w_in[kc * 128:(kc + 1) * 128, hf * 1280:(hf + 1) * 1280]), [], [Tst])
                eng = cast_engs[(kc * 2 + hf) % 2]
                if eng == "act":
                    S.op("act", lambda e, st=st, kc=kc, hf=hf: e.copy(out=win[:, kc, hf * 1280:(hf + 1) * 1280], in_=st[:]),
                         [Tst], [Twin[kc]])
                else:
                    S.op("pool", lambda e, st=st, kc=kc, hf=hf: e.tensor_copy(out=win[:, kc, hf * 1280:(hf + 1) * 1280], in_=st[:]),
                         [Tst], [Twin[kc]])

        def mm_group(ps, Tps, lhs_fn, rhs_fn, reads_fn):
            for kc in range(8):
                S.op("pe", lambda e, kc=kc: e.matmul(ps, lhs_fn(kc), rhs_fn(kc), start=(kc == 0), stop=(kc == 7)),
                     reads_fn(kc), [Tps])

        def do_tile(it):
            own = it >= 16
            src = c.x_own if own else c.x_full
            t0 = (it - 16) * 512 if own else it * 512
            xsl = []
            for b in range(4):
                xt, Txt = xs.next()
                xsl.append((xt, Txt))
                S.dma("sync", lambda e, xt=xt, b=b: e.dma_start(out=xt[:], in_=src[t0 + b * 128:t0 + (b + 1) * 128, :]),
                      [], [Txt])
                for hh in range(2):
                    S.op("dve", lambda e, xt=xt, b=b, hh=hh: e.bn_stats(out=stt[:, b, hh * 6:(hh + 1) * 6],
                                                                     in_=xt[:, hh * 512:(hh + 1) * 512]), [Txt], [Tstt])
                S.op("dve", lambda e, b=b: e.bn_aggr(out=mv[:, b, :], in_=stt[:, b, :]), [Tstt], [Tmv])
                if b == 2 or b == 3:
                    pass
                S.op("act", lambda e, b=b: e.activation(out=rstd[:, b:b + 1], in_=mv[:, b, 1:2], func=AF.Sqrt,
                                                        bias=c.epsc[:, 0:1], scale=1.0), [Tmv], [Trstd])
                S.op("dve", lambda e, b=b: e.reciprocal(out=rstd[:, b:b + 1], in_=rstd[:, b:b + 1]), [Trstd], [Trstd])
                S.op("dve", lambda e, b=b: e.scalar_tensor_tensor(out=nmr[:, b:b + 1], in0=mv[:, b, 0:1], scalar=-1.0,
                                                                  in1=rstd[:, b:b + 1], op0=ALU.mult, op1=ALU.mult),
                     [Tmv, Trstd], [Tnmr])
                S.op("act", lambda e, xt=xt, b=b: e.activation(out=xn[:, b, :], in_=xt[:], func=AF.Identity,
                                                                scale=rstd[:, b:b + 1], bias=nmr[:, b:b + 1]),
                     [Txt, Trstd, Tnmr], [Txn[b]])
            for kc in range(8):
                pt, Tpt = pst[(kc // 2) % 2]
                off = (kc % 2) * 512
                for b in range(4):
                    S.op("pe", lambda e, pt=pt, off=off, b=b, kc=kc: e.transpose(
                        out=pt[:, off + b * 128:off + (b + 1) * 128], in_=xn[:, b, kc * 128:(kc + 1) * 128],
                        identity=c.ident_bf), [Txn[b], Tcst], [Tpt])
                S.op("act", lambda e, pt=pt, off=off, kc=kc: e.activation(
                    out=xT[:, kc, :], in_=pt[:, off:off + 512], func=AF.Identity,
                    scale=colp[:, kc:kc + 1], bias=colp[:, 8 + kc:9 + kc]), [Tpt, Tcolp], [TxT[kc]])
            if not own:
                for h in range(4):
                    ps, Tps = psr.next()
                    mm_group(ps[:], Tps, lambda kc, h=h: win[:, kc, 512 + h * 128:512 + (h + 1) * 128],
                             lambda kc: xT[:, kc, :], lambda kc: [Twin[kc], TxT[kc]])
                    S.op("dve" if h % 2 else "act",
                         (lambda e, ps=ps, h=h: e.tensor_copy(out=kst[:, h, :], in_=ps[:])) if h % 2 else
                         (lambda e, ps=ps, h=h: e.copy(out=kst[:, h, :], in_=ps[:])), [Tps], [Tkst])
                for h in range(4):
                    S.dma("sync", lambda e, h=h: e.dma_start(out=c.k_scr[h, :, t0:t0 + 512], in_=kst[:, h, :]),
                          [Tkst], [c.Tk_scr], sem_tile=c.Tk_scr)
                for b in range(4):
                    ps, Tps = psr.next()
                    mm_group(ps[:], Tps, lambda kc, b=b: xT[:, kc, b * 128:(b + 1) * 128],
                             lambda kc: win[:, kc, 1024:1536], lambda kc: [Twin[kc], TxT[kc]])
                    S.op("dve" if b % 2 else "act",
                         (lambda e, ps=ps, b=b: e.tensor_copy(out=vst[:, b, :], in_=ps[:])) if b % 2 else
                         (lambda e, ps=ps, b=b: e.copy(out=vst[:, b, :], in_=ps[:])), [Tps], [Tvst])
                for h in range(4):
                    S.dma("sync", lambda e, h=h: e.dma_start(out=c.v_scr[h, :, it * 4:(it + 1) * 4, :],
                                                              in_=vst[:, :, h * 128:(h + 1) * 128]),
                          [Tvst], [c.Tv_scr], sem_tile=c.Tv_scr)
                xl, Txl = xlb[it % 2]
                xl2, Txl2 = xlb[(it + 1) % 2]
                hcur, Thcur = hb[it % 2]
                hprev, Thprev = hb[(it + 1) % 2]
                for ch in range(4):
                    ps, Tps = psr.next()
                    mm_group(ps[:], Tps, lambda kc, ch=ch: win[:, kc, 1536 + ch * 128:1536 + (ch + 1) * 128],
                             lambda kc: xT[:, kc, :], lambda kc: [Twin[kc], TxT[kc]])
                    S.op("dve", lambda e, ps=ps, ch=ch, xl=xl: e.tensor_copy(out=xl[:, ch, 3:515], in_=ps[:]), [Tps], [Txl])
                S.op("pool", lambda e, xl=xl, xl2=xl2: e.tensor_copy(out=xl2[:, :, 0:3], in_=xl[:, :, 512:515]),
                     [Txl], [Txl2])
                for ch in range(4):
                    S.op("dve", lambda e, ch=ch, xl=xl: e.tensor_scalar(
                        out=xc[:, ch, :], in0=xl[:, ch, 3:515], scalar1=colp[:, 16 + ch * 4 + 3:16 + ch * 4 + 4],
                        scalar2=colp[:, 32 + ch:33 + ch], op0=ALU.mult, op1=ALU.add), [Txl, Tcolp], [Txc])
                    for w in (2, 1, 0):
                        S.op("dve", lambda e, ch=ch, w=w, xl=xl: e.scalar_tensor_tensor(
                            out=xc[:, ch, :], in0=xl[:, ch, w:w + 512], scalar=colp[:, 16 + ch * 4 + w:16 + ch * 4 + w + 1],
                            in1=xc[:, ch, :], op0=ALU.mult, op1=ALU.add), [Txl, Tcolp, Txc], [Txc])
                S.op("pool", lambda e: e.tensor_copy(out=xcb[:].rearrange("p a b -> p (a b)"),
                                                     in_=xc[:].rearrange("p a b -> p (a b)")), [Txc], [Txcb])
                for ch in range(4):
                    for m, (dstt, Tdst, bo) in enumerate(((rg, Trg, 36), (ig, Tig, 40))):
                        ps, Tps = psr.next()
                        S.op("pe", lambda e, ps=ps, m=m, ch=ch: e.matmul(ps[:], bd[:, m, ch, :], xcb[:, ch, :],
                                                                         start=True, stop=True), [Tbd, Txcb], [Tps])
                        S.op("act", lambda e, ps=ps, ch=ch, dstt=dstt, bo=bo: e.activation(
                            out=dstt[:, ch, :], in_=ps[:], func=AF.Sigmoid, bias=colp[:, bo + ch:bo + ch + 1], scale=1.0),
                            [Tps, Tcolp], [Tdst])
                for ch in range(4):
                    S.op("act", lambda e, ch=ch: e.activation(out=aa[:, ch, :], in_=rg[:, ch, :], func=AF.Exp,
                                                              scale=lrp[:, ch:ch + 1]), [Trg, Tlrp], [Taa])
                    S.op("act", lambda e, ch=ch: e.activation(out=sq[:, ch, :], in_=rg[:, ch, :], func=AF.Exp,
                                                              scale=lrp[:, 4 + ch:5 + ch]), [Trg, Tlrp], [Tsq])
                S.op("act", lambda e: e.activation(out=sq[:].rearrange("p a b -> p (a b)"),
                                                   in_=sq[:].rearrange("p a b -> p (a b)"), func=AF.Sqrt,
                                                   bias=c.onec[:, 0:1], scale=-1.0), [Tsq], [Tsq])
                S.op("pool", lambda e: e.tensor_tensor(out=ig[:].rearrange("p a b -> p (a b)"),
                                                       in0=ig[:].rearrange("p a b -> p (a b)"),
                                                       in1=xc[:].rearrange("p a b -> p (a b)"), op=ALU.mult),
                     [Tig, Txc], [Tig])
                S.op("pool", lambda e: e.tensor_tensor(out=ig[:].rearrange("p a b -> p (a b)"),
                                                       in0=ig[:].rearrange("p a b -> p (a b)"),
                                                       in1=sq[:].rearrange("p a b -> p (a b)"), op=ALU.mult),
                     [Tig, Tsq], [Tig])
                for ch in range(4):
                    init = 0.0 if it == 0 else hprev[:, ch, 511:512]
                    S.op("dve", lambda e, ch=ch, init=init, hcur=hcur: e.tensor_tensor_scan(
                        out=hcur[:, ch, :], data0=aa[:, ch, :], data1=ig[:, ch, :], initial=init,
                        op0=ALU.mult, op1=ALU.add), [Taa, Tig] + ([] if it == 0 else [Thprev]), [Thcur])
                S.op("dve", lambda e, hcur=hcur: e.tensor_scalar(out=hsel[:], in0=hcur[:, :, 0:128], scalar1=c.sel[:, 0:1],
                                                                 scalar2=None, op0=ALU.mult), [Thcur, Tcst], [Thsel])
                for t in (1, 2):
                    S.op("dve", lambda e, t=t, hcur=hcur: e.scalar_tensor_tensor(
                        out=hsel[:], in0=hcur[:, :, t * 128:(t + 1) * 128], scalar=c.sel[:, t:t + 1], in1=hsel[:],
                        op0=ALU.mult, op1=ALU.add), [Thcur, Tcst, Thsel], [Thsel])
                S.op("dve", lambda e, hcur=hcur, it=it: e.scalar_tensor_tensor(
                    out=c.recT[:, :, it * 128:(it + 1) * 128], in0=hcur[:, :, 384:512], scalar=c.sel[:, 3:4], in1=hsel[:],
                    op0=ALU.mult, op1=ALU.add), [Thcur, Tcst, Thsel], [TrecT])
            else:
                ot = it - 16
                for h in range(4):
                    ps, Tps = psr.next()
                    mm_group(ps[:], Tps, lambda kc, h=h: win[:, kc, h * 128:(h + 1) * 128],
                             lambda kc: xT[:, kc, :], lambda kc: [Twin[kc], TxT[kc]])
                    S.op("act", lambda e, ps=ps, h=h: e.mul(out=qst[0:64, h, 0, :], in_=ps[0:64, :], mul=0.125), [Tps], [Tqst])
                    S.op("dve", lambda e, ps=ps, h=h: e.tensor_scalar(out=qst[64:128, h, 1, :], in0=ps[64:128, :], scalar1=0.125,
                                                                      scalar2=None, op0=ALU.mult), [Tps], [Tqst])
                for h in range(4):
                    S.dma("sync", lambda e, h=h, ot=ot: e.dma_start(out=c.q_scr[h, :, :, ot * 512:(ot + 1) * 512],
                                                                     in_=qst[:, h, :, :]), [Tqst], [c.Tq_scr], sem_tile=c.Tq_scr)
                for ch in range(4):
                    ps, Tps = psr.next()
                    mm_group(ps[:], Tps, lambda kc, ch=ch: win[:, kc, 2048 + ch * 128:2048 + (ch + 1) * 128],
                             lambda kc: xT[:, kc, :], lambda kc: [Twin[kc], TxT[kc]])
                    S.op("act", lambda e, ps=ps: e.activation(out=ge[:], in_=ps[:], func=AF.Gelu), [Tps], [Tge])
                    S.op("dve", lambda e, ch=ch, ot=ot: e.tensor_tensor(
                        out=c.recT[:, ch, ot * 512:(ot + 1) * 512], in0=c.recT[:, ch, ot * 512:(ot + 1) * 512], in1=ge[:],
                        op=ALU.mult), [Tge, TrecT], [TrecT])

        for it in range(20):
            do_tile(it)
        with nc.Block() as blk:
            S.emit(blk)


def phase2(c):
    nc = c.nc
    with ExitStack() as es:
        S = Sched(nc, es, "p2")
        sb = lambda n, s, d: _sb(nc, es, n, s, d)
        kbt = sb("kbt_s", [128, 4, 64], F32); dq = sb("dq_s", [128, 4, 512], F32); mf = sb("mf_s", [128, 4, 4, 128], F32)
        lamt = sb("lamt", [128, 4, 64], F32); lw = sb("lw", [128, 8], F32); junk = sb("junk2", [128, 64], F32)
        Tc = T("c2"); Tlam = T("lam"); Tlw = T("lw"); Tjunk = T("junk"); TattT = T("attT"); Tcst = T("cst")
        Kr = Ring([(sb(f"Kh{i}", [128, SEQ], BF16), T(f"Kh{i}")) for i in range(2)])
        Vr = Ring([(sb(f"Vh{i}", [128, NBLK, 128], BF16), T(f"Vh{i}")) for i in range(2)])
        Qr = Ring([(sb(f"Qh{i}", [128, 2, OWN], BF16), T(f"Qh{i}")) for i in range(2)])
        sbr = Ring([(sb(f"sbs{i}", [128, 512], F32), T(f"sbs{i}")) for i in range(4)])
        pbr = Ring([(sb(f"pb{i}", [128, 512], BF16), T(f"pb{i}")) for i in range(6)])
        rz = [(sb(f"rz{i}", [128, 512], F32), T(f"rz{i}")) for i in range(2)]
        oo = [(sb(f"oo{i}", [128, 512], F32), T(f"oo{i}")) for i in range(2)]
        osq = sb("osq", [128, 512], F32); Tosq = T("osq")
        rst = sb("rst", [128, 512], F32); Trst = T("rst")
        Sr = Ring([(_ps(nc, es, f"S{i}", [128, 512], F32), T(f"S{i}")) for i in range(4)])
        Ab = [(_ps(nc, es, f"A{i}", [128, 512], F32), T(f"A{i}")) for i in range(2)]
        Zb = [(_ps(nc, es, f"Z{i}", [128, 512], F32), T(f"Z{i}")) for i in range(2)]
        colp = c.colp_sb

        S.dma("sync", lambda e: e.dma_start(out=kbt[:].rearrange("p a b -> p (a b)"), in_=c.kbt[:, :]), [], [Tc])
        S.dma("sync", lambda e: e.dma_start(out=dq[:].rearrange("p a b -> p (a b)"), in_=c.dq[:, :]), [], [Tc])
        S.dma("sync", lambda e: e.dma_start(out=mf[:].rearrange("p a b c -> p (a b c)"), in_=c.mfull[:, :]), [], [Tc])
        S.dma("sync", lambda e: e.dma_start(out=lamt[:].rearrange("p a b -> p (a b)"), in_=_bcast_rows(c.lamv, 128, 256)),
              [], [Tlam])
        for i in range(2):
            S.op("dve", lambda e, i=i: e.scalar_tensor_tensor(out=junk[:], in0=lamt[:, 2 * i, :], scalar=1.0,
                                                             in1=lamt[:, 2 * i + 1, :], op0=ALU.mult, op1=ALU.mult,
                                                             accum_out=lw[:, i:i + 1]), [Tlam], [Tjunk, Tlw])
        S.op("act", lambda e: e.activation(out=lw[:, 2:4], in_=lw[:, 0:2], func=AF.Exp), [Tlw], [Tlw])
        S.op("dve", lambda e: e.tensor_tensor(out=lw[:, 4:5], in0=lw[:, 3:4], in1=lw[:, 2:3], op=ALU.subtract), [Tlw], [Tlw])
        S.op("dve", lambda e: e.tensor_scalar(out=lw[:, 5:6], in0=lw[:, 4:5], scalar1=-LAM_INIT, scalar2=None, op0=ALU.add),
             [Tlw], [Tlw])
        S.op("dve", lambda e: e.tensor_scalar(out=lw[:, 6:7], in0=colp[:, 48:49], scalar1=(1.0 - LAM_INIT), scalar2=None,
                                              op0=ALU.mult), [], [Tlw])

        def head(h):
            Kh, TK = Kr.next(); Vh, TV = Vr.next(); Qh, TQ = Qr.next()
            S.dma("sync", lambda e: e.dma_start(out=Kh[:], in_=c.k_scr[h, :, :]), [], [TK])
            S.dma("sync", lambda e: e.dma_start(out=Vh[:], in_=c.v_scr[h, :, :, :]), [], [TV])
            S.dma("sync", lambda e: e.dma_start(out=Qh[:], in_=c.q_scr[h, :, :, :]), [], [TQ])

            def qtile(g):
                nj = 16 * g + 16
                pend = {}
                j0 = max(0, 16 * g - 1 - int(math.ceil(1.0 / SLOPES[h])))

                def scores(j):
                    c0 = 0 if j < 16 * g else 128 * ((j - 16 * g) // 4)
                    n = j - 16 * g + 48
                    ps_l = []
                    for m in range(2):
                        ps, Tps = Sr.next()
                        S.op("pe", lambda e, ps=ps, m=m: e.matmul(ps[:, c0:512], Kh[:, j * 128:(j + 1) * 128],
                                                                   Qh[:, m, g * 512 + c0:(g + 1) * 512], start=True, stop=True),
                             [TK, TQ], [Tps])
                        sbt, Tsb = sbr.next()
                        if j >= 16 * g:
                            jj = (j - 16 * g) % 4
                            S.op("dve", lambda e, ps=ps, sbt=sbt: e.tensor_tensor(out=sbt[:, c0:c0 + 128], in0=ps[:, c0:c0 + 128],
                                                                                  in1=mf[:, h, jj, :], op=ALU.add), [Tps, Tc], [Tsb])
                            if c0 + 128 < 512:
                                S.op("dve", lambda e, ps=ps, sbt=sbt: e.scalar_tensor_tensor(
                                    out=sbt[:, c0 + 128:512], in0=ps[:, c0 + 128:512], scalar=kbt[:, h, n:n + 1],
                                    in1=dq[:, h, c0 + 128:512], op0=ALU.add, op1=ALU.add), [Tps, Tc], [Tsb])
                        else:
                            S.op("dve", lambda e, ps=ps, sbt=sbt: e.scalar_tensor_tensor(
                                out=sbt[:, :], in0=ps[:, :], scalar=kbt[:, h, n:n + 1], in1=dq[:, h, :],
                                op0=ALU.add, op1=ALU.add), [Tps, Tc], [Tsb])
                        pb, Tpb = pbr.next()
                        S.op("act", lambda e, sbt=sbt, pb=pb: e.activation(out=pb[:, c0:512], in_=sbt[:, c0:512], func=AF.Exp),
                             [Tsb], [Tpb])
                        ps_l.append((pb, Tpb))
                    pend[j] = (c0, ps_l)

                def av(j):
                    c0, ps_l = pend.pop(j)
                    for m in range(2):
                        pb, Tpb = ps_l[m]
                        S.op("pe", lambda e, pb=pb, m=m: e.matmul(Ab[m][0][:, c0:512], Vh[:, j, :], pb[:, c0:512],
                                                                   start=(j == j0), stop=(j == nj - 1)), [TV, Tpb], [Ab[m][1]])
                        S.op("pe", lambda e, pb=pb, m=m: e.matmul(Zb[m][0][:, c0:512], c.ones_bf, pb[:, c0:512],
                                                                   start=(j == j0), stop=(j == nj - 1)), [Tcst, Tpb], [Zb[m][1]])

                j0 = max(0, 16 * g - 1 - int(math.ceil(1.0 / SLOPES[h])))
                for st in range(j0, nj + 1):
                    if st < nj:
                        scores(st)
                    if st >= j0 + 1:
                        av(st - 1)
                for m in range(2):
                    S.op("dve", lambda e, m=m: e.reciprocal(out=rz[m][0][:], in_=Zb[m][0][:]), [Zb[m][1]], [rz[m][1]])
                    S.op("dve", lambda e, m=m: e.tensor_tensor(out=oo[m][0][:], in0=Ab[m][0][:], in1=rz[m][0][:], op=ALU.mult),
                         [Ab[m][1], rz[m][1]], [oo[m][1]])
                S.op("dve", lambda e: e.scalar_tensor_tensor(out=oo[0][0][:], in0=oo[1][0][:], scalar=lw[:, 5:6], in1=oo[0][0][:],
                                                             op0=ALU.mult, op1=ALU.add), [oo[0][1], oo[1][1], Tlw], [oo[0][1]])
                S.op("pool", lambda e: e.tensor_tensor(out=osq[:], in0=oo[0][0][:], in1=oo[0][0][:], op=ALU.mult), [oo[0][1]], [Tosq])
                ps, Tps = Sr.next()
                S.op("pe", lambda e, ps=ps: e.matmul(ps[:], c.ones_f, osq[:], start=True, stop=True), [Tosq, Tcst], [Tps])
                S.op("act", lambda e, ps=ps: e.activation(out=rst[:], in_=ps[:], func=AF.Sqrt, bias=c.epsc[:, 0:1],
                                                          scale=1.0 / 128.0), [Tps, Tcst], [Trst])
                S.op("dve", lambda e: e.reciprocal(out=rst[:], in_=rst[:]), [Trst], [Trst])
                S.op("dve", lambda e: e.scalar_tensor_tensor(out=c.attT[:, h, g * 512:(g + 1) * 512], in0=oo[0][0][:],
                                                             scalar=lw[:, 6:7], in1=rst[:], op0=ALU.mult, op1=ALU.mult),
                     [oo[0][1], Trst, Tlw], [TattT])

            for g in range(4):
                qtile(g)

        for h in range(4):
            head(h)
        with nc.Block() as blk:
            S.emit(blk)


class LNbufs:
    def __init__(self, nc, es, tag):
        self.stt = _sb(nc, es, f"ln_stt_{tag}", [128, 12], F32); self.Tstt = T("stt")
        self.mv = _sb(nc, es, f"ln_mv_{tag}", [128, 2], F32); self.Tmv = T("mv")
        self.rs = _sb(nc, es, f"ln_rs_{tag}", [128, 1], F32); self.Trs = T("rs")
        self.nm = _sb(nc, es, f"ln_nm_{tag}", [128, 1], F32); self.Tnm = T("nm")
        self.tmp = _sb(nc, es, f"ln_tmp_{tag}", [128, D], F32); self.Ttmp = T("tmp")


def ln_tm(c, S, B, src, Tsrc, dst, Tdst, grow, brow, Trows):
    for hh in range(2):
        S.op("dve", lambda e, hh=hh: e.bn_stats(out=B.stt[:, hh * 6:(hh + 1) * 6], in_=src[:, hh * 512:(hh + 1) * 512]),
             [Tsrc], [B.Tstt])
    S.op("dve", lambda e: e.bn_aggr(out=B.mv[:], in_=B.stt[:]), [B.Tstt], [B.Tmv])
    S.op("act", lambda e: e.activation(out=B.rs[:], in_=B.mv[:, 1:2], func=AF.Sqrt, bias=c.epsc[:, 0:1], scale=1.0),
         [B.Tmv], [B.Trs])
    S.op("dve", lambda e: e.reciprocal(out=B.rs[:], in_=B.rs[:]), [B.Trs], [B.Trs])
    S.op("dve", lambda e: e.scalar_tensor_tensor(out=B.nm[:], in0=B.mv[:, 0:1], scalar=-1.0, in1=B.rs[:],
                                                 op0=ALU.mult, op1=ALU.mult), [B.Tmv, B.Trs], [B.Tnm])
    S.op("act", lambda e: e.activation(out=B.tmp[:], in_=src[:], func=AF.Identity, scale=B.rs[:, 0:1], bias=B.nm[:, 0:1]),
         [Tsrc, B.Trs, B.Tnm], [B.Ttmp])
    S.op("dve", lambda e: e.tensor_tensor(out=B.tmp[:], in0=B.tmp[:], in1=grow, op=ALU.mult), [B.Ttmp, Trows], [B.Ttmp])
    S.op("pool", lambda e: e.tensor_tensor(out=dst[:], in0=B.tmp[:], in1=brow, op=ALU.add), [B.Ttmp, Trows], [Tdst])


def phase3(c):
    nc = c.nc
    with ExitStack() as es:
        S = Sched(nc, es, "p3")
        rc = {}
        sb = lambda n, s, d: _sb(nc, es, n, s, d)
        wo = sb("wo", [128, 8, D], BF16); Two = [T(f"wo{k}") for k in range(8)]
        wost = Ring([(sb(f"wost{i}", [128, D], F32), T(f"wost{i}")) for i in range(2)])
        wr = sb("wr", [128, 8, NE], F32); Twr = T("wr")
        rowsb = sb("rowsb", [128, 4, D], F32); Trows = T("rows")
        brt = sb("brt", [128, NE], F32); Tbrt = T("brt")
        msk = sb("msk", [128, NOB, NE], BF16); Tmsk = [T(f"msk{i}") for i in range(NOB)]
        xs = Ring([(sb(f"xs3_{i}", [128, D], F32), T(f"xs3_{i}")) for i in range(2)])
        x0 = sb("x0", [128, D], F32); Tx0 = T("x0")
        yy = sb("yy", [128, D], F32); Tyy = T("yy")
        x1r = Ring([(sb(f"x1_{i}", [128, D], F32), T(f"x1_{i}")) for i in range(2)])
        x1br = Ring([(sb(f"x1b_{i}", [128, D], BF16), T(f"x1b_{i}")) for i in range(2)])
        x1T = sb("x1T", [128, 8, 128], F32); Tx1T = T("x1T")
        lg = sb("lg", [128, NE], F32); Tlg = T("lg")
        t8 = sb("t8", [128, 8], F32); Tt8 = T("t8")
        sm = sb("sm3", [128, 16], F32); Tsm = T("sm3")
        destf = sb("destf", [128, NE], F32); Tdestf = T("destf")
        junk = sb("junk3", [128, NE], F32); Tjunk = T("junk3")
        B0 = LNbufs(nc, es, "a"); B1 = LNbufs(nc, es, "b")
        mixr = Ring([(_ps(nc, es, f"mix{i}", [128, 512], F32), T(f"mix{i}")) for i in range(4)])
        tpf = [(_ps(nc, es, f"tpf{i}", [128, 512], F32), T(f"tpf{i}")) for i in range(2)]
        lgp = _ps(nc, es, "lgp", [128, 512], F32); Tlgp = T("lgp")
        posp = _ps(nc, es, "posp", [128, 512], F32); Tposp = T("posp")
        Tcst = T("cst"); Tatt = T("att"); Trec = T("rec"); Tgts = T("gts"); Tdst = T("dst")
        Txg = T("xg_scr"); Tx1s = T("x1_scr")

        for i in range(4):
            S.dma("sync", lambda e, i=i: e.dma_start(out=rowsb[:, i, :], in_=_bcast_rows(c.rows, 128, D, offset=i * D)),
                  [], [Trows])
        S.dma("sync", lambda e: e.dma_start(out=brt[:], in_=_bcast_rows(c.b_router, 128, NE)), [], [Tbrt])
        for kc in range(8):
            S.dma("sync", lambda e, kc=kc: e.dma_start(out=wr[:, kc, :], in_=c.w_router[kc * 128:(kc + 1) * 128, :]), [], [Twr])
            st, Tst = wost.next()
            S.dma("sync", lambda e, st=st, kc=kc: e.dma_start(out=st[:], in_=c.w_out[kc * 128:(kc + 1) * 128, :]), [], [Tst])
            if kc % 2:
                S.op("act", lambda e, st=st, kc=kc: e.copy(out=wo[:, kc, :], in_=st[:]), [Tst], [Two[kc]])
            else:
                S.op("pool", lambda e, st=st, kc=kc: e.tensor_copy(out=wo[:, kc, :], in_=st[:]), [Tst], [Two[kc]])

        def block(ob):
            cs = slice(ob * 128, (ob + 1) * 128)
            xt, Txt = xs.next()
            S.dma("sync", lambda e: e.dma_start(out=xt[:], in_=c.x_own[cs, :]), [], [Txt])
            ln_tm(c, S, B0, xt, Txt, x0, Tx0, rowsb[:, 0, :], rowsb[:, 1, :], Trows)
            for hf in range(2):
                ps, Tps = mixr.next()
                for kc in range(8):
                    lhs = c.attT[:, kc, cs] if kc < 4 else c.recT[:, kc - 4, cs]
                    S.op("pe", lambda e, ps=ps, lhs=lhs, kc=kc, hf=hf: e.matmul(ps[:], lhs, wo[:, kc, hf * 512:(hf + 1) * 512],
                                                                              start=(kc == 0), stop=(kc == 7)),
                         [Two[kc], Tatt, Trec], [Tps])
                S.op("dve", lambda e, ps=ps, hf=hf: e.scalar_tensor_tensor(
                    out=yy[:, hf * 512:(hf + 1) * 512], in0=x0[:, hf * 512:(hf + 1) * 512], scalar=float(ALPHA), in1=ps[:],
                    op0=ALU.mult, op1=ALU.add), [Tx0, Tps], [Tyy])
            x1, Tx1 = x1r.next()
            ln_tm(c, S, B1, yy, Tyy, x1, Tx1, rowsb[:, 2, :], rowsb[:, 3, :], Trows)
            S.dma("sync", lambda e: e.dma_start(out=c.x1_scr[cs, :], in_=x1[:]), [Tx1], [Tx1s], sem_tile=Tx1s)
            x1b, Tx1b = x1br.next()
            S.op("act", lambda e: e.copy(out=x1b[:], in_=x1[:]), [Tx1], [Tx1b])
            for kc in range(8):
                pt, Tpt = tpf[kc // 4]
                S.op("pe", lambda e, pt=pt, kc=kc: e.transpose(out=pt[:, (kc % 4) * 128:(kc % 4 + 1) * 128],
                                                               in_=x1[:, kc * 128:(kc + 1) * 128], identity=c.ident_f),
                     [Tx1, Tcst], [Tpt])
            S.op("act", lambda e: e.copy(out=x1T[:, 0:4, :].rearrange("p a b -> p (a b)"), in_=tpf[0][0][:]), [tpf[0][1]], [Tx1T])
            S.op("dve", lambda e: e.tensor_copy(out=x1T[:, 4:8, :].rearrange("p a b -> p (a b)"), in_=tpf[1][0][:]),
                 [tpf[1][1]], [Tx1T])
            for kc in range(8):
                S.op("pe", lambda e, kc=kc: e.matmul(lgp[:, 0:NE], x1T[:, kc, :], wr[:, kc, :], start=(kc == 0), stop=(kc == 7)),
                     [Tx1T, Twr], [Tlgp])
            S.op("dve", lambda e: e.tensor_tensor(out=lg[:], in0=lgp[:, 0:NE], in1=brt[:], op=ALU.add), [Tlgp, Tbrt], [Tlg])
            S.op("dve", lambda e: e.max(out=t8[:], in_=lg[:]), [Tlg], [Tt8])
            S.op("dve", lambda e: e.tensor_scalar(out=sm[:, 0:1], in0=t8[:, 0:1], scalar1=-1.0, scalar2=None, op0=ALU.mult),
                 [Tt8], [Tsm])
            S.op("act", lambda e: e.activation(out=sm[:, 4:8], in_=t8[:, 0:4], func=AF.Exp, bias=sm[:, 0:1], scale=1.0,
                                               accum_out=sm[:, 1:2]), [Tt8, Tsm], [Tsm])
            S.op("dve", lambda e: e.reciprocal(out=sm[:, 1:2], in_=sm[:, 1:2]), [Tsm], [Tsm])
            S.op("dve", lambda e: e.tensor_scalar(out=c.gts[:, ob, :], in0=sm[:, 4:8], scalar1=sm[:, 1:2], scalar2=None,
                                                  op0=ALU.mult), [Tsm], [Tgts])
            S.op("dve", lambda e: e.tensor_scalar(out=msk[:, ob, :], in0=lg[:], scalar1=t8[:, 3:4], scalar2=None, op0=ALU.is_ge),
                 [Tlg, Tt8], [Tmsk[ob]])
            for o2 in range(ob + 1):
                lhs = c.ltri_bf if o2 == ob else c.ones_bf
                S.op("pe", lambda e, lhs=lhs, o2=o2: e.matmul(posp[:, 0:NE], lhs, msk[:, o2, :], start=(o2 == 0), stop=(o2 == ob)),
                     [Tmsk[o2], Tcst], [Tposp])
            S.op("dve", lambda e: e.tensor_tensor(out=destf[:], in0=posp[:, 0:NE], in1=c.ecap, op=ALU.add), [Tposp, Tcst], [Tdestf])
            for k in range(4):
                S.op("dve", lambda e, k=k: e.scalar_tensor_tensor(out=junk[:], in0=lg[:], scalar=t8[:, k:k + 1], in1=destf[:],
                                                                 op0=ALU.is_equal, op1=ALU.mult, accum_out=sm[:, 8 + k:9 + k]),
                     [Tlg, Tt8, Tdestf], [Tjunk, Tsm])
            S.op("dve", lambda e: e.tensor_copy(out=c.dst[:, ob, :], in_=sm[:, 8:12]), [Tsm], [Tdst])
            for k in range(4):
                S.dma("pool", lambda e, k=k: e.indirect_dma_start(
                    out=c.xg_scr[:, :], out_offset=bass.IndirectOffsetOnAxis(ap=c.dst[:, ob, k:k + 1], axis=0),
                    in_=x1b[:, :], in_offset=None, bounds_check=_breg(e, rc), oob_is_err=False), [Tx1b, Tdst], [Txg], sem_tile=Txg)

        for ob in range(NOB):
            block(ob)
        with nc.Block() as blk:
            S.emit(blk)


def phase4(c):
    nc = c.nc
    with ExitStack() as es:
        S = Sched(nc, es, "p4")
        sb = lambda n, s, d: _sb(nc, es, n, s, d)
        wg = [(sb(f"wg{i}", [128, 8, 2048], BF16), [T(f"wg{i}_{k}") for k in range(8)]) for i in range(2)]
        wd = [(sb(f"wd{i}", [128, 8, D], BF16), [T(f"wd{i}_{k}") for k in range(8)]) for i in range(2)]
        stg = Ring([(sb(f"stg{i}", [128, 2048], F32), T(f"stg{i}")) for i in range(3)])
        xgt = sb("xgt", [128, 3, D], BF16); Txgt = T("xgt")
        xgT = [(sb(f"xgT{i}", [128, 8, CAP], BF16), T(f"xgT{i}")) for i in range(2)]
        actT = [(sb(f"actT{i}", [128, 8, CAP], BF16), T(f"actT{i}")) for i in range(2)]
        glu = Ring([(sb(f"glu{i}", [128, CAP], F32), T(f"glu{i}")) for i in range(2)])
        sg = Ring([(sb(f"sg{i}", [128, CAP], F32), T(f"sg{i}")) for i in range(2)])
        lin = Ring([(sb(f"lin{i}", [128, CAP], F32), T(f"lin{i}")) for i in range(2)])
        ysb = Ring([(sb(f"ysb{i}", [128, D], F32), T(f"ysb{i}")) for i in range(2)])
        bdf = sb("bdf", [1, D], F32); Tbdf = T("bdf")
        bdb = sb("bdb", [128, D], BF16); Tbdb = T("bdb")
        bg = sb("bg", [128, NE * 16], F32); Tbg = T("bg")
        bl1 = sb("bl1", [128, NE * 16], F32); Tbl1 = T("bl1")
        tpl = [_ps(nc, es, f"tp4_{i}", [128, 1024], BF16) for i in range(2)]; Ttp = [T("tp4a"), T("tp4b")]
        psg = Ring([(_ps(nc, es, f"psg{i}", [128, 512], F32), T(f"psg{i}")) for i in range(2)])
        psl = Ring([(_ps(nc, es, f"psl{i}", [128, 512], F32), T(f"psl{i}")) for i in range(2)])
        psy = Ring([(_ps(nc, es, f"psy{i}", [128, 512], F32), T(f"psy{i}")) for i in range(2)])
        Tcst = T("cst"); Tys = T("y_scr")
        S.dma("sync", lambda e: e.dma_start(out=bg[:], in_=c.bgu[:, :]), [], [Tbg])
        S.op("pool", lambda e: e.memset(bdb[:], 0.0), [], [Tbdb])
        S.op("pool", lambda e: e.tensor_scalar(out=bl1[:], in0=bg[:], scalar1=1.0, scalar2=None, op0=ALU.add), [Tbg], [Tbl1])
        cast_cycle = ["act", "dve", "act", "act", "dve", "act", "act", "dve"]
        cc = [0]

        def cast(out, in_, reads, writes):
            eng = cast_cycle[cc[0] % len(cast_cycle)]
            cc[0] += 1
            if eng == "act":
                S.op("act", lambda e: e.copy(out=out, in_=in_), reads, writes)
            else:
                S.op(eng, lambda e: e.tensor_copy(out=out, in_=in_), reads, writes)

        def load_weights(e_):
            wgt, Twg = wg[e_ % 2]
            wdt, Twd = wd[e_ % 2]
            for kc in range(8):
                st, Tst = stg.next()
                S.dma("sync", lambda e, st=st, kc=kc: e.dma_start(out=st[:], in_=c.w_gu[e_, kc * 128:(kc + 1) * 128, :]), [], [Tst])
                v = st[:].rearrange("p (f two) -> p two f", two=2)
                cast(wgt[:, kc, 0:1024], v[:, 0, :], [Tst], [Twg[kc]])
                cast(wgt[:, kc, 1024:2048], v[:, 1, :], [Tst], [Twg[kc]])
            for kc in range(0, 8, 2):
                st, Tst = stg.next()
                S.dma("sync", lambda e, st=st, kc=kc: e.dma_start(
                    out=st[:].rearrange("p (a d) -> p a d", a=2),
                    in_=c.w_down[e_, kc * 128:(kc + 2) * 128, :].rearrange("(a p) d -> p a d", p=128)), [], [Tst])
                cast(wdt[:, kc, :], st[:, 0:D], [Tst], [Twd[kc]])
                cast(wdt[:, kc + 1, :], st[:, D:2 * D], [Tst], [Twd[kc + 1]])

        def expert(e_):
            wgt, Twg = wg[e_ % 2]
            wdt, Twd = wd[e_ % 2]
            xT_, TxT_ = xgT[e_ % 2]
            aT, TaT = actT[e_ % 2]
            S.dma("sync", lambda e: e.dma_start(out=xgt[:], in_=c.xg_scr[e_ * CAP:(e_ + 1) * CAP, :].rearrange("(s p) d -> p s d", p=128)),
                  [], [Txgt])
            S.dma("sync", lambda e: e.dma_start(out=bdf[:], in_=c.b_down[e_:e_ + 1, :]), [], [Tbdf])
            S.op("act", lambda e: e.copy(out=bdb[0:1, :], in_=bdf[:]), [Tbdf], [Tbdb])
            for kc in range(8 if c.p4mask & 2 else 0):
                hf = kc % 2
                for sc in range(3):
                    S.op("pe", lambda e, kc=kc, sc=sc, hf=hf: e.transpose(
                        out=tpl[hf][:, sc * 128:(sc + 1) * 128], in_=xgt[:, sc, kc * 128:(kc + 1) * 128],
                        identity=c.ident_bf), [Txgt, Tcst], [Ttp[hf]])
                if kc % 2:
                    S.op("act", lambda e, kc=kc, hf=hf: e.copy(out=xT_[:, kc, :], in_=tpl[hf][:, 0:CAP]), [Ttp[hf]], [TxT_])
                else:
                    S.op("dve", lambda e, kc=kc, hf=hf: e.tensor_copy(out=xT_[:, kc, :], in_=tpl[hf][:, 0:CAP]),
                         [Ttp[hf]], [TxT_])
            for fc in range(8 if c.p4mask & 4 else 0):
                pg, Tpg = psg.next()
                pl, Tpl = psl.next()
                for kc in range(8):
                    S.op("pe", lambda e, pg=pg, kc=kc, fc=fc: e.matmul(pg[:, 0:CAP], wgt[:, kc, fc * 128:(fc + 1) * 128], xT_[:, kc, :],
                                                                     start=(kc == 0), stop=(kc == 7)), [Twg[kc], TxT_], [Tpg])
                for kc in range(8):
                    S.op("pe", lambda e, pl=pl, kc=kc, fc=fc: e.matmul(pl[:, 0:CAP], wgt[:, kc, 1024 + fc * 128:1024 + (fc + 1) * 128],
                                                                     xT_[:, kc, :], start=(kc == 0), stop=(kc == 7)),
                         [Twg[kc], TxT_], [Tpl])
                gl_, Tgl = glu.next(); sg_, Tsg = sg.next(); ln_, Tln = lin.next()
                col = e_ * 16 + fc
                S.op("dve", lambda e, pg=pg, gl_=gl_, col=col: e.tensor_scalar(out=gl_[:], in0=pg[:, 0:CAP], scalar1=bg[:, col:col + 1],
                                                                              scalar2=7.0, op0=ALU.add, op1=ALU.min), [Tpg, Tbg], [Tgl])
                S.op("act", lambda e, gl_=gl_, sg_=sg_: e.activation(out=sg_[:], in_=gl_[:], func=AF.Sigmoid, scale=1.702), [Tgl], [Tsg])
                S.op("dve", lambda e, pl=pl, ln_=ln_, col=col: e.tensor_scalar(out=ln_[:], in0=pl[:, 0:CAP], scalar1=bl1[:, col + 8:col + 9],
                                                                              scalar2=-6.0, op0=ALU.add, op1=ALU.max), [Tpl, Tbl1], [Tln])
                S.op("pool", lambda e, gl_=gl_, sg_=sg_: e.tensor_tensor(out=sg_[:], in0=gl_[:], in1=sg_[:], op=ALU.mult), [Tgl, Tsg], [Tsg])
                S.op("dve", lambda e, ln_=ln_, sg_=sg_, fc=fc: e.scalar_tensor_tensor(out=aT[:, fc, :], in0=ln_[:], scalar=8.0, in1=sg_[:],
                                                                                     op0=ALU.min, op1=ALU.mult), [Tln, Tsg], [TaT])
            for sc in range(3 if c.p4mask & 16 else 0):
                yt, Tyt = ysb.next()
                for hf in range(2):
                    py, Tpy = psy.next()
                    for fc in range(8):
                        S.op("pe", lambda e, py=py, fc=fc, hf=hf, sc=sc: e.matmul(py[:], aT[:, fc, sc * 128:(sc + 1) * 128],
                                                                                wdt[:, fc, hf * 512:(hf + 1) * 512],
                                                                                start=(fc == 0), stop=False), [TaT, Twd[fc]], [Tpy])
                    S.op("pe", lambda e, py=py, hf=hf: e.matmul(py[:], c.row0_bf, bdb[:, hf * 512:(hf + 1) * 512],
                                                               start=False, stop=True), [Tcst, Tbdb], [Tpy])
                    if hf:
                        S.op("act", lambda e, py=py, yt=yt, hf=hf: e.copy(out=yt[:, hf * 512:(hf + 1) * 512], in_=py[:]), [Tpy], [Tyt])
                    else:
                        S.op("dve", lambda e, py=py, yt=yt, hf=hf: e.tensor_copy(out=yt[:, hf * 512:(hf + 1) * 512], in_=py[:]), [Tpy], [Tyt])
                S.dma("sync", lambda e, yt=yt, sc=sc: e.dma_start(out=c.y_scr[e_ * CAP + sc * 128:e_ * CAP + (sc + 1) * 128, :], in_=yt[:]),
                      [Tyt], [Tys], sem_tile=Tys)

        load_weights(0)
        for e_ in range(c.nexp):
            if e_ + 1 < c.nexp:
                load_weights(e_ + 1)
            expert(e_)
        with nc.Block() as blk:
            S.emit(blk)


def phase5(c):
    nc = c.nc
    with ExitStack() as es:
        S = Sched(nc, es, "p5")
        rc = {}
        sb = lambda n, s, d: _sb(nc, es, n, s, d)
        rowsb = sb("rows5", [128, 2, D], F32); Trows = T("rows5")
        yk = [Ring([(sb(f"yk{k}_{i}", [128, D], F32), T(f"yk{k}_{i}")) for i in range(2)]) for k in range(4)]
        x1r = Ring([(sb(f"x15_{i}", [128, D], F32), T(f"x15_{i}")) for i in range(2)])
        acc = sb("acc5", [128, D], F32); Tacc = T("acc5")
        outr = Ring([(sb(f"o5_{i}", [128, D], F32), T(f"o5_{i}")) for i in range(2)])
        B = LNbufs(nc, es, "c")
        Tgts = T("gts"); Tdst = T("dst"); Tout = T("out")
        for i in range(2):
            S.dma("sync", lambda e, i=i: e.dma_start(out=rowsb[:, i, :], in_=_bcast_rows(c.rows, 128, D, offset=(4 + i) * D)),
                  [], [Trows])

        def block(ob):
            cs = slice(ob * 128, (ob + 1) * 128)
            ys = []
            for k in range(4):
                yt, Tyt = yk[k].next()
                ys.append((yt, Tyt))
                S.dma("pool", lambda e, yt=yt, k=k: e.indirect_dma_start(
                    out=yt[:, :], out_offset=None, in_=c.y_scr[:, :],
                    in_offset=bass.IndirectOffsetOnAxis(ap=c.dst[:, ob, k:k + 1], axis=0),
                    bounds_check=_breg(e, rc), oob_is_err=False), [Tdst], [Tyt])
            x1, Tx1 = x1r.next()
            S.dma("sync", lambda e: e.dma_start(out=x1[:], in_=c.x1_scr[cs, :]), [], [Tx1])
            S.op("dve", lambda e: e.tensor_scalar(out=acc[:], in0=ys[0][0][:], scalar1=c.gts[:, ob, 0:1], scalar2=None, op0=ALU.mult),
                 [ys[0][1], Tgts], [Tacc])
            for k in range(1, 4):
                S.op("dve", lambda e, k=k: e.scalar_tensor_tensor(out=acc[:], in0=ys[k][0][:], scalar=c.gts[:, ob, k:k + 1], in1=acc[:],
                                                                 op0=ALU.mult, op1=ALU.add), [ys[k][1], Tgts, Tacc], [Tacc])
            S.op("dve", lambda e: e.scalar_tensor_tensor(out=acc[:], in0=x1[:], scalar=float(ALPHA), in1=acc[:],
                                                         op0=ALU.mult, op1=ALU.add), [Tx1, Tacc], [Tacc])
            ot, Tot = outr.next()
            ln_tm(c, S, B, acc, Tacc, ot, Tot, rowsb[:, 0, :], rowsb[:, 1, :], Trows)
            S.dma("sync", lambda e: e.dma_start(out=c.out[cs, :], in_=ot[:]), [Tot], [Tout], sem_tile=Tout)

        for ob in range(NOB):
            block(ob)
        with nc.Block() as blk:
            S.emit(blk)


def _bf(a):
    return np.ascontiguousarray(a).astype(ml_dtypes.bfloat16)


def make_core_consts(r):
    f = np.float32
    p = np.arange(128, dtype=np.float64)
    kbt = np.zeros((128, 4, 64), f)
    dq = np.zeros((128, 4, 512), f)
    mfull = np.zeros((128, 4, 4, 128), f)
    q = np.arange(512)
    for h, sl in enumerate(SLOPES):
        for n in range(64):
            kbt[:, h, n] = sl * (128.0 * (n - 48 - r) + p)
        dq[:, h, :] = (-sl * (512.0 * (q // 128) + (q % 128)))[None, :]
        for jj in range(4):
            kpos = 128 * jj + np.arange(128)[:, None]
            qpos = 128 * r + np.arange(128)[None, :]
            allowed = (kpos // 64) <= (qpos // 64)
            mfull[:, h, jj, :] = np.where(allowed, -sl * np.abs(qpos - kpos), NEG)
    ident = np.eye(128, dtype=f)
    ones = np.ones((128, 128), f)
    ltri = (np.arange(128)[:, None] < np.arange(128)[None, :]).astype(f)
    row0 = np.zeros((128, 128), f)
    row0[0, :] = 1.0
    cst_bf = _bf(np.concatenate([ident, ones, ltri, row0], axis=1))
    cst_f = np.zeros((128, NCF), f)
    cst_f[:, 0:128] = ident
    cst_f[:, 128:256] = 1.0
    cst_f[:, 256:256 + NE] = (np.arange(NE) * CAP)[None, :]
    cst_f[:, 256 + NE + r] = 1.0
    cst_f[:, 292] = LN_EPS
    cst_f[:, 293] = 1.0
    cst_f[:, 294] = -0.5
    return {"kbt": kbt.reshape(128, -1), "dq": dq.reshape(128, -1), "mfull": mfull.reshape(128, -1),
            "cst_bf": cst_bf, "cst_f": cst_f}


def make_in_maps(inp, ne_w=NE):
    f = np.float32
    g = lambda k: np.asarray(inp[k], dtype=f)
    x = g("x")
    colp = np.zeros((128, 64), f)
    chunk = lambda v, n: np.asarray(v, f).reshape(n, 128).T
    colp[:, 0:8] = chunk(g("ln0_g"), 8)
    colp[:, 8:16] = chunk(g("ln0_b"), 8)
    cw = g("conv_w")[0]
    for ch in range(4):
        for w in range(4):
            colp[:, 16 + ch * 4 + w] = cw[w, ch * 128:(ch + 1) * 128]
    colp[:, 32:36] = chunk(g("conv_b")[0], 4)
    colp[:, 36:40] = chunk(g("b_rg_a")[0].reshape(512), 4)
    colp[:, 40:44] = chunk(g("b_rg_x")[0].reshape(512), 4)
    colp[:, 44:48] = chunk(g("lru_lambda")[0], 4)
    colp[:, 48] = g("subln_g")[0]
    rows = np.zeros((8, D), f)
    rows[0], rows[1] = g("ln0_g"), g("ln0_b")
    rows[2], rows[3] = g("ln1_g")[0], g("ln1_b")[0]
    rows[4], rows[5] = g("ln2_g")[0], g("ln2_b")[0]
    bgu = g("b_gu")[0]
    bg = bgu[:, 0::2].reshape(NE, 8, 128)
    bl = bgu[:, 1::2].reshape(NE, 8, 128)
    bgu_l = np.concatenate([bg, bl], axis=1).transpose(2, 0, 1).reshape(128, NE * 16)
    shared = {
        "w_in": np.ascontiguousarray(g("w_in")[0]), "w_out": np.ascontiguousarray(g("w_out")[0]),
        "w_router": np.ascontiguousarray(g("w_router")[0]), "w_gu": np.ascontiguousarray(g("w_gu")[0][:ne_w]),
        "w_down": np.ascontiguousarray(g("w_down")[0][:ne_w]), "b_down": np.ascontiguousarray(g("b_down")[0]),
        "rows": rows, "colp": colp,
        "w_rg": np.ascontiguousarray(np.stack([g("w_rg_a")[0], g("w_rg_x")[0]])),
        "lamv": np.ascontiguousarray(np.stack([g("lam_q1")[0], g("lam_k1")[0], g("lam_q2")[0], g("lam_k2")[0]])),
        "b_router": np.ascontiguousarray(g("b_router")), "bgu": np.ascontiguousarray(bgu_l),
    }
    maps = []
    for core in range(NCORES):
        b, r = core // 4, core % 4
        m = dict(shared)
        m["x_full"] = np.ascontiguousarray(x[b])
        m["x_own"] = np.ascontiguousarray(x[b].reshape(NOB, 4, 128, D)[:, r].reshape(OWN, D))
        m.update(make_core_consts(r))
        maps.append(m)
    return maps


def assemble(results):
    out = np.zeros((2, SEQ, D), np.float32)
    for core in range(NCORES):
        b, r = core // 4, core % 4
        o = np.asarray(results[core]["out"], np.float32).reshape(NOB, 128, D)
        out[b].reshape(NOB, 4, 128, D)[:, r] = o
    return out


_NC_CACHE = {}


def kernel(**inputs):
    if "nc" not in _NC_CACHE:
        _NC_CACHE["nc"] = build_program()
    nc = _NC_CACHE["nc"]
    maps = make_in_maps(inputs)
    res = run_bass_kernel_spmd(nc, maps, core_ids=list(range(NCORES)))
    return assemble(res.results)
```

```python
import math
from contextlib import ExitStack

import numpy as np
import ml_dtypes
import concourse.bass as bass
import concourse.mybir as mybir
from concourse.bass_utils import run_bass_kernel_spmd

F32 = mybir.dt.float32
BF16 = mybir.dt.bfloat16
U32 = mybir.dt.uint32
AF = mybir.ActivationFunctionType
ALU = mybir.AluOpType

NCORES = 8
D = 1024
SEQ = 8192
NBLK = SEQ // 128
OWN = 2048
NOB = OWN // 128
NE = 32
CAP = 384
DEPTH = 1
ALPHA = (2.0 * DEPTH) ** 0.25
LN_EPS = 1e-5
LAM_INIT = 0.8 - 0.6 * math.exp(-0.3 * 0)
SLOPES = [2.0 ** (-8.0 * (i + 1) / 4) for i in range(4)]
NEG = -30000.0
NCF = 128 + 128 + NE + 4 + 4


class T:
    __slots__ = ("name", "w", "r", "dsem", "dcnt")

    def __init__(self, name):
        self.name = name
        self.w = None
        self.r = {}
        self.dsem = None
        self.dcnt = 0


ENGS = ("sync", "pe", "act", "dve", "pool")


SEM_STACK = [None]


class Sched:
    def __init__(self, nc, es, tag):
        self.nc = nc
        self.es = SEM_STACK[0]
        self.tag = tag
        self.q = {e: [] for e in ENGS}
        self.sem = {}
        self.cnt = {}
        for e in ("pe", "act", "dve", "pool"):
            self.sem[e] = self.es.enter_context(nc.semaphore(f"{tag}_{e}"))
            self.cnt[e] = 0
        self.seen = {e: {} for e in ENGS}
        self.semobj = {e: self.sem[e] for e in self.sem}
        self.dma_tiles = []
        self.nsem = 4

    def _waits(self, eng, reads, writes, strict=False):
        deps = {}

        def add(m, same_ok):
            if m is None:
                return
            k, v = m
            if k == eng and not (same_ok or strict):
                return
            if deps.get(k, 0) < v:
                deps[k] = v

        for t in reads:
            add(t.w, eng != "pe")
        for t in writes:
            add(t.w, False)
            for k, v in t.r.items():
                add((k, v), False)
        out = []
        for k, v in deps.items():
            if self.seen[eng].get(k, 0) >= v:
                continue
            self.seen[eng][k] = v
            out.append((self.semobj[k], v))
        return out

    def _mark(self, mark, reads, writes):
        k, v = mark
        for t in reads:
            if t.r.get(k, 0) < v:
                t.r[k] = v
        for t in writes:
            t.w = mark
            t.r = {}

    def op(self, eng, fn, reads=(), writes=()):
        waits = self._waits(eng, reads, writes)
        self.cnt[eng] += 1
        mark = (eng, self.cnt[eng])
        self.q[eng].append((waits, fn, (self.sem[eng], 1)))
        self._mark(mark, reads, writes)

    def dma(self, queue, fn, reads, writes, sem_tile=None):
        st = sem_tile if sem_tile is not None else writes[0]
        if st.dsem is None:
            st.dsem = self.es.enter_context(self.nc.semaphore(f"{self.tag}_d{self.nsem}"))
            self.nsem += 1
            self.semobj[id(st)] = st.dsem
            self.dma_tiles.append(st)
        waits = self._waits(queue, reads, writes, strict=True)
        st.dcnt += 16
        mark = (id(st), st.dcnt)
        self.q[queue].append((waits, fn, (st.dsem, 16)))
        if queue == "pool":
            pass
        self._mark(mark, reads, writes)

    def emit(self, block):
        final = [(t.dsem, t.dcnt) for t in self.dma_tiles]
        self.q["sync"].append((final, None, None))
        table = (("sync", block.sync), ("pe", block.tensor), ("act", block.scalar),
                 ("dve", block.vector), ("pool", block.gpsimd))
        for name, deco in table:
            ops = self.q[name]

            def body(eng, ops=ops):
                for waits, fn, inc in ops:
                    for s, v in waits:
                        eng.wait_ge(s, v)
                    if fn is not None:
                        ins = fn(eng)
                        if inc is not None:
                            ins.then_inc(inc[0], inc[1])

            deco(body)


class Ring:
    def __init__(self, bufs):
        self.bufs = bufs
        self.i = 0

    def next(self):
        b = self.bufs[self.i % len(self.bufs)]
        self.i += 1
        return b


class Ctx:
    pass


def _sb(nc, es, name, shape, dt):
    return es.enter_context(nc.sbuf_tensor(name, shape, dt))


def _ps(nc, es, name, shape, dt):
    return es.enter_context(nc.psum_tensor(name, shape, dt))


def _breg(e, cache):
    if "r" not in cache:
        cache["r"] = e.to_reg(NE * CAP - 1)
    return cache["r"]


def _bcast_rows(handle, nrows, ncols, offset=0):
    return bass.AP(handle, offset, [[0, nrows], [1, ncols]])


def build_program(debug=False, upto=5, nexp=NE, p4mask=31, only=None):
    nc = bass.Bass("TRN2", target_bir_lowering=False)
    c = Ctx()
    c.nc = nc
    dk = "ExternalOutput" if debug else "Internal"

    def din(name, shape, dt=F32):
        return nc.dram_tensor(name, list(shape), dt, kind="ExternalInput")

    c.x_full = din("x_full", [SEQ, D])
    c.x_own = din("x_own", [OWN, D])
    c.w_in = din("w_in", [D, 2560])
    c.w_out = din("w_out", [D, D])
    c.w_router = din("w_router", [D, NE])
    ne_w = nexp if upto >= 4 else 1
    c.nexp = nexp
    c.p4mask = p4mask
    c.w_gu = din("w_gu", [ne_w, D, 2048])
    c.w_down = din("w_down", [ne_w, D, D])
    c.b_down = din("b_down", [NE, D])
    c.rows = din("rows", [8, D])
    c.colp = din("colp", [128, 64])
    c.w_rg = din("w_rg", [2, 8, 64, 64])
    c.lamv = din("lamv", [4, 64])
    c.b_router = din("b_router", [1, NE])
    c.bgu = din("bgu", [128, NE * 16])
    c.kbt = din("kbt", [128, 4 * 64])
    c.dq = din("dq", [128, 4 * 512])
    c.mfull = din("mfull", [128, 16 * 128])
    c.cst_bf = din("cst_bf", [128, 4 * 128], BF16)
    c.cst_f = din("cst_f", [128, NCF])
    c.out = nc.dram_tensor("out", [OWN, D], F32, kind="ExternalOutput")
    c.k_scr = nc.dram_tensor("k_scr", [4, 128, SEQ], BF16, kind=dk)
    c.v_scr = nc.dram_tensor("v_scr", [4, 128, NBLK, 128], BF16, kind=dk)
    c.q_scr = nc.dram_tensor("q_scr", [4, 128, 2, OWN], BF16, kind=dk)
    c.xg_scr = nc.dram_tensor("xg_scr", [NE * CAP, D], BF16, kind=dk)
    c.y_scr = nc.dram_tensor("y_scr", [NE * CAP, D], F32, kind=dk)
    c.x1_scr = nc.dram_tensor("x1_scr", [OWN, D], F32, kind=dk)
    c.dbg = nc.dram_tensor("dbg", [128, 4 * OWN * 2], BF16, kind=dk)
    c.dbg_gts = nc.dram_tensor("dbg_gts", [128, NOB * 4], F32, kind=dk)
    c.dbg_dst = nc.dram_tensor("dbg_dst", [128, NOB * 4], U32, kind=dk)

    with ExitStack() as es:
        SEM_STACK[0] = es
        c.cbf = _sb(nc, es, "cbf", [128, 4 * 128], BF16)
        c.cf = _sb(nc, es, "cf", [128, NCF], F32)
        c.colp_sb = _sb(nc, es, "colp_sb", [128, 64], F32)
        c.gts = _sb(nc, es, "gts", [128, NOB, 4], F32)
        c.dst = _sb(nc, es, "dst", [128, NOB, 4], U32)
        c.ident_bf = c.cbf[:, 0:128]
        c.ones_bf = c.cbf[:, 128:256]
        c.ltri_bf = c.cbf[:, 256:384]
        c.row0_bf = c.cbf[:, 384:512]
        c.ident_f = c.cf[:, 0:128]
        c.ones_f = c.cf[:, 128:256]
        c.ecap = c.cf[:, 256:256 + NE]
        c.sel = c.cf[:, 256 + NE:256 + NE + 4]
        c.epsc = c.cf[:, 292:293]
        c.onec = c.cf[:, 293:294]

        if only is not None:
            S0 = Sched(nc, es, "p0")
            S0.dma("sync", lambda e: e.dma_start(out=c.cbf[:], in_=c.cst_bf[:, :]), [], [T("a")])
            S0.dma("sync", lambda e: e.dma_start(out=c.cf[:], in_=c.cst_f[:, :]), [], [T("b")])
            with nc.Block() as blk:
                S0.emit(blk)
            {4: phase4, 5: phase5}[only](c)
            return nc
        with ExitStack() as es2:
            c.recT = _sb(nc, es2, "recT", [128, 4, OWN], BF16)
            phase1(c)
            c.attT = _sb(nc, es2, "attT", [128, 4, OWN], BF16)
            if upto >= 2:
                phase2(c)
            if upto >= 3:
                phase3(c)
            if debug:
                phase_dbg(c)
        if upto >= 4:
            phase4(c)
        if upto >= 5:
            phase5(c)
    return nc


def phase_dbg(c):
    nc = c.nc
    with ExitStack() as es:
        S = Sched(nc, es, "pd")
        td = T("dbg")
        S.dma("sync", lambda e: e.dma_start(out=c.dbg[:, 0:4 * OWN], in_=c.recT[:].rearrange("p a b -> p (a b)")), [], [td])
        S.dma("sync", lambda e: e.dma_start(out=c.dbg[:, 4 * OWN:8 * OWN], in_=c.attT[:].rearrange("p a b -> p (a b)")), [], [td])
        S.dma("sync", lambda e: e.dma_start(out=c.dbg_gts[:, :], in_=c.gts[:].rearrange("p a b -> p (a b)")), [], [td])
        S.dma("sync", lambda e: e.dma_start(out=c.dbg_dst[:, :], in_=c.dst[:].rearrange("p a b -> p (a b)")), [], [td])
        with nc.Block() as blk:
            S.emit(blk)


def phase1(c):
    nc = c.nc
    with ExitStack() as es:
        S = Sched(nc, es, "p1")
        sb = lambda n, s, d: _sb(nc, es, n, s, d)
        c.Tk_scr, c.Tv_scr, c.Tq_scr = T("k_scr"), T("v_scr"), T("q_scr")
        win = sb("win", [128, 8, 2560], BF16)
        Twin = [T(f"win{k}") for k in range(8)]
        wst = Ring([(sb(f"wst{i}", [128, 1280], F32), T(f"wst{i}")) for i in range(2)])
        xs = Ring([(sb(f"xs{i}", [128, D], F32), T(f"xs{i}")) for i in range(3)])
        stt = sb("stt", [128, 4, 12], F32); Tstt = T("stt")
        mv = sb("mv", [128, 4, 2], F32); Tmv = T("mv")
        rstd = sb("rstd", [128, 4], F32); Trstd = T("rstd")
        nmr = sb("nmr", [128, 4], F32); Tnmr = T("nmr")
        xn = sb("xn", [128, 4, D], BF16); Txn = [T(f"xn{b}") for b in range(4)]
        xT = sb("xT", [128, 8, 512], BF16); TxT = [T(f"xT{k}") for k in range(8)]
        kst = sb("kst", [128, 4, 512], BF16); Tkst = T("kst")
        vst = sb("vst", [128, 4, 512], BF16); Tvst = T("vst")
        qst = sb("qst", [128, 4, 2, 512], BF16); Tqst = T("qst")
        xlb = [(sb(f"xlb{i}", [128, 4, 515], F32), T(f"xlb{i}")) for i in range(2)]
        xc = sb("xc", [128, 4, 512], F32); Txc = T("xc")
        xcb = sb("xcb", [128, 4, 512], BF16); Txcb = T("xcb")
        rg = sb("rg", [128, 4, 512], F32); Trg = T("rg")
        ig = sb("ig", [128, 4, 512], F32); Tig = T("ig")
        aa = sb("aa", [128, 4, 512], F32); Taa = T("aa")
        sq = sb("sq", [128, 4, 512], F32); Tsq = T("sq")
        hb = [(sb(f"hb{i}", [128, 4, 512], F32), T(f"hb{i}")) for i in range(2)]
        hsel = sb("hsel", [128, 4, 128], F32); Thsel = T("hsel")
        ge = sb("ge", [128, 512], F32); Tge = T("ge")
        bdst = sb("bdst", [128, 2, 4, 128], F32); Tbdst = T("bdst")
        bd = sb("bd", [128, 2, 4, 128], BF16); Tbd = T("bd")
        lrp = sb("lrp", [128, 12], F32); Tlrp = T("lrp")
        Tcst = T("cst"); Tcolp = T("colp"); TrecT = T("recT")
        pst = [(_ps(nc, es, f"pst{i}", [128, 1024], BF16), T(f"pst{i}")) for i in range(2)]
        psr = Ring([(_ps(nc, es, f"psm{i}", [128, 512], F32), T(f"psm{i}")) for i in range(6)])
        colp = c.colp_sb

        S.dma("sync", lambda e: e.dma_start(out=c.cbf[:], in_=c.cst_bf[:, :]), [], [Tcst])
        S.dma("sync", lambda e: e.dma_start(out=c.cf[:], in_=c.cst_f[:, :]), [], [Tcst])
        S.dma("sync", lambda e: e.dma_start(out=colp[:], in_=c.colp[:, :]), [], [Tcolp])
        Tparts = [T(f"bdp{i}") for i in range(16)]
        S.op("pool", lambda e: e.memset(bdst[:].rearrange("p a b c -> p (a b c)"), 0.0), [], [Tbdst] + Tparts)
        for m in range(2):
            for ch in range(4):
                for u in range(2):
                    S.dma("sync", lambda e, m=m, ch=ch, u=u: e.dma_start(
                        out=bdst[u * 64:(u + 1) * 64, m, ch, u * 64:(u + 1) * 64],
                        in_=c.w_rg[m, 2 * ch + u, :, :]), [], [Tparts[m * 8 + ch * 2 + u]])
        S.op("act", lambda e: e.copy(out=bd[:].rearrange("p a b c -> p (a b c)"),
                                     in_=bdst[:].rearrange("p a b c -> p (a b c)")), [Tbdst] + Tparts, [Tbd])
        S.op("act", lambda e: e.activation(out=lrp[:, 8:12], in_=colp[:, 44:48], func=AF.Exp, scale=-1.0),
             [Tcolp], [Tlrp])
        S.op("act", lambda e: e.activation(out=lrp[:, 8:12], in_=lrp[:, 8:12], func=AF.Ln, bias=1.0),
             [Tlrp], [Tlrp])
        S.op("dve", lambda e: e.tensor_scalar(out=lrp[:, 0:4], in0=lrp[:, 8:12], scalar1=-8.0, scalar2=None,
                                              op0=ALU.mult), [Tlrp], [Tlrp])
        S.op("dve", lambda e: e.tensor_scalar(out=lrp[:, 4:8], in0=lrp[:, 8:12], scalar1=-16.0, scalar2=None,
                                              op0=ALU.mult), [Tlrp], [Tlrp])
        S.op("pool", lambda e: e.memset(qst[:].rearrange("p a b c -> p (a b c)"), 0.0), [], [Tqst])
        S.op("pool", lambda e: e.memset(xlb[0][0][:, :, 0:3], 0.0), [], [xlb[0][1]])
        cast_engs = ("act", "pool")
        for kc in range(8):
            for hf in range(2):
                st, Tst = wst.next()
                S.dma("sync", lambda e, st=st, kc=kc, hf=hf: e.dma_start(
                    out=st[:], in_=c.w_in[kc * 128:(kc + 1) * 128, hf * 1280:(hf + 1) * 1280]), [], [Tst])
                eng = cast_engs[(kc * 2 + hf) % 2]
                if eng == "act":
                    S.op("act", lambda e, st=st, kc=kc, hf=hf: e.copy(out=win[:, kc, hf * 1280:(hf + 1) * 1280], in_=st[:]),
                         [Tst], [Twin[kc]])
                else:
                    S.op("pool", lambda e, st=st, kc=kc, hf=hf: e.tensor_copy(out=win[:, kc, hf * 1280:(hf + 1) * 1280], in_=st[:]),
                         [Tst], [Twin[kc]])

        def mm_group(ps, Tps, lhs_fn, rhs_fn, reads_fn):
            for kc in range(8):
                S.op("pe", lambda e, kc=kc: e.matmul(ps, lhs_fn(kc), rhs_fn(kc), start=(kc == 0), stop=(kc == 7)),
                     reads_fn(kc), [Tps])

        def do_tile(it):
            own = it >= 16
            src = c.x_own if own else c.x_full
            t0 = (it - 16) * 512 if own else it * 512
            xsl = []
            for b in range(4):
                xt, Txt = xs.next()
                xsl.append((xt, Txt))
                S.dma("sync", lambda e, xt=xt, b=b: e.dma_start(out=xt[:], in_=src[t0 + b * 128:t0 + (b + 1) * 128, :]),
                      [], [Txt])
                for hh in range(2):
                    S.op("dve", lambda e, xt=xt, b=b, hh=hh: e.bn_stats(out=stt[:, b, hh * 6:(hh + 1) * 6],
                                                                     in_=xt[:, hh * 512:(hh + 1) * 512]), [Txt], [Tstt])
                S.op("dve", lambda e, b=b: e.bn_aggr(out=mv[:, b, :], in_=stt[:, b, :]), [Tstt], [Tmv])
                if b == 2 or b == 3:
                    pass
                S.op("act", lambda e, b=b: e.activation(out=rstd[:, b:b + 1], in_=mv[:, b, 1:2], func=AF.Sqrt,
                                                        bias=c.epsc[:, 0:1], scale=1.0), [Tmv], [Trstd])
                S.op("dve", lambda e, b=b: e.reciprocal(out=rstd[:, b:b + 1], in_=rstd[:, b:b + 1]), [Trstd], [Trstd])
                S.op("dve", lambda e, b=b: e.scalar_tensor_tensor(out=nmr[:, b:b + 1], in0=mv[:, b, 0:1], scalar=-1.0,
                                                                  in1=rstd[:, b:b + 1], op0=ALU.mult, op1=ALU.mult),
                     [Tmv, Trstd], [Tnmr])
                S.op("act", lambda e, xt=xt, b=b: e.activation(out=xn[:, b, :], in_=xt[:], func=AF.Identity,
                                                                scale=rstd[:, b:b + 1], bias=nmr[:, b:b + 1]),
                     [Txt, Trstd, Tnmr], [Txn[b]])
            for kc in range(8):
                pt, Tpt = pst[(kc // 2) % 2]
                off = (kc % 2) * 512
                for b in range(4):
                    S.op("pe", lambda e, pt=pt, off=off, b=b, kc=kc: e.transpose(
                        out=pt[:, off + b * 128:off + (b + 1) * 128], in_=xn[:, b, kc * 128:(kc + 1) * 128],
                        identity=c.ident_bf), [Txn[b], Tcst], [Tpt])
                S.op("act", lambda e, pt=pt, off=off, kc=kc: e.activation(
                    out=xT[:, kc, :], in_=pt[:, off:off + 512], func=AF.Identity,
                    scale=colp[:, kc:kc + 1], bias=colp[:, 8 + kc:9 + kc]), [Tpt, Tcolp], [TxT[kc]])
            if not own:
                for h in range(4):
                    ps, Tps = psr.next()
                    mm_group(ps[:], Tps, lambda kc, h=h: win[:, kc, 512 + h * 128:512 + (h + 1) * 128],
                             lambda kc: xT[:, kc, :], lambda kc: [Twin[kc], TxT[kc]])
                    S.op("dve" if h % 2 else "act",
                         (lambda e, ps=ps, h=h: e.tensor_copy(out=kst[:, h, :], in_=ps[:])) if h % 2 else
                         (lambda e, ps=ps, h=h: e.copy(out=kst[:, h, :], in_=ps[:])), [Tps], [Tkst])
                for h in range(4):
                    S.dma("sync", lambda e, h=h: e.dma_start(out=c.k_scr[h, :, t0:t0 + 512], in_=kst[:, h, :]),
                          [Tkst], [c.Tk_scr], sem_tile=c.Tk_scr)
                for b in range(4):
                    ps, Tps = psr.next()
                    mm_group(ps[:], Tps, lambda kc, b=b: xT[:, kc, b * 128:(b + 1) * 128],
                             lambda kc: win[:, kc, 1024:1536], lambda kc: [Twin[kc], TxT[kc]])
                    S.op("dve" if b % 2 else "act",
                         (lambda e, ps=ps, b=b: e.tensor_copy(out=vst[:, b, :], in_=ps[:])) if b % 2 else
                         (lambda e, ps=ps, b=b: e.copy(out=vst[:, b, :], in_=ps[:])), [Tps], [Tvst])
                for h in range(4):
                    S.dma("sync", lambda e, h=h: e.dma_start(out=c.v_scr[h, :, it * 4:(it + 1) * 4, :],
                                                              in_=vst[:, :, h * 128:(h + 1) * 128]),
                          [Tvst], [c.Tv_scr], sem_tile=c.Tv_scr)
                xl, Txl = xlb[it % 2]
                xl2, Txl2 = xlb[(it + 1) % 2]
                hcur, Thcur = hb[it % 2]
                hprev, Thprev = hb[(it + 1) % 2]
                for ch in range(4):
                    ps, Tps = psr.next()
                    mm_group(ps[:], Tps, lambda kc, ch=ch: win[:, kc, 1536 + ch * 128:1536 + (ch + 1) * 128],
                             lambda kc: xT[:, kc, :], lambda kc: [Twin[kc], TxT[kc]])
                    S.op("dve", lambda e, ps=ps, ch=ch, xl=xl: e.tensor_copy(out=xl[:, ch, 3:515], in_=ps[:]), [Tps], [Txl])
                S.op("pool", lambda e, xl=xl, xl2=xl2: e.tensor_copy(out=xl2[:, :, 0:3], in_=xl[:, :, 512:515]),
                     [Txl], [Txl2])
                for ch in range(4):
                    S.op("dve", lambda e, ch=ch, xl=xl: e.tensor_scalar(
                        out=xc[:, ch, :], in0=xl[:, ch, 3:515], scalar1=colp[:, 16 + ch * 4 + 3:16 + ch * 4 + 4],
                        scalar2=colp[:, 32 + ch:33 + ch], op0=ALU.mult, op1=ALU.add), [Txl, Tcolp], [Txc])
                    for w in (2, 1, 0):
                        S.op("dve", lambda e, ch=ch, w=w, xl=xl: e.scalar_tensor_tensor(
                            out=xc[:, ch, :], in0=xl[:, ch, w:w + 512], scalar=colp[:, 16 + ch * 4 + w:16 + ch * 4 + w + 1],
                            in1=xc[:, ch, :], op0=ALU.mult, op1=ALU.add), [Txl, Tcolp, Txc], [Txc])
                S.op("pool", lambda e: e.tensor_copy(out=xcb[:].rearrange("p a b -> p (a b)"),
                                                     in_=xc[:].rearrange("p a b -> p (a b)")), [Txc], [Txcb])
                for ch in range(4):
                    for m, (dstt, Tdst, bo) in enumerate(((rg, Trg, 36), (ig, Tig, 40))):
                        ps, Tps = psr.next()
                        S.op("pe", lambda e, ps=ps, m=m, ch=ch: e.matmul(ps[:], bd[:, m, ch, :], xcb[:, ch, :],
                                                                         start=True, stop=True), [Tbd, Txcb], [Tps])
                        S.op("act", lambda e, ps=ps, ch=ch, dstt=dstt, bo=bo: e.activation(
                            out=dstt[:, ch, :], in_=ps[:], func=AF.Sigmoid, bias=colp[:, bo + ch:bo + ch + 1], scale=1.0),
                            [Tps, Tcolp], [Tdst])
                for ch in range(4):
                    S.op("act", lambda e, ch=ch: e.activation(out=aa[:, ch, :], in_=rg[:, ch, :], func=AF.Exp,
                                                              scale=lrp[:, ch:ch + 1]), [Trg, Tlrp], [Taa])
                    S.op("act", lambda e, ch=ch: e.activation(out=sq[:, ch, :], in_=rg[:, ch, :], func=AF.Exp,
                                                              scale=lrp[:, 4 + ch:5 + ch]), [Trg, Tlrp], [Tsq])
                S.op("act", lambda e: e.activation(out=sq[:].rearrange("p a b -> p (a b)"),
                                                   in_=sq[:].rearrange("p a b -> p (a b)"), func=AF.Sqrt,
                                                   bias=c.onec[:, 0:1], scale=-1.0), [Tsq], [Tsq])
                S.op("pool", lambda e: e.tensor_tensor(out=ig[:].rearrange("p a b -> p (a b)"),
                                                       in0=ig[:].rearrange("p a b -> p (a b)"),
                                                       in1=xc[:].rearrange("p a b -> p (a b)"), op=ALU.mult),
                     [Tig, Txc], [Tig])
                S.op("pool", lambda e: e.tensor_tensor(out=ig[:].rearrange("p a b -> p (a b)"),
                                                       in0=ig[:].rearrange("p a b -> p (a b)"),
                                                       in1=sq[:].rearrange("p a b -> p (a b)"), op=ALU.mult),
                     [Tig, Tsq], [Tig])
                for ch in range(4):
                    init = 0.0 if it == 0 else hprev[:, ch, 511:512]
                    S.op("dve", lambda e, ch=ch, init=init, hcur=hcur: e.tensor_tensor_scan(
                        out=hcur[:, ch, :], data0=aa[:, ch, :], data1=ig[:, ch, :], initial=init,
                        op0=ALU.mult, op1=ALU.add), [Taa, Tig] + ([] if it == 0 else [Thprev]), [Thcur])
                S.op("dve", lambda e, hcur=hcur: e.tensor_scalar(out=hsel[:], in0=hcur[:, :, 0:128], scalar1=c.sel[:, 0:1],
                                                                 scalar2=None, op0=ALU.mult), [Thcur, Tcst], [Thsel])
                for t in (1, 2):
                    S.op("dve", lambda e, t=t, hcur=hcur: e.scalar_tensor_tensor(
                        out=hsel[:], in0=hcur[:, :, t * 128:(t + 1) * 128], scalar=c.sel[:, t:t + 1], in1=hsel[:],
                        op0=ALU.mult, op1=ALU.add), [Thcur, Tcst, Thsel], [Thsel])
                S.op("dve", lambda e, hcur=hcur, it=it: e.scalar_tensor_tensor(
                    out=c.recT[:, :, it * 128:(it + 1) * 128], in0=hcur[:, :, 384:512], scalar=c.sel[:, 3:4], in1=hsel[:],
                    op0=ALU.mult, op1=ALU.add), [Thcur, Tcst, Thsel], [TrecT])
            else:
                ot = it - 16
                for h in range(4):
                    ps, Tps = psr.next()
                    mm_group(ps[:], Tps, lambda kc, h=h: win[:, kc, h * 128:(h + 1) * 128],
                             lambda kc: xT[:, kc, :], lambda kc: [Twin[kc], TxT[kc]])
                    S.op("act", lambda e, ps=ps, h=h: e.mul(out=qst[0:64, h, 0, :], in_=ps[0:64, :], mul=0.125), [Tps], [Tqst])
                    S.op("dve", lambda e, ps=ps, h=h: e.tensor_scalar(out=qst[64:128, h, 1, :], in0=ps[64:128, :], scalar1=0.125,
                                                                      scalar2=None, op0=ALU.mult), [Tps], [Tqst])
                for h in range(4):
                    S.dma("sync", lambda e, h=h, ot=ot: e.dma_start(out=c.q_scr[h, :, :, ot * 512:(ot + 1) * 512],
                                                                     in_=qst[:, h, :, :]), [Tqst], [c.Tq_scr], sem_tile=c.Tq_scr)
                for ch in range(4):
                    ps, Tps = psr.next()
                    mm_group(ps[:], Tps, lambda kc, ch=ch: win[:, kc, 2048 + ch * 128:2048 + (ch + 1) * 128],
                             lambda kc: xT[:, kc, :], lambda kc: [Twin[kc], TxT[kc]])
                    S.op("act", lambda e, ps=ps: e.activation(out=ge[:], in_=ps[:], func=AF.Gelu), [Tps], [Tge])
                    S.op("dve", lambda e, ch=ch, ot=ot: e.tensor_tensor(
                        out=c.recT[:, ch, ot * 512:(ot + 1) * 512], in0=c.recT[:, ch, ot * 512:(ot + 1) * 512], in1=ge[:],
                        op=ALU.mult), [Tge, TrecT], [TrecT])

        for it in range(20):
            do_tile(it)
        with nc.Block() as blk:
            S.emit(blk)


def phase2(c):
    nc = c.nc
    with ExitStack() as es:
        S = Sched(nc, es, "p2")
        sb = lambda n, s, d: _sb(nc, es, n, s, d)
        kbt = sb("kbt_s", [128, 4, 64], F32); dq = sb("dq_s", [128, 4, 512], F32); mf = sb("mf_s", [128, 4, 4, 128], F32)
        lamt = sb("lamt", [128, 4, 64], F32); lw = sb("lw", [128, 8], F32); junk = sb("junk2", [128, 64], F32)
        Tc = T("c2"); Tlam = T("lam"); Tlw = T("lw"); Tjunk = T("junk"); TattT = T("attT"); Tcst = T("cst")
        Kr = Ring([(sb(f"Kh{i}", [128, SEQ], BF16), T(f"Kh{i}")) for i in range(2)])
        Vr = Ring([(sb(f"Vh{i}", [128, NBLK, 128], BF16), T(f"Vh{i}")) for i in range(2)])
        Qr = Ring([(sb(f"Qh{i}", [128, 2, OWN], BF16), T(f"Qh{i}")) for i in range(2)])
        sbr = Ring([(sb(f"sbs{i}", [128, 512], F32), T(f"sbs{i}")) for i in range(4)])
        pbr = Ring([(sb(f"pb{i}", [128, 512], BF16), T(f"pb{i}")) for i in range(6)])
        rz = [(sb(f"rz{i}", [128, 512], F32), T(f"rz{i}")) for i in range(2)]
        oo = [(sb(f"oo{i}", [128, 512], F32), T(f"oo{i}")) for i in range(2)]
        osq = sb("osq", [128, 512], F32); Tosq = T("osq")
        rst = sb("rst", [128, 512], F32); Trst = T("rst")
        Sr = Ring([(_ps(nc, es, f"S{i}", [128, 512], F32), T(f"S{i}")) for i in range(4)])
        Ab = [(_ps(nc, es, f"A{i}", [128, 512], F32), T(f"A{i}")) for i in range(2)]
        Zb = [(_ps(nc, es, f"Z{i}", [128, 512], F32), T(f"Z{i}")) for i in range(2)]
        colp = c.colp_sb

        S.dma("sync", lambda e: e.dma_start(out=kbt[:].rearrange("p a b -> p (a b)"), in_=c.kbt[:, :]), [], [Tc])
        S.dma("sync", lambda e: e.dma_start(out=dq[:].rearrange("p a b -> p (a b)"), in_=c.dq[:, :]), [], [Tc])
        S.dma("sync", lambda e: e.dma_start(out=mf[:].rearrange("p a b c -> p (a b c)"), in_=c.mfull[:, :]), [], [Tc])
        S.dma("sync", lambda e: e.dma_start(out=lamt[:].rearrange("p a b -> p (a b)"), in_=_bcast_rows(c.lamv, 128, 256)),
              [], [Tlam])
        for i in range(2):
            S.op("dve", lambda e, i=i: e.scalar_tensor_tensor(out=junk[:], in0=lamt[:, 2 * i, :], scalar=1.0,
                                                             in1=lamt[:, 2 * i + 1, :], op0=ALU.mult, op1=ALU.mult,
                                                             accum_out=lw[:, i:i + 1]), [Tlam], [Tjunk, Tlw])
        S.op("act", lambda e: e.activation(out=lw[:, 2:4], in_=lw[:, 0:2], func=AF.Exp), [Tlw], [Tlw])
        S.op("dve", lambda e: e.tensor_tensor(out=lw[:, 4:5], in0=lw[:, 3:4], in1=lw[:, 2:3], op=ALU.subtract), [Tlw], [Tlw])
        S.op("dve", lambda e: e.tensor_scalar(out=lw[:, 5:6], in0=lw[:, 4:5], scalar1=-LAM_INIT, scalar2=None, op0=ALU.add),
             [Tlw], [Tlw])
        S.op("dve", lambda e: e.tensor_scalar(out=lw[:, 6:7], in0=colp[:, 48:49], scalar1=(1.0 - LAM_INIT), scalar2=None,
                                              op0=ALU.mult), [], [Tlw])

        def head(h):
            Kh, TK = Kr.next(); Vh, TV = Vr.next(); Qh, TQ = Qr.next()
            S.dma("sync", lambda e: e.dma_start(out=Kh[:], in_=c.k_scr[h, :, :]), [], [TK])
            S.dma("sync", lambda e: e.dma_start(out=Vh[:], in_=c.v_scr[h, :, :, :]), [], [TV])
            S.dma("sync", lambda e: e.dma_start(out=Qh[:], in_=c.q_scr[h, :, :, :]), [], [TQ])

            def qtile(g):
                nj = 16 * g + 16
                pend = {}
                j0 = max(0, 16 * g - 1 - int(math.ceil(1.0 / SLOPES[h])))

                def scores(j):
                    c0 = 0 if j < 16 * g else 128 * ((j - 16 * g) // 4)
                    n = j - 16 * g + 48
                    ps_l = []
                    for m in range(2):
                        ps, Tps = Sr.next()
                        S.op("pe", lambda e, ps=ps, m=m: e.matmul(ps[:, c0:512], Kh[:, j * 128:(j + 1) * 128],
                                                                   Qh[:, m, g * 512 + c0:(g + 1) * 512], start=True, stop=True),
                             [TK, TQ], [Tps])
                        sbt, Tsb = sbr.next()
                        if j >= 16 * g:
                            jj = (j - 16 * g) % 4
                            S.op("dve", lambda e, ps=ps, sbt=sbt: e.tensor_tensor(out=sbt[:, c0:c0 + 128], in0=ps[:, c0:c0 + 128],
                                                                                  in1=mf[:, h, jj, :], op=ALU.add), [Tps, Tc], [Tsb])
                            if c0 + 128 < 512:
                                S.op("dve", lambda e, ps=ps, sbt=sbt: e.scalar_tensor_tensor(
                                    out=sbt[:, c0 + 128:512], in0=ps[:, c0 + 128:512], scalar=kbt[:, h, n:n + 1],
                                    in1=dq[:, h, c0 + 128:512], op0=ALU.add, op1=ALU.add), [Tps, Tc], [Tsb])
                        else:
                            S.op("dve", lambda e, ps=ps, sbt=sbt: e.scalar_tensor_tensor(
                                out=sbt[:, :], in0=ps[:, :], scalar=kbt[:, h, n:n + 1], in1=dq[:, h, :],
                                op0=ALU.add, op1=ALU.add), [Tps, Tc], [Tsb])
                        pb, Tpb = pbr.next()
                        S.op("act", lambda e, sbt=sbt, pb=pb: e.activation(out=pb[:, c0:512], in_=sbt[:, c0:512], func=AF.Exp),
                             [Tsb], [Tpb])
                        ps_l.append((pb, Tpb))
                    pend[j] = (c0, ps_l)

                def av(j):
                    c0, ps_l = pend.pop(j)
                    for m in range(2):
                        pb, Tpb = ps_l[m]
                        S.op("pe", lambda e, pb=pb, m=m: e.matmul(Ab[m][0][:, c0:512], Vh[:, j, :], pb[:, c0:512],
                                                                   start=(j == j0), stop=(j == nj - 1)), [TV, Tpb], [Ab[m][1]])
                        S.op("pe", lambda e, pb=pb, m=m: e.matmul(Zb[m][0][:, c0:512], c.ones_bf, pb[:, c0:512],
                                                                   start=(j == j0), stop=(j == nj - 1)), [Tcst, Tpb], [Zb[m][1]])

                j0 = max(0, 16 * g - 1 - int(math.ceil(1.0 / SLOPES[h])))
                for st in range(j0, nj + 1):
                    if st < nj:
                        scores(st)
                    if st >= j0 + 1:
                        av(st - 1)
                for m in range(2):
                    S.op("dve", lambda e, m=m: e.reciprocal(out=rz[m][0][:], in_=Zb[m][0][:]), [Zb[m][1]], [rz[m][1]])
                    S.op("dve", lambda e, m=m: e.tensor_tensor(out=oo[m][0][:], in0=Ab[m][0][:], in1=rz[m][0][:], op=ALU.mult),
                         [Ab[m][1], rz[m][1]], [oo[m][1]])
                S.op("dve", lambda e: e.scalar_tensor_tensor(out=oo[0][0][:], in0=oo[1][0][:], scalar=lw[:, 5:6], in1=oo[0][0][:],
                                                             op0=ALU.mult, op1=ALU.add), [oo[0][1], oo[1][1], Tlw], [oo[0][1]])
                S.op("pool", lambda e: e.tensor_tensor(out=osq[:], in0=oo[0][0][:], in1=oo[0][0][:], op=ALU.mult), [oo[0][1]], [Tosq])
                ps, Tps = Sr.next()
                S.op("pe", lambda e, ps=ps: e.matmul(ps[:], c.ones_f, osq[:], start=True, stop=True), [Tosq, Tcst], [Tps])
                S.op("act", lambda e, ps=ps: e.activation(out=rst[:], in_=ps[:], func=AF.Sqrt, bias=c.epsc[:, 0:1],
                                                          scale=1.0 / 128.0), [Tps, Tcst], [Trst])
                S.op("dve", lambda e: e.reciprocal(out=rst[:], in_=rst[:]), [Trst], [Trst])
                S.op("dve", lambda e: e.scalar_tensor_tensor(out=c.attT[:, h, g * 512:(g + 1) * 512], in0=oo[0][0][:],
                                                             scalar=lw[:, 6:7], in1=rst[:], op0=ALU.mult, op1=ALU.mult),
                     [oo[0][1], Trst, Tlw], [TattT])

            for g in range(4):
                qtile(g)

        for h in range(4):
            head(h)
        with nc.Block() as blk:
            S.emit(blk)


class LNbufs:
    def __init__(self, nc, es, tag):
        self.stt = _sb(nc, es, f"ln_stt_{tag}", [128, 12], F32); self.Tstt = T("stt")
        self.mv = _sb(nc, es, f"ln_mv_{tag}", [128, 2], F32); self.Tmv = T("mv")
        self.rs = _sb(nc, es, f"ln_rs_{tag}", [128, 1], F32); self.Trs = T("rs")
        self.nm = _sb(nc, es, f"ln_nm_{tag}", [128, 1], F32); self.Tnm = T("nm")
        self.tmp = _sb(nc, es, f"ln_tmp_{tag}", [128, D], F32); self.Ttmp = T("tmp")


def ln_tm(c, S, B, src, Tsrc, dst, Tdst, grow, brow, Trows):
    for hh in range(2):
        S.op("dve", lambda e, hh=hh: e.bn_stats(out=B.stt[:, hh * 6:(hh + 1) * 6], in_=src[:, hh * 512:(hh + 1) * 512]),
             [Tsrc], [B.Tstt])
    S.op("dve", lambda e: e.bn_aggr(out=B.mv[:], in_=B.stt[:]), [B.Tstt], [B.Tmv])
    S.op("act", lambda e: e.activation(out=B.rs[:], in_=B.mv[:, 1:2], func=AF.Sqrt, bias=c.epsc[:, 0:1], scale=1.0),
         [B.Tmv], [B.Trs])
    S.op("dve", lambda e: e.reciprocal(out=B.rs[:], in_=B.rs[:]), [B.Trs], [B.Trs])
    S.op("dve", lambda e: e.scalar_tensor_tensor(out=B.nm[:], in0=B.mv[:, 0:1], scalar=-1.0, in1=B.rs[:],
                                                 op0=ALU.mult, op1=ALU.mult), [B.Tmv, B.Trs], [B.Tnm])
    S.op("act", lambda e: e.activation(out=B.tmp[:], in_=src[:], func=AF.Identity, scale=B.rs[:, 0:1], bias=B.nm[:, 0:1]),
         [Tsrc, B.Trs, B.Tnm], [B.Ttmp])
    S.op("dve", lambda e: e.tensor_tensor(out=B.tmp[:], in0=B.tmp[:], in1=grow, op=ALU.mult), [B.Ttmp, Trows], [B.Ttmp])
    S.op("pool", lambda e: e.tensor_tensor(out=dst[:], in0=B.tmp[:], in1=brow, op=ALU.add), [B.Ttmp, Trows], [Tdst])


def phase3(c):
    nc = c.nc
    with ExitStack() as es:
        S = Sched(nc, es, "p3")
        rc = {}
        sb = lambda n, s, d: _sb(nc, es, n, s, d)
        wo = sb("wo", [128, 8, D], BF16); Two = [T(f"wo{k}") for k in range(8)]
        wost = Ring([(sb(f"wost{i}", [128, D], F32), T(f"wost{i}")) for i in range(2)])
        wr = sb("wr", [128, 8, NE], F32); Twr = T("wr")
        rowsb = sb("rowsb", [128, 4, D], F32); Trows = T("rows")
        brt = sb("brt", [128, NE], F32); Tbrt = T("brt")
        msk = sb("msk", [128, NOB, NE], BF16); Tmsk = [T(f"msk{i}") for i in range(NOB)]
        xs = Ring([(sb(f"xs3_{i}", [128, D], F32), T(f"xs3_{i}")) for i in range(2)])
        x0_l = [(sb(f"x0_{i}", [128, D], F32), T(f"x0_{i}")) for i in range(2)]
        yy_l = [(sb(f"yy_{i}", [128, D], F32), T(f"yy_{i}")) for i in range(2)]
        x1r = Ring([(sb(f"x1_{i}", [128, D], F32), T(f"x1_{i}")) for i in range(2)])
        x1br = Ring([(sb(f"x1b_{i}", [128, D], BF16), T(f"x1b_{i}")) for i in range(2)])
        x1T_l = [(sb(f"x1T_{i}", [128, 8, 128], F32), T(f"x1T_{i}")) for i in range(2)]
        lg_l = [(sb(f"lg_{i}", [128, NE], F32), T(f"lg_{i}")) for i in range(2)]
        t8_l = [(sb(f"t8_{i}", [128, 8], F32), T(f"t8_{i}")) for i in range(2)]
        sm_l = [(sb(f"sm3_{i}", [128, 16], F32), T(f"sm3_{i}")) for i in range(2)]
        destf_l = [(sb(f"destf_{i}", [128, NE], F32), T(f"destf_{i}")) for i in range(2)]
        junk_l = [(sb(f"junk3_{i}", [128, NE], F32), T(f"junk3_{i}")) for i in range(2)]
        B0_l = [LNbufs(nc, es, f"a{i}") for i in range(2)]; B1_l = [LNbufs(nc, es, f"b{i}") for i in range(2)]
        mixr = Ring([(_ps(nc, es, f"mix{i}", [128, 512], F32), T(f"mix{i}")) for i in range(4)])
        tpf = [(_ps(nc, es, f"tpf{i}", [128, 512], F32), T(f"tpf{i}")) for i in range(2)]
        lgp = _ps(nc, es, "lgp", [128, 512], F32); Tlgp = T("lgp")
        posp = _ps(nc, es, "posp", [128, 512], F32); Tposp = T("posp")
        Tcst = T("cst"); Tatt = T("att"); Trec = T("rec"); Tgts = T("gts"); Tdst = T("dst")
        Txg = T("xg_scr"); Tx1s = T("x1_scr")

        for i in range(4):
            S.dma("sync", lambda e, i=i: e.dma_start(out=rowsb[:, i, :], in_=_bcast_rows(c.rows, 128, D, offset=i * D)),
                  [], [Trows])
        S.dma("sync", lambda e: e.dma_start(out=brt[:], in_=_bcast_rows(c.b_router, 128, NE)), [], [Tbrt])
        for kc in range(8):
            S.dma("sync", lambda e, kc=kc: e.dma_start(out=wr[:, kc, :], in_=c.w_router[kc * 128:(kc + 1) * 128, :]), [], [Twr])
            st, Tst = wost.next()
            S.dma("sync", lambda e, st=st, kc=kc: e.dma_start(out=st[:], in_=c.w_out[kc * 128:(kc + 1) * 128, :]), [], [Tst])
            if kc % 2:
                S.op("act", lambda e, st=st, kc=kc: e.copy(out=wo[:, kc, :], in_=st[:]), [Tst], [Two[kc]])
            else:
                S.op("pool", lambda e, st=st, kc=kc: e.tensor_copy(out=wo[:, kc, :], in_=st[:]), [Tst], [Two[kc]])

        def block(ob):
            cs = slice(ob * 128, (ob + 1) * 128)
            x0, Tx0 = x0_l[ob % 2]; yy, Tyy = yy_l[ob % 2]; x1T, Tx1T = x1T_l[ob % 2]; lg, Tlg = lg_l[ob % 2]
            t8, Tt8 = t8_l[ob % 2]; sm, Tsm = sm_l[ob % 2]; destf, Tdestf = destf_l[ob % 2]; junk, Tjunk = junk_l[ob % 2]
            B0 = B0_l[ob % 2]; B1 = B1_l[ob % 2]
            xt, Txt = xs.next()
            S.dma("sync", lambda e: e.dma_start(out=xt[:], in_=c.x_own[cs, :]), [], [Txt])
            ln_tm(c, S, B0, xt, Txt, x0, Tx0, rowsb[:, 0, :], rowsb[:, 1, :], Trows)
            for hf in range(2):
                ps, Tps = mixr.next()
                for kc in range(8):
                    lhs = c.attT[:, kc, cs] if kc < 4 else c.recT[:, kc - 4, cs]
                    S.op("pe", lambda e, ps=ps, lhs=lhs, kc=kc, hf=hf: e.matmul(ps[:], lhs, wo[:, kc, hf * 512:(hf + 1) * 512],
                                                                              start=(kc == 0), stop=(kc == 7)),
                         [Two[kc], Tatt, Trec], [Tps])
                S.op("dve", lambda e, ps=ps, hf=hf: e.scalar_tensor_tensor(
                    out=yy[:, hf * 512:(hf + 1) * 512], in0=x0[:, hf * 512:(hf + 1) * 512], scalar=float(ALPHA), in1=ps[:],
                    op0=ALU.mult, op1=ALU.add), [Tx0, Tps], [Tyy])
            x1, Tx1 = x1r.next()
            ln_tm(c, S, B1, yy, Tyy, x1, Tx1, rowsb[:, 2, :], rowsb[:, 3, :], Trows)
            S.dma("sync", lambda e: e.dma_start(out=c.x1_scr[cs, :], in_=x1[:]), [Tx1], [Tx1s], sem_tile=Tx1s)
            x1b, Tx1b = x1br.next()
            S.op("act", lambda e: e.copy(out=x1b[:], in_=x1[:]), [Tx1], [Tx1b])
            for kc in range(8):
                pt, Tpt = tpf[kc // 4]
                S.op("pe", lambda e, pt=pt, kc=kc: e.transpose(out=pt[:, (kc % 4) * 128:(kc % 4 + 1) * 128],
                                                               in_=x1[:, kc * 128:(kc + 1) * 128], identity=c.ident_f),
                     [Tx1, Tcst], [Tpt])
            S.op("act", lambda e: e.copy(out=x1T[:, 0:4, :].rearrange("p a b -> p (a b)"), in_=tpf[0][0][:]), [tpf[0][1]], [Tx1T])
            S.op("dve", lambda e: e.tensor_copy(out=x1T[:, 4:8, :].rearrange("p a b -> p (a b)"), in_=tpf[1][0][:]),
                 [tpf[1][1]], [Tx1T])
            for kc in range(8):
                S.op("pe", lambda e, kc=kc: e.matmul(lgp[:, 0:NE], x1T[:, kc, :], wr[:, kc, :], start=(kc == 0), stop=(kc == 7)),
                     [Tx1T, Twr], [Tlgp])
            S.op("dve", lambda e: e.tensor_tensor(out=lg[:], in0=lgp[:, 0:NE], in1=brt[:], op=ALU.add), [Tlgp, Tbrt], [Tlg])
            S.op("dve", lambda e: e.max(out=t8[:], in_=lg[:]), [Tlg], [Tt8])
            S.op("dve", lambda e: e.tensor_scalar(out=sm[:, 0:1], in0=t8[:, 0:1], scalar1=-1.0, scalar2=None, op0=ALU.mult),
                 [Tt8], [Tsm])
            S.op("act", lambda e: e.activation(out=sm[:, 4:8], in_=t8[:, 0:4], func=AF.Exp, bias=sm[:, 0:1], scale=1.0,
                                               accum_out=sm[:, 1:2]), [Tt8, Tsm], [Tsm])
            S.op("dve", lambda e: e.reciprocal(out=sm[:, 1:2], in_=sm[:, 1:2]), [Tsm], [Tsm])
            S.op("dve", lambda e: e.tensor_scalar(out=c.gts[:, ob, :], in0=sm[:, 4:8], scalar1=sm[:, 1:2], scalar2=None,
                                                  op0=ALU.mult), [Tsm], [Tgts])
            S.op("dve", lambda e: e.tensor_scalar(out=msk[:, ob, :], in0=lg[:], scalar1=t8[:, 3:4], scalar2=None, op0=ALU.is_ge),
                 [Tlg, Tt8], [Tmsk[ob]])
            for o2 in range(ob + 1):
                lhs = c.ltri_bf if o2 == ob else c.ones_bf
                S.op("pe", lambda e, lhs=lhs, o2=o2: e.matmul(posp[:, 0:NE], lhs, msk[:, o2, :], start=(o2 == 0), stop=(o2 == ob)),
                     [Tmsk[o2], Tcst], [Tposp])
            S.op("dve", lambda e: e.tensor_tensor(out=destf[:], in0=posp[:, 0:NE], in1=c.ecap, op=ALU.add), [Tposp, Tcst], [Tdestf])
            for k in range(4):
                S.op("dve", lambda e, k=k: e.scalar_tensor_tensor(out=junk[:], in0=lg[:], scalar=t8[:, k:k + 1], in1=destf[:],
                                                                 op0=ALU.is_equal, op1=ALU.mult, accum_out=sm[:, 8 + k:9 + k]),
                     [Tlg, Tt8, Tdestf], [Tjunk, Tsm])
            S.op("dve", lambda e: e.tensor_copy(out=c.dst[:, ob, :], in_=sm[:, 8:12]), [Tsm], [Tdst])
            for k in range(4):
                S.dma("pool", lambda e, k=k: e.indirect_dma_start(
                    out=c.xg_scr[:, :], out_offset=bass.IndirectOffsetOnAxis(ap=c.dst[:, ob, k:k + 1], axis=0),
                    in_=x1b[:, :], in_offset=None, bounds_check=_breg(e, rc), oob_is_err=False), [Tx1b, Tdst], [Txg], sem_tile=Txg)

        for ob in range(NOB):
            block(ob)
        with nc.Block() as blk:
            S.emit(blk)


def phase4(c):
    nc = c.nc
    with ExitStack() as es:
        S = Sched(nc, es, "p4")
        sb = lambda n, s, d: _sb(nc, es, n, s, d)
        wg = [(sb(f"wg{i}", [128, 8, 2048], BF16), [T(f"wg{i}_{k}") for k in range(8)]) for i in range(2)]
        wd = [(sb(f"wd{i}", [128, 8, D], BF16), [T(f"wd{i}_{k}") for k in range(8)]) for i in range(2)]
        stg = Ring([(sb(f"stg{i}", [128, 2048], F32), T(f"stg{i}")) for i in range(3)])
        xgt = sb("xgt", [128, 3, D], BF16); Txgt = T("xgt")
        xgT = [(sb(f"xgT{i}", [128, 8, CAP], BF16), T(f"xgT{i}")) for i in range(2)]
        actT = [(sb(f"actT{i}", [128, 8, CAP], BF16), T(f"actT{i}")) for i in range(2)]
        glu = Ring([(sb(f"glu{i}", [128, CAP], F32), T(f"glu{i}")) for i in range(2)])
        sg = Ring([(sb(f"sg{i}", [128, CAP], F32), T(f"sg{i}")) for i in range(2)])
        lin = Ring([(sb(f"lin{i}", [128, CAP], F32), T(f"lin{i}")) for i in range(2)])
        ysb = Ring([(sb(f"ysb{i}", [128, D], F32), T(f"ysb{i}")) for i in range(2)])
        bdf = sb("bdf", [1, D], F32); Tbdf = T("bdf")
        bdb = sb("bdb", [128, D], BF16); Tbdb = T("bdb")
        bg = sb("bg", [128, NE * 16], F32); Tbg = T("bg")
        bl1 = sb("bl1", [128, NE * 16], F32); Tbl1 = T("bl1")
        tpl = [_ps(nc, es, f"tp4_{i}", [128, 1024], BF16) for i in range(2)]; Ttp = [T("tp4a"), T("tp4b")]
        psg = Ring([(_ps(nc, es, f"psg{i}", [128, 512], F32), T(f"psg{i}")) for i in range(2)])
        psl = Ring([(_ps(nc, es, f"psl{i}", [128, 512], F32), T(f"psl{i}")) for i in range(2)])
        psy = Ring([(_ps(nc, es, f"psy{i}", [128, 512], F32), T(f"psy{i}")) for i in range(2)])
        Tcst = T("cst"); Tys = T("y_scr")
        S.dma("sync", lambda e: e.dma_start(out=bg[:], in_=c.bgu[:, :]), [], [Tbg])
        S.op("pool", lambda e: e.memset(bdb[:], 0.0), [], [Tbdb])
        S.op("pool", lambda e: e.tensor_scalar(out=bl1[:], in0=bg[:], scalar1=1.0, scalar2=None, op0=ALU.add), [Tbg], [Tbl1])
        cast_cycle = ["act", "dve", "act", "act", "dve", "act", "act", "dve"]
        cc = [0]

        def cast(out, in_, reads, writes):
            eng = cast_cycle[cc[0] % len(cast_cycle)]
            cc[0] += 1
            if eng == "act":
                S.op("act", lambda e: e.copy(out=out, in_=in_), reads, writes)
            else:
                S.op(eng, lambda e: e.tensor_copy(out=out, in_=in_), reads, writes)

        def weight_chunks(e_):
            wgt, Twg = wg[e_ % 2]
            wdt, Twd = wd[e_ % 2]
            out = []

            def gu(kc):
                st, Tst = stg.next()
                S.dma("sync", lambda e: e.dma_start(out=st[:], in_=c.w_gu[e_, kc * 128:(kc + 1) * 128, :]), [], [Tst])
                v = st[:].rearrange("p (f two) -> p two f", two=2)
                cast(wgt[:, kc, 0:1024], v[:, 0, :], [Tst], [Twg[kc]])
                cast(wgt[:, kc, 1024:2048], v[:, 1, :], [Tst], [Twg[kc]])

            def dn(kc):
                st, Tst = stg.next()
                S.dma("sync", lambda e: e.dma_start(
                    out=st[:].rearrange("p (a d) -> p a d", a=2),
                    in_=c.w_down[e_, kc * 128:(kc + 2) * 128, :].rearrange("(a p) d -> p a d", p=128)), [], [Tst])
                cast(wdt[:, kc, :], st[:, 0:D], [Tst], [Twd[kc]])
                cast(wdt[:, kc + 1, :], st[:, D:2 * D], [Tst], [Twd[kc + 1]])

            for kc in range(8):
                out.append(lambda kc=kc: gu(kc))
            for kc in range(0, 8, 2):
                out.append(lambda kc=kc: dn(kc))
            return out

        def load_weights(e_):
            for f in weight_chunks(e_):
                f()

        hooks = []

        def step():
            if hooks:
                hooks.pop(0)()

        def expert(e_):
            wgt, Twg = wg[e_ % 2]
            wdt, Twd = wd[e_ % 2]
            xT_, TxT_ = xgT[e_ % 2]
            aT, TaT = actT[e_ % 2]
            S.dma("sync", lambda e: e.dma_start(out=xgt[:], in_=c.xg_scr[e_ * CAP:(e_ + 1) * CAP, :].rearrange("(s p) d -> p s d", p=128)),
                  [], [Txgt])
            S.dma("sync", lambda e: e.dma_start(out=bdf[:], in_=c.b_down[e_:e_ + 1, :]), [], [Tbdf])
            S.op("act", lambda e: e.copy(out=bdb[0:1, :], in_=bdf[:]), [Tbdf], [Tbdb])
            for kc in range(8 if c.p4mask & 2 else 0):
                hf = kc % 2
                for sc in range(3):
                    S.op("pe", lambda e, kc=kc, sc=sc, hf=hf: e.transpose(
                        out=tpl[hf][:, sc * 128:(sc + 1) * 128], in_=xgt[:, sc, kc * 128:(kc + 1) * 128],
                        identity=c.ident_bf), [Txgt, Tcst], [Ttp[hf]])
                if kc % 2:
                    S.op("act", lambda e, kc=kc, hf=hf: e.copy(out=xT_[:, kc, :], in_=tpl[hf][:, 0:CAP]), [Ttp[hf]], [TxT_])
                else:
                    S.op("dve", lambda e, kc=kc, hf=hf: e.tensor_copy(out=xT_[:, kc, :], in_=tpl[hf][:, 0:CAP]),
                         [Ttp[hf]], [TxT_])
            for fc in range(8 if c.p4mask & 4 else 0):
                pg, Tpg = psg.next()
                pl, Tpl = psl.next()
                for kc in range(8):
                    S.op("pe", lambda e, pg=pg, kc=kc, fc=fc: e.matmul(pg[:, 0:CAP], wgt[:, kc, fc * 128:(fc + 1) * 128], xT_[:, kc, :],
                                                                     start=(kc == 0), stop=(kc == 7)), [Twg[kc], TxT_], [Tpg])
                for kc in range(8):
                    S.op("pe", lambda e, pl=pl, kc=kc, fc=fc: e.matmul(pl[:, 0:CAP], wgt[:, kc, 1024 + fc * 128:1024 + (fc + 1) * 128],
                                                                     xT_[:, kc, :], start=(kc == 0), stop=(kc == 7)),
                         [Twg[kc], TxT_], [Tpl])
                gl_, Tgl = glu.next(); sg_, Tsg = sg.next(); ln_, Tln = lin.next()
                col = e_ * 16 + fc
                S.op("dve", lambda e, pg=pg, gl_=gl_, col=col: e.tensor_scalar(out=gl_[:], in0=pg[:, 0:CAP], scalar1=bg[:, col:col + 1],
                                                                              scalar2=7.0, op0=ALU.add, op1=ALU.min), [Tpg, Tbg], [Tgl])
                S.op("act", lambda e, gl_=gl_, sg_=sg_: e.activation(out=sg_[:], in_=gl_[:], func=AF.Sigmoid, scale=1.702), [Tgl], [Tsg])
                S.op("dve", lambda e, pl=pl, ln_=ln_, col=col: e.tensor_scalar(out=ln_[:], in0=pl[:, 0:CAP], scalar1=bl1[:, col + 8:col + 9],
                                                                              scalar2=-6.0, op0=ALU.add, op1=ALU.max), [Tpl, Tbl1], [Tln])
                S.op("pool", lambda e, gl_=gl_, sg_=sg_: e.tensor_tensor(out=sg_[:], in0=gl_[:], in1=sg_[:], op=ALU.mult), [Tgl, Tsg], [Tsg])
                S.op("dve", lambda e, ln_=ln_, sg_=sg_, fc=fc: e.scalar_tensor_tensor(out=aT[:, fc, :], in0=ln_[:], scalar=8.0, in1=sg_[:],
                                                                                     op0=ALU.min, op1=ALU.mult), [Tln, Tsg], [TaT])
                step()
            for sc in range(3 if c.p4mask & 16 else 0):
                yt, Tyt = ysb.next()
                for hf in range(2):
                    py, Tpy = psy.next()
                    for fc in range(8):
                        S.op("pe", lambda e, py=py, fc=fc, hf=hf, sc=sc: e.matmul(py[:], aT[:, fc, sc * 128:(sc + 1) * 128],
                                                                                wdt[:, fc, hf * 512:(hf + 1) * 512],
                                                                                start=(fc == 0), stop=False), [TaT, Twd[fc]], [Tpy])
                    S.op("pe", lambda e, py=py, hf=hf: e.matmul(py[:], c.row0_bf, bdb[:, hf * 512:(hf + 1) * 512],
                                                               start=False, stop=True), [Tcst, Tbdb], [Tpy])
                    if hf:
                        S.op("act", lambda e, py=py, yt=yt, hf=hf: e.copy(out=yt[:, hf * 512:(hf + 1) * 512], in_=py[:]), [Tpy], [Tyt])
                    else:
                        S.op("dve", lambda e, py=py, yt=yt, hf=hf: e.tensor_copy(out=yt[:, hf * 512:(hf + 1) * 512], in_=py[:]), [Tpy], [Tyt])
                S.dma("sync", lambda e, yt=yt, sc=sc: e.dma_start(out=c.y_scr[e_ * CAP + sc * 128:e_ * CAP + (sc + 1) * 128, :], in_=yt[:]),
                      [Tyt], [Tys], sem_tile=Tys)
                step()
            while hooks:
                step()

        load_weights(0)
        for e_ in range(c.nexp):
            if e_ + 1 < c.nexp:
                hooks.extend(weight_chunks(e_ + 1))
                step()
            expert(e_)
        with nc.Block() as blk:
            S.emit(blk)


def phase5(c):
    nc = c.nc
    with ExitStack() as es:
        S = Sched(nc, es, "p5")
        rc = {}
        sb = lambda n, s, d: _sb(nc, es, n, s, d)
        rowsb = sb("rows5", [128, 2, D], F32); Trows = T("rows5")
        yk = [Ring([(sb(f"yk{k}_{i}", [128, D], F32), T(f"yk{k}_{i}")) for i in range(2)]) for k in range(4)]
        x1r = Ring([(sb(f"x15_{i}", [128, D], F32), T(f"x15_{i}")) for i in range(2)])
        acc_l = [(sb(f"acc5_{i}", [128, D], F32), T(f"acc5_{i}")) for i in range(2)]
        outr = Ring([(sb(f"o5_{i}", [128, D], F32), T(f"o5_{i}")) for i in range(2)])
        B_l = [LNbufs(nc, es, f"c{i}") for i in range(2)]
        Tgts = T("gts"); Tdst = T("dst"); Tout = T("out")
        for i in range(2):
            S.dma("sync", lambda e, i=i: e.dma_start(out=rowsb[:, i, :], in_=_bcast_rows(c.rows, 128, D, offset=(4 + i) * D)),
                  [], [Trows])

        def block(ob):
            cs = slice(ob * 128, (ob + 1) * 128)
            acc, Tacc = acc_l[ob % 2]; B = B_l[ob % 2]
            ys = []
            for k in range(4):
                yt, Tyt = yk[k].next()
                ys.append((yt, Tyt))
                S.dma("pool", lambda e, yt=yt, k=k: e.indirect_dma_start(
                    out=yt[:, :], out_offset=None, in_=c.y_scr[:, :],
                    in_offset=bass.IndirectOffsetOnAxis(ap=c.dst[:, ob, k:k + 1], axis=0),
                    bounds_check=_breg(e, rc), oob_is_err=False), [Tdst], [Tyt])
            x1, Tx1 = x1r.next()
            S.dma("sync", lambda e: e.dma_start(out=x1[:], in_=c.x1_scr[cs, :]), [], [Tx1])
            S.op("dve", lambda e: e.tensor_scalar(out=acc[:], in0=ys[0][0][:], scalar1=c.gts[:, ob, 0:1], scalar2=None, op0=ALU.mult),
                 [ys[0][1], Tgts], [Tacc])
            for k in range(1, 4):
                S.op("dve", lambda e, k=k: e.scalar_tensor_tensor(out=acc[:], in0=ys[k][0][:], scalar=c.gts[:, ob, k:k + 1], in1=acc[:],
                                                                 op0=ALU.mult, op1=ALU.add), [ys[k][1], Tgts, Tacc], [Tacc])
            S.op("dve", lambda e: e.scalar_tensor_tensor(out=acc[:], in0=x1[:], scalar=float(ALPHA), in1=acc[:],
                                                         op0=ALU.mult, op1=ALU.add), [Tx1, Tacc], [Tacc])
            ot, Tot = outr.next()
            ln_tm(c, S, B, acc, Tacc, ot, Tot, rowsb[:, 0, :], rowsb[:, 1, :], Trows)
            S.dma("sync", lambda e: e.dma_start(out=c.out[cs, :], in_=ot[:]), [Tot], [Tout], sem_tile=Tout)

        for ob in range(NOB):
            block(ob)
        with nc.Block() as blk:
            S.emit(blk)


def _bf(a):
    return np.ascontiguousarray(a).astype(ml_dtypes.bfloat16)


def make_core_consts(r):
    f = np.float32
    p = np.arange(128, dtype=np.float64)
    kbt = np.zeros((128, 4, 64), f)
    dq = np.zeros((128, 4, 512), f)
    mfull = np.zeros((128, 4, 4, 128), f)
    q = np.arange(512)
    for h, sl in enumerate(SLOPES):
        for n in range(64):
            kbt[:, h, n] = sl * (128.0 * (n - 48 - r) + p)
        dq[:, h, :] = (-sl * (512.0 * (q // 128) + (q % 128)))[None, :]
        for jj in range(4):
            kpos = 128 * jj + np.arange(128)[:, None]
            qpos = 128 * r + np.arange(128)[None, :]
            allowed = (kpos // 64) <= (qpos // 64)
            mfull[:, h, jj, :] = np.where(allowed, -sl * np.abs(qpos - kpos), NEG)
    ident = np.eye(128, dtype=f)
    ones = np.ones((128, 128), f)
    ltri = (np.arange(128)[:, None] < np.arange(128)[None, :]).astype(f)
    row0 = np.zeros((128, 128), f)
    row0[0, :] = 1.0
    cst_bf = _bf(np.concatenate([ident, ones, ltri, row0], axis=1))
    cst_f = np.zeros((128, NCF), f)
    cst_f[:, 0:128] = ident
    cst_f[:, 128:256] = 1.0
    cst_f[:, 256:256 + NE] = (np.arange(NE) * CAP)[None, :]
    cst_f[:, 256 + NE + r] = 1.0
    cst_f[:, 292] = LN_EPS
    cst_f[:, 293] = 1.0
    cst_f[:, 294] = -0.5
    return {"kbt": kbt.reshape(128, -1), "dq": dq.reshape(128, -1), "mfull": mfull.reshape(128, -1),
            "cst_bf": cst_bf, "cst_f": cst_f}


def make_in_maps(inp, ne_w=NE):
    f = np.float32
    g = lambda k: np.asarray(inp[k], dtype=f)
    x = g("x")
    colp = np.zeros((128, 64), f)
    chunk = lambda v, n: np.asarray(v, f).reshape(n, 128).T
    colp[:, 0:8] = chunk(g("ln0_g"), 8)
    colp[:, 8:16] = chunk(g("ln0_b"), 8)
    cw = g("conv_w")[0]
    for ch in range(4):
        for w in range(4):
            colp[:, 16 + ch * 4 + w] = cw[w, ch * 128:(ch + 1) * 128]
    colp[:, 32:36] = chunk(g("conv_b")[0], 4)
    colp[:, 36:40] = chunk(g("b_rg_a")[0].reshape(512), 4)
    colp[:, 40:44] = chunk(g("b_rg_x")[0].reshape(512), 4)
    colp[:, 44:48] = chunk(g("lru_lambda")[0], 4)
    colp[:, 48] = g("subln_g")[0]
    rows = np.zeros((8, D), f)
    rows[0], rows[1] = g("ln0_g"), g("ln0_b")
    rows[2], rows[3] = g("ln1_g")[0], g("ln1_b")[0]
    rows[4], rows[5] = g("ln2_g")[0], g("ln2_b")[0]
    bgu = g("b_gu")[0]
    bg = bgu[:, 0::2].reshape(NE, 8, 128)
    bl = bgu[:, 1::2].reshape(NE, 8, 128)
    bgu_l = np.concatenate([bg, bl], axis=1).transpose(2, 0, 1).reshape(128, NE * 16)
    shared = {
        "w_in": np.ascontiguousarray(g("w_in")[0]), "w_out": np.ascontiguousarray(g("w_out")[0]),
        "w_router": np.ascontiguousarray(g("w_router")[0]), "w_gu": np.ascontiguousarray(g("w_gu")[0][:ne_w]),
        "w_down": np.ascontiguousarray(g("w_down")[0][:ne_w]), "b_down": np.ascontiguousarray(g("b_down")[0]),
        "rows": rows, "colp": colp,
        "w_rg": np.ascontiguousarray(np.stack([g("w_rg_a")[0], g("w_rg_x")[0]])),
        "lamv": np.ascontiguousarray(np.stack([g("lam_q1")[0], g("lam_k1")[0], g("lam_q2")[0], g("lam_k2")[0]])),
        "b_router": np.ascontiguousarray(g("b_router")), "bgu": np.ascontiguousarray(bgu_l),
    }
    maps = []
    for core in range(NCORES):
        b, r = core // 4, core % 4
        m = dict(shared)
        m["x_full"] = np.ascontiguousarray(x[b])
        m["x_own"] = np.ascontiguousarray(x[b].reshape(NOB, 4, 128, D)[:, r].reshape(OWN, D))
        m.update(make_core_consts(r))
        maps.append(m)
    return maps


def assemble(results):
    out = np.zeros((2, SEQ, D), np.float32)
    for core in range(NCORES):
        b, r = core // 4, core % 4
        o = np.asarray(results[core]["out"], np.float32).reshape(NOB, 128, D)
        out[b].reshape(NOB, 4, 128, D)[:, r] = o
    return out


_NC_CACHE = {}


def kernel(**inputs):
    if "nc" not in _NC_CACHE:
        _NC_CACHE["nc"] = build_program()
    nc = _NC_CACHE["nc"]
    maps = make_in_maps(inputs)
    res = run_bass_kernel_spmd(nc, maps, core_ids=list(range(NCORES)))
    return assemble(res.results)
```

```python
import math
from contextlib import ExitStack

import numpy as np
import ml_dtypes
import concourse.bass as bass
import concourse.mybir as mybir
from concourse.bass_utils import run_bass_kernel_spmd

F32 = mybir.dt.float32
BF16 = mybir.dt.bfloat16
U32 = mybir.dt.uint32
AF = mybir.ActivationFunctionType
ALU = mybir.AluOpType

NCORES = 8
D = 1024
SEQ = 8192
NBLK = SEQ // 128
OWN = 2048
NOB = OWN // 128
NE = 32
CAP = 384
DEPTH = 1
ALPHA = (2.0 * DEPTH) ** 0.25
LN_EPS = 1e-5
LAM_INIT = 0.8 - 0.6 * math.exp(-0.3 * 0)
SLOPES = [2.0 ** (-8.0 * (i + 1) / 4) for i in range(4)]
NEG = -30000.0
NCF = 128 + 128 + NE + 4 + 4


class T:
    __slots__ = ("name", "w", "r", "dsem", "dcnt")

    def __init__(self, name):
        self.name = name
        self.w = None
        self.r = {}
        self.dsem = None
        self.dcnt = 0


ENGS = ("sync", "pe", "act", "dve", "pool")


SEM_STACK = [None]


class Sched:
    def __init__(self, nc, es, tag):
        self.nc = nc
        self.es = SEM_STACK[0]
        self.tag = tag
        self.q = {e: [] for e in ENGS}
        self.sem = {}
        self.cnt = {}
        for e in ("pe", "act", "dve", "pool"):
            self.sem[e] = self.es.enter_context(nc.semaphore(f"{tag}_{e}"))
            self.cnt[e] = 0
        self.seen = {e: {} for e in ENGS}
        self.semobj = {e: self.sem[e] for e in self.sem}
        self.dma_tiles = []
        self.nsem = 4

    def _waits(self, eng, reads, writes, strict=False):
        deps = {}

        def add(m, same_ok):
            if m is None:
                return
            k, v = m
            if k == eng and not (same_ok or strict):
                return
            if deps.get(k, 0) < v:
                deps[k] = v

        for t in reads:
            add(t.w, eng != "pe")
        for t in writes:
            add(t.w, False)
            for k, v in t.r.items():
                add((k, v), False)
        out = []
        for k, v in deps.items():
            if self.seen[eng].get(k, 0) >= v:
                continue
            self.seen[eng][k] = v
            out.append((self.semobj[k], v))
        return out

    def _mark(self, mark, reads, writes):
        k, v = mark
        for t in reads:
            if t.r.get(k, 0) < v:
                t.r[k] = v
        for t in writes:
            t.w = mark
            t.r = {}

    def op(self, eng, fn, reads=(), writes=()):
        waits = self._waits(eng, reads, writes)
        self.cnt[eng] += 1
        mark = (eng, self.cnt[eng])
        self.q[eng].append((waits, fn, (self.sem[eng], 1)))
        self._mark(mark, reads, writes)

    def dma(self, queue, fn, reads, writes, sem_tile=None):
        st = sem_tile if sem_tile is not None else writes[0]
        if st.dsem is None:
            st.dsem = self.es.enter_context(self.nc.semaphore(f"{self.tag}_d{self.nsem}"))
            self.nsem += 1
            self.semobj[id(st)] = st.dsem
            self.dma_tiles.append(st)
        waits = self._waits(queue, reads, writes, strict=True)
        st.dcnt += 16
        mark = (id(st), st.dcnt)
        self.q[queue].append((waits, fn, (st.dsem, 16)))
        if queue == "pool":
            pass
        self._mark(mark, reads, writes)

    def emit(self, block):
        final = [(t.dsem, t.dcnt) for t in self.dma_tiles]
        self.q["sync"].append((final, None, None))
        table = (("sync", block.sync), ("pe", block.tensor), ("act", block.scalar),
                 ("dve", block.vector), ("pool", block.gpsimd))
        for name, deco in table:
            ops = self.q[name]

            def body(eng, ops=ops):
                for waits, fn, inc in ops:
                    for s, v in waits:
                        eng.wait_ge(s, v)
                    if fn is not None:
                        ins = fn(eng)
                        if inc is not None:
                            ins.then_inc(inc[0], inc[1])

            deco(body)


class Ring:
    def __init__(self, bufs):
        self.bufs = bufs
        self.i = 0

    def next(self):
        b = self.bufs[self.i % len(self.bufs)]
        self.i += 1
        return b


class Ctx:
    pass


def _sb(nc, es, name, shape, dt):
    return es.enter_context(nc.sbuf_tensor(name, shape, dt))


def _ps(nc, es, name, shape, dt):
    return es.enter_context(nc.psum_tensor(name, shape, dt))


def _breg(e, cache):
    if "r" not in cache:
        cache["r"] = e.to_reg(NE * CAP - 1)
    return cache["r"]


def _bcast_rows(handle, nrows, ncols, offset=0):
    return bass.AP(handle, offset, [[0, nrows], [1, ncols]])


def build_program(debug=False, upto=5, nexp=NE, p4mask=31, only=None):
    nc = bass.Bass("TRN2", target_bir_lowering=False)
    c = Ctx()
    c.nc = nc
    dk = "ExternalOutput" if debug else "Internal"

    def din(name, shape, dt=F32):
        return nc.dram_tensor(name, list(shape), dt, kind="ExternalInput")

    c.x_full = din("x_full", [SEQ, D])
    c.x_own = din("x_own", [OWN, D])
    c.w_in = din("w_in", [D, 2560])
    c.w_out = din("w_out", [D, D])
    c.w_router = din("w_router", [D, NE])
    ne_w = nexp if upto >= 4 else 1
    c.nexp = nexp
    c.p4mask = p4mask
    c.w_gu = din("w_gu", [ne_w, D, 2048])
    c.w_down = din("w_down", [ne_w, D, D])
    c.b_down = din("b_down", [NE, D])
    c.rows = din("rows", [8, D])
    c.colp = din("colp", [128, 64])
    c.w_rg = din("w_rg", [2, 8, 64, 64])
    c.lamv = din("lamv", [4, 64])
    c.b_router = din("b_router", [1, NE])
    c.bgu = din("bgu", [128, NE * 16])
    c.kbt = din("kbt", [128, 4 * 64])
    c.dq = din("dq", [128, 4 * 512])
    c.mfull = din("mfull", [128, 16 * 128])
    c.cst_bf = din("cst_bf", [128, 4 * 128], BF16)
    c.cst_f = din("cst_f", [128, NCF])
    c.out = nc.dram_tensor("out", [OWN, D], F32, kind="ExternalOutput")
    c.k_scr = nc.dram_tensor("k_scr", [4, 128, SEQ], BF16, kind=dk)
    c.v_scr = nc.dram_tensor("v_scr", [4, 128, NBLK, 128], BF16, kind=dk)
    c.q_scr = nc.dram_tensor("q_scr", [4, 128, 2, OWN], BF16, kind=dk)
    c.xg_scr = nc.dram_tensor("xg_scr", [NE * CAP, D], BF16, kind=dk)
    c.y_scr = nc.dram_tensor("y_scr", [NE * CAP, D], F32, kind=dk)
    c.x1_scr = nc.dram_tensor("x1_scr", [OWN, D], F32, kind=dk)
    c.dbg = nc.dram_tensor("dbg", [128, 4 * OWN * 2], BF16, kind=dk)
    c.dbg_gts = nc.dram_tensor("dbg_gts", [128, NOB * 4], F32, kind=dk)
    c.dbg_dst = nc.dram_tensor("dbg_dst", [128, NOB * 4], U32, kind=dk)

    with ExitStack() as es:
        SEM_STACK[0] = es
        c.cbf = _sb(nc, es, "cbf", [128, 4 * 128], BF16)
        c.cf = _sb(nc, es, "cf", [128, NCF], F32)
        c.colp_sb = _sb(nc, es, "colp_sb", [128, 64], F32)
        c.gts = _sb(nc, es, "gts", [128, NOB, 4], F32)
        c.dst = _sb(nc, es, "dst", [128, NOB, 4], U32)
        c.ident_bf = c.cbf[:, 0:128]
        c.ones_bf = c.cbf[:, 128:256]
        c.ltri_bf = c.cbf[:, 256:384]
        c.row0_bf = c.cbf[:, 384:512]
        c.ident_f = c.cf[:, 0:128]
        c.ones_f = c.cf[:, 128:256]
        c.ecap = c.cf[:, 256:256 + NE]
        c.sel = c.cf[:, 256 + NE:256 + NE + 4]
        c.epsc = c.cf[:, 292:293]
        c.onec = c.cf[:, 293:294]

        if only is not None:
            S0 = Sched(nc, es, "p0")
            S0.dma("sync", lambda e: e.dma_start(out=c.cbf[:], in_=c.cst_bf[:, :]), [], [T("a")])
            S0.dma("sync", lambda e: e.dma_start(out=c.cf[:], in_=c.cst_f[:, :]), [], [T("b")])
            with nc.Block() as blk:
                S0.emit(blk)
            {4: phase4, 5: phase5}[only](c)
            return nc
        with ExitStack() as es2:
            c.recT = _sb(nc, es2, "recT", [128, 4, OWN], BF16)
            phase1(c)
            c.attT = _sb(nc, es2, "attT", [128, 4, OWN], BF16)
            if upto >= 2:
                phase2(c)
            if upto >= 3:
                phase3(c)
            if debug:
                phase_dbg(c)
        if upto >= 4:
            phase4(c)
        if upto >= 5:
            phase5(c)
    return nc


def phase_dbg(c):
    nc = c.nc
    with ExitStack() as es:
        S = Sched(nc, es, "pd")
        td = T("dbg")
        S.dma("sync", lambda e: e.dma_start(out=c.dbg[:, 0:4 * OWN], in_=c.recT[:].rearrange("p a b -> p (a b)")), [], [td])
        S.dma("sync", lambda e: e.dma_start(out=c.dbg[:, 4 * OWN:8 * OWN], in_=c.attT[:].rearrange("p a b -> p (a b)")), [], [td])
        S.dma("sync", lambda e: e.dma_start(out=c.dbg_gts[:, :], in_=c.gts[:].rearrange("p a b -> p (a b)")), [], [td])
        S.dma("sync", lambda e: e.dma_start(out=c.dbg_dst[:, :], in_=c.dst[:].rearrange("p a b -> p (a b)")), [], [td])
        with nc.Block() as blk:
            S.emit(blk)


def phase1(c):
    nc = c.nc
    with ExitStack() as es:
        S = Sched(nc, es, "p1")
        sb = lambda n, s, d: _sb(nc, es, n, s, d)
        c.Tk_scr, c.Tv_scr, c.Tq_scr = T("k_scr"), T("v_scr"), T("q_scr")
        win = sb("win", [128, 8, 2560], BF16)
        Twin = [T(f"win{k}") for k in range(8)]
        wst = Ring([(sb(f"wst{i}", [128, 1280], F32), T(f"wst{i}")) for i in range(2)])
        xs = Ring([(sb(f"xs{i}", [128, D], F32), T(f"xs{i}")) for i in range(3)])
        stt = sb("stt", [128, 4, 12], F32); Tstt_l = [T(f"stt{i}") for i in range(4)]
        mv = sb("mv", [128, 4, 2], F32); Tmv_l = [T(f"mv{i}") for i in range(4)]
        rstd = sb("rstd", [128, 4], F32); Trstd_l = [T(f"rstd{i}") for i in range(4)]
        nmr = sb("nmr", [128, 4], F32); Tnmr_l = [T(f"nmr{i}") for i in range(4)]
        xn = sb("xn", [128, 4, D], BF16); Txn = [T(f"xn{b}") for b in range(4)]
        xT = sb("xT", [128, 8, 512], BF16); TxT = [T(f"xT{k}") for k in range(8)]
        kst = sb("kst", [128, 4, 512], BF16); Tkst = T("kst")
        vst = sb("vst", [128, 4, 512], BF16); Tvst = T("vst")
        qst = sb("qst", [128, 4, 2, 512], BF16); Tqst = T("qst")
        xlb = [(sb(f"xlb{i}", [128, 4, 515], F32), T(f"xlb{i}")) for i in range(2)]
        xc = sb("xc", [128, 4, 512], F32); Txc = T("xc")
        xcb = sb("xcb", [128, 4, 512], BF16); Txcb = T("xcb")
        rg = sb("rg", [128, 4, 512], F32); Trg = T("rg")
        ig = sb("ig", [128, 4, 512], F32); Tig = T("ig")
        aa = sb("aa", [128, 4, 512], F32); Taa = T("aa")
        sq = sb("sq", [128, 4, 512], F32); Tsq = T("sq")
        hb = [(sb(f"hb{i}", [128, 4, 512], F32), T(f"hb{i}")) for i in range(2)]
        hsel = sb("hsel", [128, 4, 128], F32); Thsel = T("hsel")
        ge = sb("ge", [128, 512], F32); Tge = T("ge")
        bdst = sb("bdst", [128, 2, 4, 128], F32); Tbdst = T("bdst")
        bd = sb("bd", [128, 2, 4, 128], BF16); Tbd = T("bd")
        lrp = sb("lrp", [128, 12], F32); Tlrp = T("lrp")
        Tcst = T("cst"); Tcolp = T("colp"); TrecT = T("recT")
        pst = [(_ps(nc, es, f"pst{i}", [128, 1024], BF16), T(f"pst{i}")) for i in range(2)]
        psr = Ring([(_ps(nc, es, f"psm{i}", [128, 512], F32), T(f"psm{i}")) for i in range(6)])
        colp = c.colp_sb

        S.dma("sync", lambda e: e.dma_start(out=c.cbf[:], in_=c.cst_bf[:, :]), [], [Tcst])
        S.dma("sync", lambda e: e.dma_start(out=c.cf[:], in_=c.cst_f[:, :]), [], [Tcst])
        S.dma("sync", lambda e: e.dma_start(out=colp[:], in_=c.colp[:, :]), [], [Tcolp])
        Tparts = [T(f"bdp{i}") for i in range(16)]
        S.op("pool", lambda e: e.memset(bdst[:].rearrange("p a b c -> p (a b c)"), 0.0), [], [Tbdst] + Tparts)
        for m in range(2):
            for ch in range(4):
                for u in range(2):
                    S.dma("sync", lambda e, m=m, ch=ch, u=u: e.dma_start(
                        out=bdst[u * 64:(u + 1) * 64, m, ch, u * 64:(u + 1) * 64],
                        in_=c.w_rg[m, 2 * ch + u, :, :]), [], [Tparts[m * 8 + ch * 2 + u]])
        S.op("act", lambda e: e.copy(out=bd[:].rearrange("p a b c -> p (a b c)"),
                                     in_=bdst[:].rearrange("p a b c -> p (a b c)")), [Tbdst] + Tparts, [Tbd])
        S.op("act", lambda e: e.activation(out=lrp[:, 8:12], in_=colp[:, 44:48], func=AF.Exp, scale=-1.0),
             [Tcolp], [Tlrp])
        S.op("act", lambda e: e.activation(out=lrp[:, 8:12], in_=lrp[:, 8:12], func=AF.Ln, bias=1.0),
             [Tlrp], [Tlrp])
        S.op("dve", lambda e: e.tensor_scalar(out=lrp[:, 0:4], in0=lrp[:, 8:12], scalar1=-8.0, scalar2=None,
                                              op0=ALU.mult), [Tlrp], [Tlrp])
        S.op("dve", lambda e: e.tensor_scalar(out=lrp[:, 4:8], in0=lrp[:, 8:12], scalar1=-16.0, scalar2=None,
                                              op0=ALU.mult), [Tlrp], [Tlrp])
        S.op("pool", lambda e: e.memset(qst[:].rearrange("p a b c -> p (a b c)"), 0.0), [], [Tqst])
        S.op("pool", lambda e: e.memset(xlb[0][0][:, :, 0:3], 0.0), [], [xlb[0][1]])
        cast_engs = ("act", "pool")
        for kc in range(8):
            for hf in range(2):
                st, Tst = wst.next()
                S.dma("sync", lambda e, st=st, kc=kc, hf=hf: e.dma_start(
                    out=st[:], in_=c.w_in[kc * 128:(kc + 1) * 128, hf * 1280:(hf + 1) * 1280]), [], [Tst])
                eng = cast_engs[(kc * 2 + hf) % 2]
                if eng == "act":
                    S.op("act", lambda e, st=st, kc=kc, hf=hf: e.copy(out=win[:, kc, hf * 1280:(hf + 1) * 1280], in_=st[:]),
                         [Tst], [Twin[kc]])
                else:
                    S.op("pool", lambda e, st=st, kc=kc, hf=hf: e.tensor_copy(out=win[:, kc, hf * 1280:(hf + 1) * 1280], in_=st[:]),
                         [Tst], [Twin[kc]])

        def mm_group(ps, Tps, lhs_fn, rhs_fn, reads_fn):
            for kc in range(8):
                S.op("pe", lambda e, kc=kc: e.matmul(ps, lhs_fn(kc), rhs_fn(kc), start=(kc == 0), stop=(kc == 7)),
                     reads_fn(kc), [Tps])

        def do_tile(it):
            own = it >= 16
            src = c.x_own if own else c.x_full
            t0 = (it - 16) * 512 if own else it * 512
            xsl = []
            for b in range(4):
                xt, Txt = xs.next()
                Tstt, Tmv, Trstd, Tnmr = Tstt_l[b], Tmv_l[b], Trstd_l[b], Tnmr_l[b]
                xsl.append((xt, Txt))
                S.dma("sync", lambda e, xt=xt, b=b: e.dma_start(out=xt[:], in_=src[t0 + b * 128:t0 + (b + 1) * 128, :]),
                      [], [Txt])
                for hh in range(2):
                    S.op("dve", lambda e, xt=xt, b=b, hh=hh: e.bn_stats(out=stt[:, b, hh * 6:(hh + 1) * 6],
                                                                     in_=xt[:, hh * 512:(hh + 1) * 512]), [Txt], [Tstt])
                S.op("dve", lambda e, b=b: e.bn_aggr(out=mv[:, b, :], in_=stt[:, b, :]), [Tstt], [Tmv])
                if b == 2 or b == 3:
                    pass
                S.op("act", lambda e, b=b: e.activation(out=rstd[:, b:b + 1], in_=mv[:, b, 1:2], func=AF.Sqrt,
                                                        bias=c.epsc[:, 0:1], scale=1.0), [Tmv], [Trstd])
                S.op("dve", lambda e, b=b: e.reciprocal(out=rstd[:, b:b + 1], in_=rstd[:, b:b + 1]), [Trstd], [Trstd])
                S.op("dve", lambda e, b=b: e.scalar_tensor_tensor(out=nmr[:, b:b + 1], in0=mv[:, b, 0:1], scalar=-1.0,
                                                                  in1=rstd[:, b:b + 1], op0=ALU.mult, op1=ALU.mult),
                     [Tmv, Trstd], [Tnmr])
                S.op("act", lambda e, xt=xt, b=b: e.activation(out=xn[:, b, :], in_=xt[:], func=AF.Identity,
                                                                scale=rstd[:, b:b + 1], bias=nmr[:, b:b + 1]),
                     [Txt, Trstd, Tnmr], [Txn[b]])
            for kc in range(8):
                pt, Tpt = pst[(kc // 2) % 2]
                off = (kc % 2) * 512
                for b in range(4):
                    S.op("pe", lambda e, pt=pt, off=off, b=b, kc=kc: e.transpose(
                        out=pt[:, off + b * 128:off + (b + 1) * 128], in_=xn[:, b, kc * 128:(kc + 1) * 128],
                        identity=c.ident_bf), [Txn[b], Tcst], [Tpt])
                S.op("act", lambda e, pt=pt, off=off, kc=kc: e.activation(
                    out=xT[:, kc, :], in_=pt[:, off:off + 512], func=AF.Identity,
                    scale=colp[:, kc:kc + 1], bias=colp[:, 8 + kc:9 + kc]), [Tpt, Tcolp], [TxT[kc]])
            if not own:
                for h in range(4):
                    ps, Tps = psr.next()
                    mm_group(ps[:], Tps, lambda kc, h=h: win[:, kc, 512 + h * 128:512 + (h + 1) * 128],
                             lambda kc: xT[:, kc, :], lambda kc: [Twin[kc], TxT[kc]])
                    S.op("dve" if h % 2 else "act",
                         (lambda e, ps=ps, h=h: e.tensor_copy(out=kst[:, h, :], in_=ps[:])) if h % 2 else
                         (lambda e, ps=ps, h=h: e.copy(out=kst[:, h, :], in_=ps[:])), [Tps], [Tkst])
                for h in range(4):
                    S.dma("sync", lambda e, h=h: e.dma_start(out=c.k_scr[h, :, t0:t0 + 512], in_=kst[:, h, :]),
                          [Tkst], [c.Tk_scr], sem_tile=c.Tk_scr)
                for b in range(4):
                    ps, Tps = psr.next()
                    mm_group(ps[:], Tps, lambda kc, b=b: xT[:, kc, b * 128:(b + 1) * 128],
                             lambda kc: win[:, kc, 1024:1536], lambda kc: [Twin[kc], TxT[kc]])
                    S.op("dve" if b % 2 else "act",
                         (lambda e, ps=ps, b=b: e.tensor_copy(out=vst[:, b, :], in_=ps[:])) if b % 2 else
                         (lambda e, ps=ps, b=b: e.copy(out=vst[:, b, :], in_=ps[:])), [Tps], [Tvst])
                for h in range(4):
                    S.dma("sync", lambda e, h=h: e.dma_start(out=c.v_scr[h, :, it * 4:(it + 1) * 4, :],
                                                              in_=vst[:, :, h * 128:(h + 1) * 128]),
                          [Tvst], [c.Tv_scr], sem_tile=c.Tv_scr)
                xl, Txl = xlb[it % 2]
                xl2, Txl2 = xlb[(it + 1) % 2]
                hcur, Thcur = hb[it % 2]
                hprev, Thprev = hb[(it + 1) % 2]
                for ch in range(4):
                    ps, Tps = psr.next()
                    mm_group(ps[:], Tps, lambda kc, ch=ch: win[:, kc, 1536 + ch * 128:1536 + (ch + 1) * 128],
                             lambda kc: xT[:, kc, :], lambda kc: [Twin[kc], TxT[kc]])
                    S.op("dve", lambda e, ps=ps, ch=ch, xl=xl: e.tensor_copy(out=xl[:, ch, 3:515], in_=ps[:]), [Tps], [Txl])
                S.op("pool", lambda e, xl=xl, xl2=xl2: e.tensor_copy(out=xl2[:, :, 0:3], in_=xl[:, :, 512:515]),
                     [Txl], [Txl2])
                for ch in range(4):
                    S.op("dve", lambda e, ch=ch, xl=xl: e.tensor_scalar(
                        out=xc[:, ch, :], in0=xl[:, ch, 3:515], scalar1=colp[:, 16 + ch * 4 + 3:16 + ch * 4 + 4],
                        scalar2=colp[:, 32 + ch:33 + ch], op0=ALU.mult, op1=ALU.add), [Txl, Tcolp], [Txc])
                    for w in (2, 1, 0):
                        S.op("dve", lambda e, ch=ch, w=w, xl=xl: e.scalar_tensor_tensor(
                            out=xc[:, ch, :], in0=xl[:, ch, w:w + 512], scalar=colp[:, 16 + ch * 4 + w:16 + ch * 4 + w + 1],
                            in1=xc[:, ch, :], op0=ALU.mult, op1=ALU.add), [Txl, Tcolp, Txc], [Txc])
                S.op("act", lambda e: e.copy(out=xcb[:].rearrange("p a b -> p (a b)"),
                                             in_=xc[:].rearrange("p a b -> p (a b)")), [Txc], [Txcb])
                for ch in range(4):
                    for m, (dstt, Tdst, bo) in enumerate(((rg, Trg, 36), (ig, Tig, 40))):
                        ps, Tps = psr.next()
                        S.op("pe", lambda e, ps=ps, m=m, ch=ch: e.matmul(ps[:], bd[:, m, ch, :], xcb[:, ch, :],
                                                                         start=True, stop=True), [Tbd, Txcb], [Tps])
                        S.op("act", lambda e, ps=ps, ch=ch, dstt=dstt, bo=bo: e.activation(
                            out=dstt[:, ch, :], in_=ps[:], func=AF.Sigmoid, bias=colp[:, bo + ch:bo + ch + 1], scale=1.0),
                            [Tps, Tcolp], [Tdst])
                for ch in range(4):
                    S.op("act", lambda e, ch=ch: e.activation(out=aa[:, ch, :], in_=rg[:, ch, :], func=AF.Exp,
                                                              scale=lrp[:, ch:ch + 1]), [Trg, Tlrp], [Taa])
                    S.op("act", lambda e, ch=ch: e.activation(out=sq[:, ch, :], in_=rg[:, ch, :], func=AF.Exp,
                                                              scale=lrp[:, 4 + ch:5 + ch]), [Trg, Tlrp], [Tsq])
                S.op("act", lambda e: e.activation(out=sq[:].rearrange("p a b -> p (a b)"),
                                                   in_=sq[:].rearrange("p a b -> p (a b)"), func=AF.Sqrt,
                                                   bias=c.onec[:, 0:1], scale=-1.0), [Tsq], [Tsq])
                S.op("pool", lambda e: e.tensor_tensor(out=ig[:].rearrange("p a b -> p (a b)"),
                                                       in0=ig[:].rearrange("p a b -> p (a b)"),
                                                       in1=xc[:].rearrange("p a b -> p (a b)"), op=ALU.mult),
                     [Tig, Txc], [Tig])
                S.op("dve", lambda e: e.tensor_tensor(out=ig[:].rearrange("p a b -> p (a b)"),
                                                      in0=ig[:].rearrange("p a b -> p (a b)"),
                                                      in1=sq[:].rearrange("p a b -> p (a b)"), op=ALU.mult),
                     [Tig, Tsq], [Tig])
                for ch in range(4):
                    init = 0.0 if it == 0 else hprev[:, ch, 511:512]
                    S.op("dve", lambda e, ch=ch, init=init, hcur=hcur: e.tensor_tensor_scan(
                        out=hcur[:, ch, :], data0=aa[:, ch, :], data1=ig[:, ch, :], initial=init,
                        op0=ALU.mult, op1=ALU.add), [Taa, Tig] + ([] if it == 0 else [Thprev]), [Thcur])
                S.op("dve", lambda e, hcur=hcur: e.tensor_scalar(out=hsel[:], in0=hcur[:, :, 0:128], scalar1=c.sel[:, 0:1],
                                                                 scalar2=None, op0=ALU.mult), [Thcur, Tcst], [Thsel])
                for t in (1, 2):
                    S.op("dve", lambda e, t=t, hcur=hcur: e.scalar_tensor_tensor(
                        out=hsel[:], in0=hcur[:, :, t * 128:(t + 1) * 128], scalar=c.sel[:, t:t + 1], in1=hsel[:],
                        op0=ALU.mult, op1=ALU.add), [Thcur, Tcst, Thsel], [Thsel])
                S.op("dve", lambda e, hcur=hcur, it=it: e.scalar_tensor_tensor(
                    out=c.recT[:, :, it * 128:(it + 1) * 128], in0=hcur[:, :, 384:512], scalar=c.sel[:, 3:4], in1=hsel[:],
                    op0=ALU.mult, op1=ALU.add), [Thcur, Tcst, Thsel], [TrecT])
            else:
                ot = it - 16
                for h in range(4):
                    ps, Tps = psr.next()
                    mm_group(ps[:], Tps, lambda kc, h=h: win[:, kc, h * 128:(h + 1) * 128],
                             lambda kc: xT[:, kc, :], lambda kc: [Twin[kc], TxT[kc]])
                    S.op("act", lambda e, ps=ps, h=h: e.mul(out=qst[0:64, h, 0, :], in_=ps[0:64, :], mul=0.125), [Tps], [Tqst])
                    S.op("dve", lambda e, ps=ps, h=h: e.tensor_scalar(out=qst[64:128, h, 1, :], in0=ps[64:128, :], scalar1=0.125,
                                                                      scalar2=None, op0=ALU.mult), [Tps], [Tqst])
                for h in range(4):
                    S.dma("sync", lambda e, h=h, ot=ot: e.dma_start(out=c.q_scr[h, :, :, ot * 512:(ot + 1) * 512],
                                                                     in_=qst[:, h, :, :]), [Tqst], [c.Tq_scr], sem_tile=c.Tq_scr)
                for ch in range(4):
                    ps, Tps = psr.next()
                    mm_group(ps[:], Tps, lambda kc, ch=ch: win[:, kc, 2048 + ch * 128:2048 + (ch + 1) * 128],
                             lambda kc: xT[:, kc, :], lambda kc: [Twin[kc], TxT[kc]])
                    S.op("act", lambda e, ps=ps: e.activation(out=ge[:], in_=ps[:], func=AF.Gelu), [Tps], [Tge])
                    S.op("dve", lambda e, ch=ch, ot=ot: e.tensor_tensor(
                        out=c.recT[:, ch, ot * 512:(ot + 1) * 512], in0=c.recT[:, ch, ot * 512:(ot + 1) * 512], in1=ge[:],
                        op=ALU.mult), [Tge, TrecT], [TrecT])

        for it in range(20):
            do_tile(it)
        with nc.Block() as blk:
            S.emit(blk)


def phase2(c):
    nc = c.nc
    with ExitStack() as es:
        S = Sched(nc, es, "p2")
        sb = lambda n, s, d: _sb(nc, es, n, s, d)
        kbt = sb("kbt_s", [128, 4, 64], F32); dq = sb("dq_s", [128, 4, 512], F32); mf = sb("mf_s", [128, 4, 4, 128], F32)
        lamt = sb("lamt", [128, 4, 64], F32); lw = sb("lw", [128, 8], F32); junk = sb("junk2", [128, 64], F32)
        Tc = T("c2"); Tlam = T("lam"); Tlw = T("lw"); Tjunk = T("junk"); TattT = T("attT"); Tcst = T("cst")
        Kr = Ring([(sb(f"Kh{i}", [128, SEQ], BF16), T(f"Kh{i}")) for i in range(2)])
        Vr = Ring([(sb(f"Vh{i}", [128, NBLK, 128], BF16), T(f"Vh{i}")) for i in range(2)])
        Qr = Ring([(sb(f"Qh{i}", [128, 2, OWN], BF16), T(f"Qh{i}")) for i in range(2)])
        sbr = Ring([(sb(f"sbs{i}", [128, 512], F32), T(f"sbs{i}")) for i in range(4)])
        pbr = Ring([(sb(f"pb{i}", [128, 512], BF16), T(f"pb{i}")) for i in range(6)])
        rz = [(sb(f"rz{i}", [128, 512], F32), T(f"rz{i}")) for i in range(2)]
        oo = [(sb(f"oo{i}", [128, 512], F32), T(f"oo{i}")) for i in range(2)]
        osq = sb("osq", [128, 512], F32); Tosq = T("osq")
        rst = sb("rst", [128, 512], F32); Trst = T("rst")
        Sr = Ring([(_ps(nc, es, f"S{i}", [128, 512], F32), T(f"S{i}")) for i in range(4)])
        Ab = [(_ps(nc, es, f"A{i}", [128, 512], F32), T(f"A{i}")) for i in range(2)]
        Zb = [(_ps(nc, es, f"Z{i}", [128, 512], F32), T(f"Z{i}")) for i in range(2)]
        colp = c.colp_sb

        S.dma("sync", lambda e: e.dma_start(out=kbt[:].rearrange("p a b -> p (a b)"), in_=c.kbt[:, :]), [], [Tc])
        S.dma("sync", lambda e: e.dma_start(out=dq[:].rearrange("p a b -> p (a b)"), in_=c.dq[:, :]), [], [Tc])
        S.dma("sync", lambda e: e.dma_start(out=mf[:].rearrange("p a b c -> p (a b c)"), in_=c.mfull[:, :]), [], [Tc])
        S.dma("sync", lambda e: e.dma_start(out=lamt[:].rearrange("p a b -> p (a b)"), in_=_bcast_rows(c.lamv, 128, 256)),
              [], [Tlam])
        for i in range(2):
            S.op("dve", lambda e, i=i: e.scalar_tensor_tensor(out=junk[:], in0=lamt[:, 2 * i, :], scalar=1.0,
                                                             in1=lamt[:, 2 * i + 1, :], op0=ALU.mult, op1=ALU.mult,
                                                             accum_out=lw[:, i:i + 1]), [Tlam], [Tjunk, Tlw])
        S.op("act", lambda e: e.activation(out=lw[:, 2:4], in_=lw[:, 0:2], func=AF.Exp), [Tlw], [Tlw])
        S.op("dve", lambda e: e.tensor_tensor(out=lw[:, 4:5], in0=lw[:, 3:4], in1=lw[:, 2:3], op=ALU.subtract), [Tlw], [Tlw])
        S.op("dve", lambda e: e.tensor_scalar(out=lw[:, 5:6], in0=lw[:, 4:5], scalar1=-LAM_INIT, scalar2=None, op0=ALU.add),
             [Tlw], [Tlw])
        S.op("dve", lambda e: e.tensor_scalar(out=lw[:, 6:7], in0=colp[:, 48:49], scalar1=(1.0 - LAM_INIT), scalar2=None,
                                              op0=ALU.mult), [], [Tlw])

        def head(h):
            Kh, TK = Kr.next(); Vh, TV = Vr.next(); Qh, TQ = Qr.next()
            S.dma("sync", lambda e: e.dma_start(out=Kh[:], in_=c.k_scr[h, :, :]), [], [TK])
            S.dma("sync", lambda e: e.dma_start(out=Vh[:], in_=c.v_scr[h, :, :, :]), [], [TV])
            S.dma("sync", lambda e: e.dma_start(out=Qh[:], in_=c.q_scr[h, :, :, :]), [], [TQ])

            def qtile(g):
                nj = 16 * g + 16
                pend = {}
                j0 = max(0, 16 * g - 1 - int(math.ceil(1.0 / SLOPES[h])))

                def scores(j):
                    c0 = 0 if j < 16 * g else 128 * ((j - 16 * g) // 4)
                    n = j - 16 * g + 48
                    ps_l = []
                    for m in range(2):
                        ps, Tps = Sr.next()
                        S.op("pe", lambda e, ps=ps, m=m: e.matmul(ps[:, c0:512], Kh[:, j * 128:(j + 1) * 128],
                                                                   Qh[:, m, g * 512 + c0:(g + 1) * 512], start=True, stop=True),
                             [TK, TQ], [Tps])
                        sbt, Tsb = sbr.next()
                        if j >= 16 * g:
                            jj = (j - 16 * g) % 4
                            S.op("dve", lambda e, ps=ps, sbt=sbt: e.tensor_tensor(out=sbt[:, c0:c0 + 128], in0=ps[:, c0:c0 + 128],
                                                                                  in1=mf[:, h, jj, :], op=ALU.add), [Tps, Tc], [Tsb])
                            if c0 + 128 < 512:
                                S.op("dve", lambda e, ps=ps, sbt=sbt: e.scalar_tensor_tensor(
                                    out=sbt[:, c0 + 128:512], in0=ps[:, c0 + 128:512], scalar=kbt[:, h, n:n + 1],
                                    in1=dq[:, h, c0 + 128:512], op0=ALU.add, op1=ALU.add), [Tps, Tc], [Tsb])
                        else:
                            S.op("dve", lambda e, ps=ps, sbt=sbt: e.scalar_tensor_tensor(
                                out=sbt[:, :], in0=ps[:, :], scalar=kbt[:, h, n:n + 1], in1=dq[:, h, :],
                                op0=ALU.add, op1=ALU.add), [Tps, Tc], [Tsb])
                        pb, Tpb = pbr.next()
                        S.op("act", lambda e, sbt=sbt, pb=pb: e.activation(out=pb[:, c0:512], in_=sbt[:, c0:512], func=AF.Exp),
                             [Tsb], [Tpb])
                        ps_l.append((pb, Tpb))
                    pend[j] = (c0, ps_l)

                def av(j):
                    c0, ps_l = pend.pop(j)
                    for m in range(2):
                        pb, Tpb = ps_l[m]
                        S.op("pe", lambda e, pb=pb, m=m: e.matmul(Ab[m][0][:, c0:512], Vh[:, j, :], pb[:, c0:512],
                                                                   start=(j == j0), stop=(j == nj - 1)), [TV, Tpb], [Ab[m][1]])
                        S.op("pe", lambda e, pb=pb, m=m: e.matmul(Zb[m][0][:, c0:512], c.ones_bf, pb[:, c0:512],
                                                                   start=(j == j0), stop=(j == nj - 1)), [Tcst, Tpb], [Zb[m][1]])

                j0 = max(0, 16 * g - 1 - int(math.ceil(1.0 / SLOPES[h])))
                for st in range(j0, nj + 1):
                    if st < nj:
                        scores(st)
                    if st >= j0 + 1:
                        av(st - 1)
                for m in range(2):
                    S.op("dve", lambda e, m=m: e.reciprocal(out=rz[m][0][:], in_=Zb[m][0][:]), [Zb[m][1]], [rz[m][1]])
                    S.op("dve", lambda e, m=m: e.tensor_tensor(out=oo[m][0][:], in0=Ab[m][0][:], in1=rz[m][0][:], op=ALU.mult),
                         [Ab[m][1], rz[m][1]], [oo[m][1]])
                S.op("dve", lambda e: e.scalar_tensor_tensor(out=oo[0][0][:], in0=oo[1][0][:], scalar=lw[:, 5:6], in1=oo[0][0][:],
                                                             op0=ALU.mult, op1=ALU.add), [oo[0][1], oo[1][1], Tlw], [oo[0][1]])
                S.op("pool", lambda e: e.tensor_tensor(out=osq[:], in0=oo[0][0][:], in1=oo[0][0][:], op=ALU.mult), [oo[0][1]], [Tosq])
                ps, Tps = Sr.next()
                S.op("pe", lambda e, ps=ps: e.matmul(ps[:], c.ones_f, osq[:], start=True, stop=True), [Tosq, Tcst], [Tps])
                S.op("act", lambda e, ps=ps: e.activation(out=rst[:], in_=ps[:], func=AF.Sqrt, bias=c.epsc[:, 0:1],
                                                          scale=1.0 / 128.0), [Tps, Tcst], [Trst])
                S.op("dve", lambda e: e.reciprocal(out=rst[:], in_=rst[:]), [Trst], [Trst])
                S.op("dve", lambda e: e.scalar_tensor_tensor(out=c.attT[:, h, g * 512:(g + 1) * 512], in0=oo[0][0][:],
                                                             scalar=lw[:, 6:7], in1=rst[:], op0=ALU.mult, op1=ALU.mult),
                     [oo[0][1], Trst, Tlw], [TattT])

            for g in range(4):
                qtile(g)

        for h in range(4):
            head(h)
        with nc.Block() as blk:
            S.emit(blk)


class LNbufs:
    def __init__(self, nc, es, tag):
        self.stt = _sb(nc, es, f"ln_stt_{tag}", [128, 12], F32); self.Tstt = T("stt")
        self.mv = _sb(nc, es, f"ln_mv_{tag}", [128, 2], F32); self.Tmv = T("mv")
        self.rs = _sb(nc, es, f"ln_rs_{tag}", [128, 1], F32); self.Trs = T("rs")
        self.nm = _sb(nc, es, f"ln_nm_{tag}", [128, 1], F32); self.Tnm = T("nm")
        self.tmp = _sb(nc, es, f"ln_tmp_{tag}", [128, D], F32); self.Ttmp = T("tmp")


def ln_tm(c, S, B, src, Tsrc, dst, Tdst, grow, brow, Trows):
    for hh in range(2):
        S.op("dve", lambda e, hh=hh: e.bn_stats(out=B.stt[:, hh * 6:(hh + 1) * 6], in_=src[:, hh * 512:(hh + 1) * 512]),
             [Tsrc], [B.Tstt])
    S.op("dve", lambda e: e.bn_aggr(out=B.mv[:], in_=B.stt[:]), [B.Tstt], [B.Tmv])
    S.op("act", lambda e: e.activation(out=B.rs[:], in_=B.mv[:, 1:2], func=AF.Sqrt, bias=c.epsc[:, 0:1], scale=1.0),
         [B.Tmv], [B.Trs])
    S.op("dve", lambda e: e.reciprocal(out=B.rs[:], in_=B.rs[:]), [B.Trs], [B.Trs])
    S.op("dve", lambda e: e.scalar_tensor_tensor(out=B.nm[:], in0=B.mv[:, 0:1], scalar=-1.0, in1=B.rs[:],
                                                 op0=ALU.mult, op1=ALU.mult), [B.Tmv, B.Trs], [B.Tnm])
    S.op("act", lambda e: e.activation(out=B.tmp[:], in_=src[:], func=AF.Identity, scale=B.rs[:, 0:1], bias=B.nm[:, 0:1]),
         [Tsrc, B.Trs, B.Tnm], [B.Ttmp])
    S.op("dve", lambda e: e.tensor_tensor(out=B.tmp[:], in0=B.tmp[:], in1=grow, op=ALU.mult), [B.Ttmp, Trows], [B.Ttmp])
    S.op("pool", lambda e: e.tensor_tensor(out=dst[:], in0=B.tmp[:], in1=brow, op=ALU.add), [B.Ttmp, Trows], [Tdst])


def phase3(c):
    nc = c.nc
    with ExitStack() as es:
        S = Sched(nc, es, "p3")
        rc = {}
        sb = lambda n, s, d: _sb(nc, es, n, s, d)
        wo = sb("wo", [128, 8, D], BF16); Two = [T(f"wo{k}") for k in range(8)]
        wost = Ring([(sb(f"wost{i}", [128, D], F32), T(f"wost{i}")) for i in range(2)])
        wr = sb("wr", [128, 8, NE], F32); Twr = T("wr")
        rowsb = sb("rowsb", [128, 4, D], F32); Trows = T("rows")
        brt = sb("brt", [128, NE], F32); Tbrt = T("brt")
        msk = sb("msk", [128, NOB, NE], BF16); Tmsk = [T(f"msk{i}") for i in range(NOB)]
        xs = Ring([(sb(f"xs3_{i}", [128, D], F32), T(f"xs3_{i}")) for i in range(2)])
        x0_l = [(sb(f"x0_{i}", [128, D], F32), T(f"x0_{i}")) for i in range(2)]
        yy_l = [(sb(f"yy_{i}", [128, D], F32), T(f"yy_{i}")) for i in range(2)]
        x1r = Ring([(sb(f"x1_{i}", [128, D], F32), T(f"x1_{i}")) for i in range(2)])
        x1br = Ring([(sb(f"x1b_{i}", [128, D], BF16), T(f"x1b_{i}")) for i in range(2)])
        x1T_l = [(sb(f"x1T_{i}", [128, 8, 128], F32), T(f"x1T_{i}")) for i in range(2)]
        lg_l = [(sb(f"lg_{i}", [128, NE], F32), T(f"lg_{i}")) for i in range(2)]
        t8_l = [(sb(f"t8_{i}", [128, 8], F32), T(f"t8_{i}")) for i in range(2)]
        sm_l = [(sb(f"sm3_{i}", [128, 16], F32), T(f"sm3_{i}")) for i in range(2)]
        destf_l = [(sb(f"destf_{i}", [128, NE], F32), T(f"destf_{i}")) for i in range(2)]
        junk_l = [(sb(f"junk3_{i}", [128, NE], F32), T(f"junk3_{i}")) for i in range(2)]
        B0_l = [LNbufs(nc, es, f"a{i}") for i in range(2)]; B1_l = [LNbufs(nc, es, f"b{i}") for i in range(2)]
        mixr = Ring([(_ps(nc, es, f"mix{i}", [128, 512], F32), T(f"mix{i}")) for i in range(4)])
        tpf = [(_ps(nc, es, f"tpf{i}", [128, 512], F32), T(f"tpf{i}")) for i in range(2)]
        lgp = _ps(nc, es, "lgp", [128, 512], F32); Tlgp = T("lgp")
        posp = _ps(nc, es, "posp", [128, 512], F32); Tposp = T("posp")
        Tcst = T("cst"); Tatt = T("att"); Trec = T("rec"); Tgts = T("gts"); Tdst = T("dst")
        Txg = T("xg_scr"); Tx1s = T("x1_scr")

        for i in range(4):
            S.dma("sync", lambda e, i=i: e.dma_start(out=rowsb[:, i, :], in_=_bcast_rows(c.rows, 128, D, offset=i * D)),
                  [], [Trows])
        S.dma("sync", lambda e: e.dma_start(out=brt[:], in_=_bcast_rows(c.b_router, 128, NE)), [], [Tbrt])
        for kc in range(8):
            S.dma("sync", lambda e, kc=kc: e.dma_start(out=wr[:, kc, :], in_=c.w_router[kc * 128:(kc + 1) * 128, :]), [], [Twr])
            st, Tst = wost.next()
            S.dma("sync", lambda e, st=st, kc=kc: e.dma_start(out=st[:], in_=c.w_out[kc * 128:(kc + 1) * 128, :]), [], [Tst])
            if kc % 2:
                S.op("act", lambda e, st=st, kc=kc: e.copy(out=wo[:, kc, :], in_=st[:]), [Tst], [Two[kc]])
            else:
                S.op("pool", lambda e, st=st, kc=kc: e.tensor_copy(out=wo[:, kc, :], in_=st[:]), [Tst], [Two[kc]])

        def block(ob):
            cs = slice(ob * 128, (ob + 1) * 128)
            x0, Tx0 = x0_l[ob % 2]; yy, Tyy = yy_l[ob % 2]; x1T, Tx1T = x1T_l[ob % 2]; lg, Tlg = lg_l[ob % 2]
            t8, Tt8 = t8_l[ob % 2]; sm, Tsm = sm_l[ob % 2]; destf, Tdestf = destf_l[ob % 2]; junk, Tjunk = junk_l[ob % 2]
            B0 = B0_l[ob % 2]; B1 = B1_l[ob % 2]
            xt, Txt = xs.next()
            S.dma("sync", lambda e: e.dma_start(out=xt[:], in_=c.x_own[cs, :]), [], [Txt])
            ln_tm(c, S, B0, xt, Txt, x0, Tx0, rowsb[:, 0, :], rowsb[:, 1, :], Trows)
            for hf in range(2):
                ps, Tps = mixr.next()
                for kc in range(8):
                    lhs = c.attT[:, kc, cs] if kc < 4 else c.recT[:, kc - 4, cs]
                    S.op("pe", lambda e, ps=ps, lhs=lhs, kc=kc, hf=hf: e.matmul(ps[:], lhs, wo[:, kc, hf * 512:(hf + 1) * 512],
                                                                              start=(kc == 0), stop=(kc == 7)),
                         [Two[kc], Tatt, Trec], [Tps])
                S.op("dve", lambda e, ps=ps, hf=hf: e.scalar_tensor_tensor(
                    out=yy[:, hf * 512:(hf + 1) * 512], in0=x0[:, hf * 512:(hf + 1) * 512], scalar=float(ALPHA), in1=ps[:],
                    op0=ALU.mult, op1=ALU.add), [Tx0, Tps], [Tyy])
            x1, Tx1 = x1r.next()
            ln_tm(c, S, B1, yy, Tyy, x1, Tx1, rowsb[:, 2, :], rowsb[:, 3, :], Trows)
            S.dma("sync", lambda e: e.dma_start(out=c.x1_scr[cs, :], in_=x1[:]), [Tx1], [Tx1s], sem_tile=Tx1s)
            x1b, Tx1b = x1br.next()
            S.op("act", lambda e: e.copy(out=x1b[:], in_=x1[:]), [Tx1], [Tx1b])
            for kc in range(8):
                pt, Tpt = tpf[kc // 4]
                S.op("pe", lambda e, pt=pt, kc=kc: e.transpose(out=pt[:, (kc % 4) * 128:(kc % 4 + 1) * 128],
                                                               in_=x1[:, kc * 128:(kc + 1) * 128], identity=c.ident_f),
                     [Tx1, Tcst], [Tpt])
            S.op("act", lambda e: e.copy(out=x1T[:, 0:4, :].rearrange("p a b -> p (a b)"), in_=tpf[0][0][:]), [tpf[0][1]], [Tx1T])
            S.op("dve", lambda e: e.tensor_copy(out=x1T[:, 4:8, :].rearrange("p a b -> p (a b)"), in_=tpf[1][0][:]),
                 [tpf[1][1]], [Tx1T])
            for kc in range(8):
                S.op("pe", lambda e, kc=kc: e.matmul(lgp[:, 0:NE], x1T[:, kc, :], wr[:, kc, :], start=(kc == 0), stop=(kc == 7)),
                     [Tx1T, Twr], [Tlgp])
            S.op("dve", lambda e: e.tensor_tensor(out=lg[:], in0=lgp[:, 0:NE], in1=brt[:], op=ALU.add), [Tlgp, Tbrt], [Tlg])
            S.op("dve", lambda e: e.max(out=t8[:], in_=lg[:]), [Tlg], [Tt8])
            S.op("dve", lambda e: e.tensor_scalar(out=sm[:, 0:1], in0=t8[:, 0:1], scalar1=-1.0, scalar2=None, op0=ALU.mult),
                 [Tt8], [Tsm])
            S.op("act", lambda e: e.activation(out=sm[:, 4:8], in_=t8[:, 0:4], func=AF.Exp, bias=sm[:, 0:1], scale=1.0,
                                               accum_out=sm[:, 1:2]), [Tt8, Tsm], [Tsm])
            S.op("dve", lambda e: e.reciprocal(out=sm[:, 1:2], in_=sm[:, 1:2]), [Tsm], [Tsm])
            S.op("dve", lambda e: e.tensor_scalar(out=c.gts[:, ob, :], in0=sm[:, 4:8], scalar1=sm[:, 1:2], scalar2=None,
                                                  op0=ALU.mult), [Tsm], [Tgts])
            S.op("dve", lambda e: e.tensor_scalar(out=msk[:, ob, :], in0=lg[:], scalar1=t8[:, 3:4], scalar2=None, op0=ALU.is_ge),
                 [Tlg, Tt8], [Tmsk[ob]])
            for o2 in range(ob + 1):
                lhs = c.ltri_bf if o2 == ob else c.ones_bf
                S.op("pe", lambda e, lhs=lhs, o2=o2: e.matmul(posp[:, 0:NE], lhs, msk[:, o2, :], start=(o2 == 0), stop=(o2 == ob)),
                     [Tmsk[o2], Tcst], [Tposp])
            S.op("dve", lambda e: e.tensor_tensor(out=destf[:], in0=posp[:, 0:NE], in1=c.ecap, op=ALU.add), [Tposp, Tcst], [Tdestf])
            for k in range(4):
                S.op("dve", lambda e, k=k: e.scalar_tensor_tensor(out=junk[:], in0=lg[:], scalar=t8[:, k:k + 1], in1=destf[:],
                                                                 op0=ALU.is_equal, op1=ALU.mult, accum_out=sm[:, 8 + k:9 + k]),
                     [Tlg, Tt8, Tdestf], [Tjunk, Tsm])
            S.op("dve", lambda e: e.tensor_copy(out=c.dst[:, ob, :], in_=sm[:, 8:12]), [Tsm], [Tdst])
            for k in range(4):
                S.dma("pool", lambda e, k=k: e.indirect_dma_start(
                    out=c.xg_scr[:, :], out_offset=bass.IndirectOffsetOnAxis(ap=c.dst[:, ob, k:k + 1], axis=0),
                    in_=x1b[:, :], in_offset=None, bounds_check=_breg(e, rc), oob_is_err=False), [Tx1b, Tdst], [Txg], sem_tile=Txg)

        for ob in range(NOB):
            block(ob)
        with nc.Block() as blk:
            S.emit(blk)


def phase4(c):
    nc = c.nc
    with ExitStack() as es:
        S = Sched(nc, es, "p4")
        sb = lambda n, s, d: _sb(nc, es, n, s, d)
        wg = [(sb(f"wg{i}", [128, 8, 2048], BF16), [T(f"wg{i}_{k}") for k in range(8)]) for i in range(2)]
        wd = [(sb(f"wd{i}", [128, 8, D], BF16), [T(f"wd{i}_{k}") for k in range(8)]) for i in range(2)]
        stg = Ring([(sb(f"stg{i}", [128, 2048], F32), T(f"stg{i}")) for i in range(5)])
        xgt = sb("xgt", [128, 3, D], BF16); Txgt = T("xgt")
        xgT = [(sb(f"xgT{i}", [128, 8, CAP], BF16), T(f"xgT{i}")) for i in range(2)]
        actT = [(sb(f"actT{i}", [128, 8, CAP], BF16), T(f"actT{i}")) for i in range(2)]
        glu = Ring([(sb(f"glu{i}", [128, CAP], F32), T(f"glu{i}")) for i in range(2)])
        sg = Ring([(sb(f"sg{i}", [128, CAP], F32), T(f"sg{i}")) for i in range(2)])
        lin = Ring([(sb(f"lin{i}", [128, CAP], F32), T(f"lin{i}")) for i in range(2)])
        ysb = Ring([(sb(f"ysb{i}", [128, D], F32), T(f"ysb{i}")) for i in range(2)])
        bdf = sb("bdf", [1, D], F32); Tbdf = T("bdf")
        bdb = sb("bdb", [128, D], BF16); Tbdb = T("bdb")
        bg = sb("bg", [128, NE * 16], F32); Tbg = T("bg")
        bl1 = sb("bl1", [128, NE * 16], F32); Tbl1 = T("bl1")
        tpl = [_ps(nc, es, f"tp4_{i}", [128, 1024], BF16) for i in range(2)]; Ttp = [T("tp4a"), T("tp4b")]
        psg = Ring([(_ps(nc, es, f"psg{i}", [128, 512], F32), T(f"psg{i}")) for i in range(2)])
        psl = Ring([(_ps(nc, es, f"psl{i}", [128, 512], F32), T(f"psl{i}")) for i in range(2)])
        psy = Ring([(_ps(nc, es, f"psy{i}", [128, 512], F32), T(f"psy{i}")) for i in range(2)])
        Tcst = T("cst"); Tys = T("y_scr")
        S.dma("sync", lambda e: e.dma_start(out=bg[:], in_=c.bgu[:, :]), [], [Tbg])
        S.op("pool", lambda e: e.memset(bdb[:], 0.0), [], [Tbdb])
        S.op("pool", lambda e: e.tensor_scalar(out=bl1[:], in0=bg[:], scalar1=1.0, scalar2=None, op0=ALU.add), [Tbg], [Tbl1])
        cast_cycle = ["act", "dve", "act", "act", "dve", "act", "act", "dve"]
        cc = [0]

        def cast(out, in_, reads, writes):
            eng = cast_cycle[cc[0] % len(cast_cycle)]
            cc[0] += 1
            if eng == "act":
                S.op("act", lambda e: e.copy(out=out, in_=in_), reads, writes)
            else:
                S.op(eng, lambda e: e.tensor_copy(out=out, in_=in_), reads, writes)

        def weight_chunks(e_):
            wgt, Twg = wg[e_ % 2]
            wdt, Twd = wd[e_ % 2]
            out = []

            def gu(kc):
                st, Tst = stg.next()
                S.dma("sync", lambda e: e.dma_start(out=st[:], in_=c.w_gu[e_, kc * 128:(kc + 1) * 128, :]), [], [Tst])
                v = st[:].rearrange("p (f two) -> p two f", two=2)
                cast(wgt[:, kc, 0:1024], v[:, 0, :], [Tst], [Twg[kc]])
                cast(wgt[:, kc, 1024:2048], v[:, 1, :], [Tst], [Twg[kc]])

            def dn(kc):
                st, Tst = stg.next()
                S.dma("sync", lambda e: e.dma_start(
                    out=st[:].rearrange("p (a d) -> p a d", a=2),
                    in_=c.w_down[e_, kc * 128:(kc + 2) * 128, :].rearrange("(a p) d -> p a d", p=128)), [], [Tst])
                cast(wdt[:, kc, :], st[:, 0:D], [Tst], [Twd[kc]])
                cast(wdt[:, kc + 1, :], st[:, D:2 * D], [Tst], [Twd[kc + 1]])

            for kc in range(8):
                out.append(lambda kc=kc: gu(kc))
            for kc in range(0, 8, 2):
                out.append(lambda kc=kc: dn(kc))
            return out

        def load_weights(e_):
            for f in weight_chunks(e_):
                f()

        hooks = []

        def step():
            if hooks:
                hooks.pop(0)()

        def expert(e_):
            wgt, Twg = wg[e_ % 2]
            wdt, Twd = wd[e_ % 2]
            xT_, TxT_ = xgT[e_ % 2]
            aT, TaT = actT[e_ % 2]
            S.dma("sync", lambda e: e.dma_start(out=xgt[:], in_=c.xg_scr[e_ * CAP:(e_ + 1) * CAP, :].rearrange("(s p) d -> p s d", p=128)),
                  [], [Txgt])
            S.dma("sync", lambda e: e.dma_start(out=bdf[:], in_=c.b_down[e_:e_ + 1, :]), [], [Tbdf])
            S.op("act", lambda e: e.copy(out=bdb[0:1, :], in_=bdf[:]), [Tbdf], [Tbdb])
            for kc in range(8 if c.p4mask & 2 else 0):
                hf = kc % 2
                for sc in range(3):
                    S.op("pe", lambda e, kc=kc, sc=sc, hf=hf: e.transpose(
                        out=tpl[hf][:, sc * 128:(sc + 1) * 128], in_=xgt[:, sc, kc * 128:(kc + 1) * 128],
                        identity=c.ident_bf), [Txgt, Tcst], [Ttp[hf]])
                if kc % 2:
                    S.op("act", lambda e, kc=kc, hf=hf: e.copy(out=xT_[:, kc, :], in_=tpl[hf][:, 0:CAP]), [Ttp[hf]], [TxT_])
                else:
                    S.op("dve", lambda e, kc=kc, hf=hf: e.tensor_copy(out=xT_[:, kc, :], in_=tpl[hf][:, 0:CAP]),
                         [Ttp[hf]], [TxT_])
            for fc in range(8 if c.p4mask & 4 else 0):
                pg, Tpg = psg.next()
                pl, Tpl = psl.next()
                for kc in range(8):
                    S.op("pe", lambda e, pg=pg, kc=kc, fc=fc: e.matmul(pg[:, 0:CAP], wgt[:, kc, fc * 128:(fc + 1) * 128], xT_[:, kc, :],
                                                                     start=(kc == 0), stop=(kc == 7)), [Twg[kc], TxT_], [Tpg])
                for kc in range(8):
                    S.op("pe", lambda e, pl=pl, kc=kc, fc=fc: e.matmul(pl[:, 0:CAP], wgt[:, kc, 1024 + fc * 128:1024 + (fc + 1) * 128],
                                                                     xT_[:, kc, :], start=(kc == 0), stop=(kc == 7)),
                         [Twg[kc], TxT_], [Tpl])
                gl_, Tgl = glu.next(); sg_, Tsg = sg.next(); ln_, Tln = lin.next()
                col = e_ * 16 + fc
                S.op("dve", lambda e, pg=pg, gl_=gl_, col=col: e.tensor_scalar(out=gl_[:], in0=pg[:, 0:CAP], scalar1=bg[:, col:col + 1],
                                                                              scalar2=7.0, op0=ALU.add, op1=ALU.min), [Tpg, Tbg], [Tgl])
                S.op("act", lambda e, gl_=gl_, sg_=sg_: e.activation(out=sg_[:], in_=gl_[:], func=AF.Sigmoid, scale=1.702), [Tgl], [Tsg])
                S.op("dve", lambda e, pl=pl, ln_=ln_, col=col: e.tensor_scalar(out=ln_[:], in0=pl[:, 0:CAP], scalar1=bl1[:, col + 8:col + 9],
                                                                              scalar2=-6.0, op0=ALU.add, op1=ALU.max), [Tpl, Tbl1], [Tln])
                S.op("pool", lambda e, gl_=gl_, sg_=sg_: e.tensor_tensor(out=sg_[:], in0=gl_[:], in1=sg_[:], op=ALU.mult), [Tgl, Tsg], [Tsg])
                S.op("dve", lambda e, ln_=ln_, sg_=sg_, fc=fc: e.scalar_tensor_tensor(out=aT[:, fc, :], in0=ln_[:], scalar=8.0, in1=sg_[:],
                                                                                     op0=ALU.min, op1=ALU.mult), [Tln, Tsg], [TaT])
                step()
            for sc in range(3 if c.p4mask & 16 else 0):
                yt, Tyt = ysb.next()
                for hf in range(2):
                    py, Tpy = psy.next()
                    for fc in range(8):
                        S.op("pe", lambda e, py=py, fc=fc, hf=hf, sc=sc: e.matmul(py[:], aT[:, fc, sc * 128:(sc + 1) * 128],
                                                                                wdt[:, fc, hf * 512:(hf + 1) * 512],
                                                                                start=(fc == 0), stop=False), [TaT, Twd[fc]], [Tpy])
                    S.op("pe", lambda e, py=py, hf=hf: e.matmul(py[:], c.row0_bf, bdb[:, hf * 512:(hf + 1) * 512],
                                                               start=False, stop=True), [Tcst, Tbdb], [Tpy])
                    if hf:
                        S.op("act", lambda e, py=py, yt=yt, hf=hf: e.copy(out=yt[:, hf * 512:(hf + 1) * 512], in_=py[:]), [Tpy], [Tyt])
                    else:
                        S.op("dve", lambda e, py=py, yt=yt, hf=hf: e.tensor_copy(out=yt[:, hf * 512:(hf + 1) * 512], in_=py[:]), [Tpy], [Tyt])
                S.dma("sync", lambda e, yt=yt, sc=sc: e.dma_start(out=c.y_scr[e_ * CAP + sc * 128:e_ * CAP + (sc + 1) * 128, :], in_=yt[:]),
                      [Tyt], [Tys], sem_tile=Tys)
                step()
            while hooks:
                step()

        load_weights(0)
        for e_ in range(c.nexp):
            if e_ + 1 < c.nexp:
                hooks.extend(weight_chunks(e_ + 1))
                step()
            expert(e_)
        with nc.Block() as blk:
            S.emit(blk)


def phase5(c):
    nc = c.nc
    with ExitStack() as es:
        S = Sched(nc, es, "p5")
        rc = {}
        sb = lambda n, s, d: _sb(nc, es, n, s, d)
        rowsb = sb("rows5", [128, 2, D], F32); Trows = T("rows5")
        yk = [Ring([(sb(f"yk{k}_{i}", [128, D], F32), T(f"yk{k}_{i}")) for i in range(2)]) for k in range(4)]
        x1r = Ring([(sb(f"x15_{i}", [128, D], F32), T(f"x15_{i}")) for i in range(2)])
        acc_l = [(sb(f"acc5_{i}", [128, D], F32), T(f"acc5_{i}")) for i in range(2)]
        outr = Ring([(sb(f"o5_{i}", [128, D], F32), T(f"o5_{i}")) for i in range(2)])
        B_l = [LNbufs(nc, es, f"c{i}") for i in range(2)]
        Tgts = T("gts"); Tdst = T("dst"); Tout = T("out")
        for i in range(2):
            S.dma("sync", lambda e, i=i: e.dma_start(out=rowsb[:, i, :], in_=_bcast_rows(c.rows, 128, D, offset=(4 + i) * D)),
                  [], [Trows])

        def block(ob):
            cs = slice(ob * 128, (ob + 1) * 128)
            acc, Tacc = acc_l[ob % 2]; B = B_l[ob % 2]
            ys = []
            for k in range(4):
                yt, Tyt = yk[k].next()
                ys.append((yt, Tyt))
                S.dma("pool", lambda e, yt=yt, k=k: e.indirect_dma_start(
                    out=yt[:, :], out_offset=None, in_=c.y_scr[:, :],
                    in_offset=bass.IndirectOffsetOnAxis(ap=c.dst[:, ob, k:k + 1], axis=0),
                    bounds_check=_breg(e, rc), oob_is_err=False), [Tdst], [Tyt])
            x1, Tx1 = x1r.next()
            S.dma("sync", lambda e: e.dma_start(out=x1[:], in_=c.x1_scr[cs, :]), [], [Tx1])
            S.op("dve", lambda e: e.tensor_scalar(out=acc[:], in0=ys[0][0][:], scalar1=c.gts[:, ob, 0:1], scalar2=None, op0=ALU.mult),
                 [ys[0][1], Tgts], [Tacc])
            for k in range(1, 4):
                S.op("dve", lambda e, k=k: e.scalar_tensor_tensor(out=acc[:], in0=ys[k][0][:], scalar=c.gts[:, ob, k:k + 1], in1=acc[:],
                                                                 op0=ALU.mult, op1=ALU.add), [ys[k][1], Tgts, Tacc], [Tacc])
            S.op("dve", lambda e: e.scalar_tensor_tensor(out=acc[:], in0=x1[:], scalar=float(ALPHA), in1=acc[:],
                                                         op0=ALU.mult, op1=ALU.add), [Tx1, Tacc], [Tacc])
            ot, Tot = outr.next()
            ln_tm(c, S, B, acc, Tacc, ot, Tot, rowsb[:, 0, :], rowsb[:, 1, :], Trows)
            S.dma("sync", lambda e: e.dma_start(out=c.out[cs, :], in_=ot[:]), [Tot], [Tout], sem_tile=Tout)

        for ob in range(NOB):
            block(ob)
        with nc.Block() as blk:
            S.emit(blk)


def _bf(a):
    return np.ascontiguousarray(a).astype(ml_dtypes.bfloat16)


def make_core_consts(r):
    f = np.float32
    p = np.arange(128, dtype=np.float64)
    kbt = np.zeros((128, 4, 64), f)
    dq = np.zeros((128, 4, 512), f)
    mfull = np.zeros((128, 4, 4, 128), f)
    q = np.arange(512)
    for h, sl in enumerate(SLOPES):
        for n in range(64):
            kbt[:, h, n] = sl * (128.0 * (n - 48 - r) + p)
        dq[:, h, :] = (-sl * (512.0 * (q // 128) + (q % 128)))[None, :]
        for jj in range(4):
            kpos = 128 * jj + np.arange(128)[:, None]
            qpos = 128 * r + np.arange(128)[None, :]
            allowed = (kpos // 64) <= (qpos // 64)
            mfull[:, h, jj, :] = np.where(allowed, -sl * np.abs(qpos - kpos), NEG)
    ident = np.eye(128, dtype=f)
    ones = np.ones((128, 128), f)
    ltri = (np.arange(128)[:, None] < np.arange(128)[None, :]).astype(f)
    row0 = np.zeros((128, 128), f)
    row0[0, :] = 1.0
    cst_bf = _bf(np.concatenate([ident, ones, ltri, row0], axis=1))
    cst_f = np.zeros((128, NCF), f)
    cst_f[:, 0:128] = ident
    cst_f[:, 128:256] = 1.0
    cst_f[:, 256:256 + NE] = (np.arange(NE) * CAP)[None, :]
    cst_f[:, 256 + NE + r] = 1.0
    cst_f[:, 292] = LN_EPS
    cst_f[:, 293] = 1.0
    cst_f[:, 294] = -0.5
    return {"kbt": kbt.reshape(128, -1), "dq": dq.reshape(128, -1), "mfull": mfull.reshape(128, -1),
            "cst_bf": cst_bf, "cst_f": cst_f}


def make_in_maps(inp, ne_w=NE):
    f = np.float32
    g = lambda k: np.asarray(inp[k], dtype=f)
    x = g("x")
    colp = np.zeros((128, 64), f)
    chunk = lambda v, n: np.asarray(v, f).reshape(n, 128).T
    colp[:, 0:8] = chunk(g("ln0_g"), 8)
    colp[:, 8:16] = chunk(g("ln0_b"), 8)
    cw = g("conv_w")[0]
    for ch in range(4):
        for w in range(4):
            colp[:, 16 + ch * 4 + w] = cw[w, ch * 128:(ch + 1) * 128]
    colp[:, 32:36] = chunk(g("conv_b")[0], 4)
    colp[:, 36:40] = chunk(g("b_rg_a")[0].reshape(512), 4)
    colp[:, 40:44] = chunk(g("b_rg_x")[0].reshape(512), 4)
    colp[:, 44:48] = chunk(g("lru_lambda")[0], 4)
    colp[:, 48] = g("subln_g")[0]
    rows = np.zeros((8, D), f)
    rows[0], rows[1] = g("ln0_g"), g("ln0_b")
    rows[2], rows[3] = g("ln1_g")[0], g("ln1_b")[0]
    rows[4], rows[5] = g("ln2_g")[0], g("ln2_b")[0]
    bgu = g("b_gu")[0]
    bg = bgu[:, 0::2].reshape(NE, 8, 128)
    bl = bgu[:, 1::2].reshape(NE, 8, 128)
    bgu_l = np.concatenate([bg, bl], axis=1).transpose(2, 0, 1).reshape(128, NE * 16)
    shared = {
        "w_in": np.ascontiguousarray(g("w_in")[0]), "w_out": np.ascontiguousarray(g("w_out")[0]),
        "w_router": np.ascontiguousarray(g("w_router")[0]), "w_gu": np.ascontiguousarray(g("w_gu")[0][:ne_w]),
        "w_down": np.ascontiguousarray(g("w_down")[0][:ne_w]), "b_down": np.ascontiguousarray(g("b_down")[0]),
        "rows": rows, "colp": colp,
        "w_rg": np.ascontiguousarray(np.stack([g("w_rg_a")[0], g("w_rg_x")[0]])),
        "lamv": np.ascontiguousarray(np.stack([g("lam_q1")[0], g("lam_k1")[0], g("lam_q2")[0], g("lam_k2")[0]])),
        "b_router": np.ascontiguousarray(g("b_router")), "bgu": np.ascontiguousarray(bgu_l),
    }
    maps = []
    for core in range(NCORES):
        b, r = core // 4, core % 4
        m = dict(shared)
        m["x_full"] = np.ascontiguousarray(x[b])
        m["x_own"] = np.ascontiguousarray(x[b].reshape(NOB, 4, 128, D)[:, r].reshape(OWN, D))
        m.update(make_core_consts(r))
        maps.append(m)
    return maps


def assemble(results):
    out = np.zeros((2, SEQ, D), np.float32)
    for core in range(NCORES):
        b, r = core // 4, core % 4
        o = np.asarray(results[core]["out"], np.float32).reshape(NOB, 128, D)
        out[b].reshape(NOB, 4, 128, D)[:, r] = o
    return out


_NC_CACHE = {}


def kernel(**inputs):
    if "nc" not in _NC_CACHE:
        _NC_CACHE["nc"] = build_program()
    nc = _NC_CACHE["nc"]
    maps = make_in_maps(inputs)
    res = run_bass_kernel_spmd(nc, maps, core_ids=list(range(NCORES)))
    return assemble(res.results)
```

```python
import math
from contextlib import ExitStack

import numpy as np
import ml_dtypes
import concourse.bass as bass
import concourse.mybir as mybir
from concourse.bass_utils import run_bass_kernel_spmd

F32 = mybir.dt.float32
BF16 = mybir.dt.bfloat16
U32 = mybir.dt.uint32
AF = mybir.ActivationFunctionType
ALU = mybir.AluOpType

NCORES = 8
D = 1024
SEQ = 8192
NBLK = SEQ // 128
OWN = 2048
NOB = OWN // 128
NE = 32
CAP = 384
DEPTH = 1
ALPHA = (2.0 * DEPTH) ** 0.25
LN_EPS = 1e-5
LAM_INIT = 0.8 - 0.6 * math.exp(-0.3 * 0)
SLOPES = [2.0 ** (-8.0 * (i + 1) / 4) for i in range(4)]
NEG = -30000.0
NCF = 128 + 128 + NE + 4 + 4


class T:
    __slots__ = ("name", "w", "r", "dsem", "dcnt")

    def __init__(self, name):
        self.name = name
        self.w = None
        self.r = {}
        self.dsem = None
        self.dcnt = 0


ENGS = ("sync", "pe", "act", "dve", "pool")


SEM_STACK = [None]


class Sched:
    def __init__(self, nc, es, tag):
        self.nc = nc
        self.es = SEM_STACK[0]
        self.tag = tag
        self.q = {e: [] for e in ENGS}
        self.sem = {}
        self.cnt = {}
        for e in ("pe", "act", "dve", "pool"):
            self.sem[e] = self.es.enter_context(nc.semaphore(f"{tag}_{e}"))
            self.cnt[e] = 0
        self.seen = {e: {} for e in ENGS}
        self.semobj = {e: self.sem[e] for e in self.sem}
        self.dma_tiles = []
        self.nsem = 4

    def _waits(self, eng, reads, writes, strict=False):
        deps = {}

        def add(m, same_ok):
            if m is None:
                return
            k, v = m
            if k == eng and not (same_ok or strict):
                return
            if deps.get(k, 0) < v:
                deps[k] = v

        for t in reads:
            add(t.w, eng != "pe")
        for t in writes:
            add(t.w, False)
            for k, v in t.r.items():
                add((k, v), False)
        out = []
        for k, v in deps.items():
            if self.seen[eng].get(k, 0) >= v:
                continue
            self.seen[eng][k] = v
            out.append((self.semobj[k], v))
        return out

    def _mark(self, mark, reads, writes):
        k, v = mark
        for t in reads:
            if t.r.get(k, 0) < v:
                t.r[k] = v
        for t in writes:
            t.w = mark
            t.r = {}

    def op(self, eng, fn, reads=(), writes=()):
        waits = self._waits(eng, reads, writes)
        self.cnt[eng] += 1
        mark = (eng, self.cnt[eng])
        self.q[eng].append((waits, fn, (self.sem[eng], 1)))
        self._mark(mark, reads, writes)

    def dma(self, queue, fn, reads, writes, sem_tile=None):
        st = sem_tile if sem_tile is not None else writes[0]
        if st.dsem is None:
            st.dsem = self.es.enter_context(self.nc.semaphore(f"{self.tag}_d{self.nsem}"))
            self.nsem += 1
            self.semobj[id(st)] = st.dsem
            self.dma_tiles.append(st)
        waits = self._waits(queue, reads, writes, strict=True)
        st.dcnt += 16
        mark = (id(st), st.dcnt)
        self.q[queue].append((waits, fn, (st.dsem, 16)))
        if queue == "pool":
            pass
        self._mark(mark, reads, writes)

    def emit(self, block):
        final = [(t.dsem, t.dcnt) for t in self.dma_tiles]
        self.q["sync"].append((final, None, None))
        table = (("sync", block.sync), ("pe", block.tensor), ("act", block.scalar),
                 ("dve", block.vector), ("pool", block.gpsimd))
        for name, deco in table:
            ops = self.q[name]

            def body(eng, ops=ops):
                for waits, fn, inc in ops:
                    for s, v in waits:
                        eng.wait_ge(s, v)
                    if fn is not None:
                        ins = fn(eng)
                        if inc is not None:
                            ins.then_inc(inc[0], inc[1])

            deco(body)


class Ring:
    def __init__(self, bufs):
        self.bufs = bufs
        self.i = 0

    def next(self):
        b = self.bufs[self.i % len(self.bufs)]
        self.i += 1
        return b


class Ctx:
    pass


def _sb(nc, es, name, shape, dt):
    return es.enter_context(nc.sbuf_tensor(name, shape, dt))


def _ps(nc, es, name, shape, dt):
    return es.enter_context(nc.psum_tensor(name, shape, dt))


def _breg(e, cache):
    if "r" not in cache:
        cache["r"] = e.to_reg(NE * CAP - 1)
    return cache["r"]


def _bcast_rows(handle, nrows, ncols, offset=0):
    return bass.AP(handle, offset, [[0, nrows], [1, ncols]])


def build_program(debug=False, upto=5, nexp=NE, p4mask=31, only=None):
    nc = bass.Bass("TRN2", target_bir_lowering=False)
    c = Ctx()
    c.nc = nc
    dk = "ExternalOutput" if debug else "Internal"

    def din(name, shape, dt=F32):
        return nc.dram_tensor(name, list(shape), dt, kind="ExternalInput")

    c.x_full = din("x_full", [SEQ, D])
    c.x_own = din("x_own", [OWN, D])
    c.w_in = din("w_in", [D, 2560])
    c.w_out = din("w_out", [D, D])
    c.w_router = din("w_router", [D, NE])
    ne_w = nexp if upto >= 4 else 1
    c.nexp = nexp
    c.p4mask = p4mask
    c.w_gu = din("w_gu", [ne_w, D, 2048])
    c.w_down = din("w_down", [ne_w, D, D])
    c.b_down = din("b_down", [NE, D])
    c.rows = din("rows", [8, D])
    c.colp = din("colp", [128, 64])
    c.w_rg = din("w_rg", [2, 8, 64, 64])
    c.lamv = din("lamv", [4, 64])
    c.b_router = din("b_router", [1, NE])
    c.bgu = din("bgu", [128, NE * 16])
    c.kbt = din("kbt", [128, 4 * 64])
    c.dq = din("dq", [128, 4 * 512])
    c.mfull = din("mfull", [128, 16 * 128])
    c.cst_bf = din("cst_bf", [128, 4 * 128], BF16)
    c.cst_f = din("cst_f", [128, NCF])
    c.out = nc.dram_tensor("out", [OWN, D], F32, kind="ExternalOutput")
    c.k_scr = nc.dram_tensor("k_scr", [4, 128, SEQ], BF16, kind=dk)
    c.v_scr = nc.dram_tensor("v_scr", [4, 128, NBLK, 128], BF16, kind=dk)
    c.q_scr = nc.dram_tensor("q_scr", [4, 128, 2, OWN], BF16, kind=dk)
    c.xg_scr = nc.dram_tensor("xg_scr", [NE * CAP, D], BF16, kind=dk)
    c.y_scr = nc.dram_tensor("y_scr", [NE * CAP, D], F32, kind=dk)
    c.x1_scr = nc.dram_tensor("x1_scr", [OWN, D], F32, kind=dk)
    c.dbg = nc.dram_tensor("dbg", [128, 4 * OWN * 2], BF16, kind=dk)
    c.dbg_gts = nc.dram_tensor("dbg_gts", [128, NOB * 4], F32, kind=dk)
    c.dbg_dst = nc.dram_tensor("dbg_dst", [128, NOB * 4], U32, kind=dk)

    with ExitStack() as es:
        SEM_STACK[0] = es
        c.cbf = _sb(nc, es, "cbf", [128, 4 * 128], BF16)
        c.cf = _sb(nc, es, "cf", [128, NCF], F32)
        c.colp_sb = _sb(nc, es, "colp_sb", [128, 64], F32)
        c.gts = _sb(nc, es, "gts", [128, NOB, 4], F32)
        c.dst = _sb(nc, es, "dst", [128, NOB, 4], U32)
        c.ident_bf = c.cbf[:, 0:128]
        c.ones_bf = c.cbf[:, 128:256]
        c.ltri_bf = c.cbf[:, 256:384]
        c.row0_bf = c.cbf[:, 384:512]
        c.ident_f = c.cf[:, 0:128]
        c.ones_f = c.cf[:, 128:256]
        c.ecap = c.cf[:, 256:256 + NE]
        c.sel = c.cf[:, 256 + NE:256 + NE + 4]
        c.epsc = c.cf[:, 292:293]
        c.onec = c.cf[:, 293:294]

        if only is not None:
            S0 = Sched(nc, es, "p0")
            S0.dma("sync", lambda e: e.dma_start(out=c.cbf[:], in_=c.cst_bf[:, :]), [], [T("a")])
            S0.dma("sync", lambda e: e.dma_start(out=c.cf[:], in_=c.cst_f[:, :]), [], [T("b")])
            with nc.Block() as blk:
                S0.emit(blk)
            {4: phase4, 5: phase5}[only](c)
            return nc
        with ExitStack() as es2:
            c.recT = _sb(nc, es2, "recT", [128, 4, OWN], BF16)
            phase1(c)
            c.attT = _sb(nc, es2, "attT", [128, 4, OWN], BF16)
            if upto >= 2:
                phase2(c)
            if upto >= 3:
                phase3(c)
            if debug:
                phase_dbg(c)
        if upto >= 4:
            phase4(c)
        if upto >= 5:
            phase5(c)
    return nc


def phase_dbg(c):
    nc = c.nc
    with ExitStack() as es:
        S = Sched(nc, es, "pd")
        td = T("dbg")
        S.dma("sync", lambda e: e.dma_start(out=c.dbg[:, 0:4 * OWN], in_=c.recT[:].rearrange("p a b -> p (a b)")), [], [td])
        S.dma("sync", lambda e: e.dma_start(out=c.dbg[:, 4 * OWN:8 * OWN], in_=c.attT[:].rearrange("p a b -> p (a b)")), [], [td])
        S.dma("sync", lambda e: e.dma_start(out=c.dbg_gts[:, :], in_=c.gts[:].rearrange("p a b -> p (a b)")), [], [td])
        S.dma("sync", lambda e: e.dma_start(out=c.dbg_dst[:, :], in_=c.dst[:].rearrange("p a b -> p (a b)")), [], [td])
        with nc.Block() as blk:
            S.emit(blk)


def phase1(c):
    nc = c.nc
    with ExitStack() as es:
        S = Sched(nc, es, "p1")
        sb = lambda n, s, d: _sb(nc, es, n, s, d)
        c.Tk_scr, c.Tv_scr, c.Tq_scr = T("k_scr"), T("v_scr"), T("q_scr")
        win = sb("win", [128, 8, 2560], BF16)
        Twin = [T(f"win{k}") for k in range(8)]
        wst = Ring([(sb(f"wst{i}", [128, 1280], F32), T(f"wst{i}")) for i in range(2)])
        xs = Ring([(sb(f"xs{i}", [128, D], F32), T(f"xs{i}")) for i in range(3)])
        stt = sb("stt", [128, 4, 12], F32); Tstt_l = [T(f"stt{i}") for i in range(4)]
        mv = sb("mv", [128, 4, 2], F32); Tmv_l = [T(f"mv{i}") for i in range(4)]
        rstd = sb("rstd", [128, 4], F32); Trstd_l = [T(f"rstd{i}") for i in range(4)]
        nmr = sb("nmr", [128, 4], F32); Tnmr_l = [T(f"nmr{i}") for i in range(4)]
        xn = sb("xn", [128, 4, D], BF16); Txn = [T(f"xn{b}") for b in range(4)]
        xT = sb("xT", [128, 8, 512], BF16); TxT = [T(f"xT{k}") for k in range(8)]
        kst = sb("kst", [128, 4, 512], BF16); Tkst = T("kst")
        vst = sb("vst", [128, 4, 512], BF16); Tvst = T("vst")
        qst = sb("qst", [128, 4, 2, 512], BF16); Tqst = T("qst")
        xlb = [(sb(f"xlb{i}", [128, 4, 515], F32), T(f"xlb{i}")) for i in range(2)]
        xc = sb("xc", [128, 4, 512], F32); Txc = T("xc")
        xcb = sb("xcb", [128, 4, 512], BF16); Txcb = T("xcb")
        rg = sb("rg", [128, 4, 512], F32); Trg = T("rg")
        ig = sb("ig", [128, 4, 512], F32); Tig = T("ig")
        aa = sb("aa", [128, 4, 512], F32); Taa = T("aa")
        sq = sb("sq", [128, 4, 512], F32); Tsq = T("sq")
        hb = [(sb(f"hb{i}", [128, 4, 512], F32), T(f"hb{i}")) for i in range(2)]
        hsel = sb("hsel", [128, 4, 128], F32); Thsel = T("hsel")
        ge = sb("ge", [128, 512], F32); Tge = T("ge")
        bdst = sb("bdst", [128, 2, 4, 128], F32); Tbdst = T("bdst")
        bd = sb("bd", [128, 2, 4, 128], BF16); Tbd = T("bd")
        lrp = sb("lrp", [128, 12], F32); Tlrp = T("lrp")
        Tcst = T("cst"); Tcolp = T("colp"); TrecT = T("recT")
        pst = [(_ps(nc, es, f"pst{i}", [128, 1024], BF16), T(f"pst{i}")) for i in range(2)]
        psr = Ring([(_ps(nc, es, f"psm{i}", [128, 512], F32), T(f"psm{i}")) for i in range(6)])
        colp = c.colp_sb

        S.dma("sync", lambda e: e.dma_start(out=c.cbf[:], in_=c.cst_bf[:, :]), [], [Tcst])
        S.dma("sync", lambda e: e.dma_start(out=c.cf[:], in_=c.cst_f[:, :]), [], [Tcst])
        S.dma("sync", lambda e: e.dma_start(out=colp[:], in_=c.colp[:, :]), [], [Tcolp])
        Tparts = [T(f"bdp{i}") for i in range(16)]
        S.op("pool", lambda e: e.memset(bdst[:].rearrange("p a b c -> p (a b c)"), 0.0), [], [Tbdst] + Tparts)
        for m in range(2):
            for ch in range(4):
                for u in range(2):
                    S.dma("sync", lambda e, m=m, ch=ch, u=u: e.dma_start(
                        out=bdst[u * 64:(u + 1) * 64, m, ch, u * 64:(u + 1) * 64],
                        in_=c.w_rg[m, 2 * ch + u, :, :]), [], [Tparts[m * 8 + ch * 2 + u]])
        S.op("act", lambda e: e.copy(out=bd[:].rearrange("p a b c -> p (a b c)"),
                                     in_=bdst[:].rearrange("p a b c -> p (a b c)")), [Tbdst] + Tparts, [Tbd])
        S.op("act", lambda e: e.activation(out=lrp[:, 8:12], in_=colp[:, 44:48], func=AF.Exp, scale=-1.0),
             [Tcolp], [Tlrp])
        S.op("act", lambda e: e.activation(out=lrp[:, 8:12], in_=lrp[:, 8:12], func=AF.Ln, bias=1.0),
             [Tlrp], [Tlrp])
        S.op("dve", lambda e: e.tensor_scalar(out=lrp[:, 0:4], in0=lrp[:, 8:12], scalar1=-8.0, scalar2=None,
                                              op0=ALU.mult), [Tlrp], [Tlrp])
        S.op("dve", lambda e: e.tensor_scalar(out=lrp[:, 4:8], in0=lrp[:, 8:12], scalar1=-16.0, scalar2=None,
                                              op0=ALU.mult), [Tlrp], [Tlrp])
        S.op("pool", lambda e: e.memset(qst[:].rearrange("p a b c -> p (a b c)"), 0.0), [], [Tqst])
        S.op("pool", lambda e: e.memset(xlb[0][0][:, :, 0:3], 0.0), [], [xlb[0][1]])
        cast_engs = ("act", "pool")
        for kc in range(8):
            for hf in range(2):
                st, Tst = wst.next()
                S.dma("sync", lambda e, st=st, kc=kc, hf=hf: e.dma_start(
                    out=st[:], in_=c.w_in[kc * 128:(kc + 1) * 128, hf * 1280:(hf + 1) * 1280]), [], [Tst])
                eng = cast_engs[(kc * 2 + hf) % 2]
                if eng == "act":
                    S.op("act", lambda e, st=st, kc=kc, hf=hf: e.copy(out=win[:, kc, hf * 1280:(hf + 1) * 1280], in_=st[:]),
                         [Tst], [Twin[kc]])
                else:
                    S.op("pool", lambda e, st=st, kc=kc, hf=hf: e.tensor_copy(out=win[:, kc, hf * 1280:(hf + 1) * 1280], in_=st[:]),
                         [Tst], [Twin[kc]])

        def mm_group(ps, Tps, lhs_fn, rhs_fn, reads_fn):
            for kc in range(8):
                S.op("pe", lambda e, kc=kc: e.matmul(ps, lhs_fn(kc), rhs_fn(kc), start=(kc == 0), stop=(kc == 7)),
                     reads_fn(kc), [Tps])

        def do_tile(it):
            own = it >= 16
            src = c.x_own if own else c.x_full
            t0 = (it - 16) * 512 if own else it * 512
            xsl = []
            for b in range(4):
                xt, Txt = xs.next()
                Tstt, Tmv, Trstd, Tnmr = Tstt_l[b], Tmv_l[b], Trstd_l[b], Tnmr_l[b]
                xsl.append((xt, Txt))
                S.dma("sync", lambda e, xt=xt, b=b: e.dma_start(out=xt[:], in_=src[t0 + b * 128:t0 + (b + 1) * 128, :]),
                      [], [Txt])
                for hh in range(2):
                    S.op("dve", lambda e, xt=xt, b=b, hh=hh: e.bn_stats(out=stt[:, b, hh * 6:(hh + 1) * 6],
                                                                     in_=xt[:, hh * 512:(hh + 1) * 512]), [Txt], [Tstt])
                S.op("dve", lambda e, b=b: e.bn_aggr(out=mv[:, b, :], in_=stt[:, b, :]), [Tstt], [Tmv])
                if b == 2 or b == 3:
                    pass
                S.op("act", lambda e, b=b: e.activation(out=rstd[:, b:b + 1], in_=mv[:, b, 1:2], func=AF.Sqrt,
                                                        bias=c.epsc[:, 0:1], scale=1.0), [Tmv], [Trstd])
                S.op("dve", lambda e, b=b: e.reciprocal(out=rstd[:, b:b + 1], in_=rstd[:, b:b + 1]), [Trstd], [Trstd])
                S.op("dve", lambda e, b=b: e.scalar_tensor_tensor(out=nmr[:, b:b + 1], in0=mv[:, b, 0:1], scalar=-1.0,
                                                                  in1=rstd[:, b:b + 1], op0=ALU.mult, op1=ALU.mult),
                     [Tmv, Trstd], [Tnmr])
                S.op("act", lambda e, xt=xt, b=b: e.activation(out=xn[:, b, :], in_=xt[:], func=AF.Identity,
                                                                scale=rstd[:, b:b + 1], bias=nmr[:, b:b + 1]),
                     [Txt, Trstd, Tnmr], [Txn[b]])
            for kc in range(8):
                pt, Tpt = pst[(kc // 2) % 2]
                off = (kc % 2) * 512
                for b in range(4):
                    S.op("pe", lambda e, pt=pt, off=off, b=b, kc=kc: e.transpose(
                        out=pt[:, off + b * 128:off + (b + 1) * 128], in_=xn[:, b, kc * 128:(kc + 1) * 128],
                        identity=c.ident_bf), [Txn[b], Tcst], [Tpt])
                S.op("act", lambda e, pt=pt, off=off, kc=kc: e.activation(
                    out=xT[:, kc, :], in_=pt[:, off:off + 512], func=AF.Identity,
                    scale=colp[:, kc:kc + 1], bias=colp[:, 8 + kc:9 + kc]), [Tpt, Tcolp], [TxT[kc]])
            if not own:
                for h in range(4):
                    ps, Tps = psr.next()
                    mm_group(ps[:], Tps, lambda kc, h=h: win[:, kc, 512 + h * 128:512 + (h + 1) * 128],
                             lambda kc: xT[:, kc, :], lambda kc: [Twin[kc], TxT[kc]])
                    S.op("dve" if h % 2 else "act",
                         (lambda e, ps=ps, h=h: e.tensor_copy(out=kst[:, h, :], in_=ps[:])) if h % 2 else
                         (lambda e, ps=ps, h=h: e.copy(out=kst[:, h, :], in_=ps[:])), [Tps], [Tkst])
                for h in range(4):
                    S.dma("sync", lambda e, h=h: e.dma_start(out=c.k_scr[h, :, t0:t0 + 512], in_=kst[:, h, :]),
                          [Tkst], [c.Tk_scr], sem_tile=c.Tk_scr)
                for b in range(4):
                    ps, Tps = psr.next()
                    mm_group(ps[:], Tps, lambda kc, b=b: xT[:, kc, b * 128:(b + 1) * 128],
                             lambda kc: win[:, kc, 1024:1536], lambda kc: [Twin[kc], TxT[kc]])
                    S.op("dve" if b % 2 else "act",
                         (lambda e, ps=ps, b=b: e.tensor_copy(out=vst[:, b, :], in_=ps[:])) if b % 2 else
                         (lambda e, ps=ps, b=b: e.copy(out=vst[:, b, :], in_=ps[:])), [Tps], [Tvst])
                for h in range(4):
                    S.dma("sync", lambda e, h=h: e.dma_start(out=c.v_scr[h, :, it * 4:(it + 1) * 4, :],
                                                              in_=vst[:, :, h * 128:(h + 1) * 128]),
                          [Tvst], [c.Tv_scr], sem_tile=c.Tv_scr)
                xl, Txl = xlb[it % 2]
                xl2, Txl2 = xlb[(it + 1) % 2]
                hcur, Thcur = hb[it % 2]
                hprev, Thprev = hb[(it + 1) % 2]
                for ch in range(4):
                    ps, Tps = psr.next()
                    mm_group(ps[:], Tps, lambda kc, ch=ch: win[:, kc, 1536 + ch * 128:1536 + (ch + 1) * 128],
                             lambda kc: xT[:, kc, :], lambda kc: [Twin[kc], TxT[kc]])
                    S.op("dve", lambda e, ps=ps, ch=ch, xl=xl: e.tensor_copy(out=xl[:, ch, 3:515], in_=ps[:]), [Tps], [Txl])
                S.op("pool", lambda e, xl=xl, xl2=xl2: e.tensor_copy(out=xl2[:, :, 0:3], in_=xl[:, :, 512:515]),
                     [Txl], [Txl2])
                for ch in range(4):
                    S.op("dve", lambda e, ch=ch, xl=xl: e.tensor_scalar(
                        out=xc[:, ch, :], in0=xl[:, ch, 3:515], scalar1=colp[:, 16 + ch * 4 + 3:16 + ch * 4 + 4],
                        scalar2=colp[:, 32 + ch:33 + ch], op0=ALU.mult, op1=ALU.add), [Txl, Tcolp], [Txc])
                    for w in (2, 1, 0):
                        S.op("dve", lambda e, ch=ch, w=w, xl=xl: e.scalar_tensor_tensor(
                            out=xc[:, ch, :], in0=xl[:, ch, w:w + 512], scalar=colp[:, 16 + ch * 4 + w:16 + ch * 4 + w + 1],
                            in1=xc[:, ch, :], op0=ALU.mult, op1=ALU.add), [Txl, Tcolp, Txc], [Txc])
                S.op("act", lambda e: e.copy(out=xcb[:].rearrange("p a b -> p (a b)"),
                                             in_=xc[:].rearrange("p a b -> p (a b)")), [Txc], [Txcb])
                for ch in range(4):
                    for m, (dstt, Tdst, bo) in enumerate(((rg, Trg, 36), (ig, Tig, 40))):
                        ps, Tps = psr.next()
                        S.op("pe", lambda e, ps=ps, m=m, ch=ch: e.matmul(ps[:], bd[:, m, ch, :], xcb[:, ch, :],
                                                                         start=True, stop=True), [Tbd, Txcb], [Tps])
                        S.op("act", lambda e, ps=ps, ch=ch, dstt=dstt, bo=bo: e.activation(
                            out=dstt[:, ch, :], in_=ps[:], func=AF.Sigmoid, bias=colp[:, bo + ch:bo + ch + 1], scale=1.0),
                            [Tps, Tcolp], [Tdst])
                for ch in range(4):
                    S.op("act", lambda e, ch=ch: e.activation(out=aa[:, ch, :], in_=rg[:, ch, :], func=AF.Exp,
                                                              scale=lrp[:, ch:ch + 1]), [Trg, Tlrp], [Taa])
                    S.op("act", lambda e, ch=ch: e.activation(out=sq[:, ch, :], in_=rg[:, ch, :], func=AF.Exp,
                                                              scale=lrp[:, 4 + ch:5 + ch]), [Trg, Tlrp], [Tsq])
                S.op("act", lambda e: e.activation(out=sq[:].rearrange("p a b -> p (a b)"),
                                                   in_=sq[:].rearrange("p a b -> p (a b)"), func=AF.Sqrt,
                                                   bias=c.onec[:, 0:1], scale=-1.0), [Tsq], [Tsq])
                S.op("pool", lambda e: e.tensor_tensor(out=ig[:].rearrange("p a b -> p (a b)"),
                                                       in0=ig[:].rearrange("p a b -> p (a b)"),
                                                       in1=xc[:].rearrange("p a b -> p (a b)"), op=ALU.mult),
                     [Tig, Txc], [Tig])
                S.op("dve", lambda e: e.tensor_tensor(out=ig[:].rearrange("p a b -> p (a b)"),
                                                      in0=ig[:].rearrange("p a b -> p (a b)"),
                                                      in1=sq[:].rearrange("p a b -> p (a b)"), op=ALU.mult),
                     [Tig, Tsq], [Tig])
                for ch in range(4):
                    init = 0.0 if it == 0 else hprev[:, ch, 511:512]
                    S.op("dve", lambda e, ch=ch, init=init, hcur=hcur: e.tensor_tensor_scan(
                        out=hcur[:, ch, :], data0=aa[:, ch, :], data1=ig[:, ch, :], initial=init,
                        op0=ALU.mult, op1=ALU.add), [Taa, Tig] + ([] if it == 0 else [Thprev]), [Thcur])
                S.op("dve", lambda e, hcur=hcur: e.tensor_scalar(out=hsel[:], in0=hcur[:, :, 0:128], scalar1=c.sel[:, 0:1],
                                                                 scalar2=None, op0=ALU.mult), [Thcur, Tcst], [Thsel])
                for t in (1, 2):
                    S.op("dve", lambda e, t=t, hcur=hcur: e.scalar_tensor_tensor(
                        out=hsel[:], in0=hcur[:, :, t * 128:(t + 1) * 128], scalar=c.sel[:, t:t + 1], in1=hsel[:],
                        op0=ALU.mult, op1=ALU.add), [Thcur, Tcst, Thsel], [Thsel])
                S.op("dve", lambda e, hcur=hcur, it=it: e.scalar_tensor_tensor(
                    out=c.recT[:, :, it * 128:(it + 1) * 128], in0=hcur[:, :, 384:512], scalar=c.sel[:, 3:4], in1=hsel[:],
                    op0=ALU.mult, op1=ALU.add), [Thcur, Tcst, Thsel], [TrecT])
            else:
                ot = it - 16
                for h in range(4):
                    ps, Tps = psr.next()
                    mm_group(ps[:], Tps, lambda kc, h=h: win[:, kc, h * 128:(h + 1) * 128],
                             lambda kc: xT[:, kc, :], lambda kc: [Twin[kc], TxT[kc]])
                    S.op("act", lambda e, ps=ps, h=h: e.mul(out=qst[0:64, h, 0, :], in_=ps[0:64, :], mul=0.125), [Tps], [Tqst])
                    S.op("dve", lambda e, ps=ps, h=h: e.tensor_scalar(out=qst[64:128, h, 1, :], in0=ps[64:128, :], scalar1=0.125,
                                                                      scalar2=None, op0=ALU.mult), [Tps], [Tqst])
                for h in range(4):
                    S.dma("sync", lambda e, h=h, ot=ot: e.dma_start(out=c.q_scr[h, :, :, ot * 512:(ot + 1) * 512],
                                                                     in_=qst[:, h, :, :]), [Tqst], [c.Tq_scr], sem_tile=c.Tq_scr)
                for ch in range(4):
                    ps, Tps = psr.next()
                    mm_group(ps[:], Tps, lambda kc, ch=ch: win[:, kc, 2048 + ch * 128:2048 + (ch + 1) * 128],
                             lambda kc: xT[:, kc, :], lambda kc: [Twin[kc], TxT[kc]])
                    S.op("act", lambda e, ps=ps: e.activation(out=ge[:], in_=ps[:], func=AF.Gelu), [Tps], [Tge])
                    S.op("dve", lambda e, ch=ch, ot=ot: e.tensor_tensor(
                        out=c.recT[:, ch, ot * 512:(ot + 1) * 512], in0=c.recT[:, ch, ot * 512:(ot + 1) * 512], in1=ge[:],
                        op=ALU.mult), [Tge, TrecT], [TrecT])

        for it in range(20):
            do_tile(it)
        with nc.Block() as blk:
            S.emit(blk)


def phase2(c):
    nc = c.nc
    with ExitStack() as es:
        S = Sched(nc, es, "p2")
        sb = lambda n, s, d: _sb(nc, es, n, s, d)
        kbt = sb("kbt_s", [128, 4, 64], F32); dq = sb("dq_s", [128, 4, 512], F32); mf = sb("mf_s", [128, 4, 4, 128], F32)
        lamt = sb("lamt", [128, 4, 64], F32); lw = sb("lw", [128, 8], F32); junk = sb("junk2", [128, 64], F32)
        Tc = T("c2"); Tlam = T("lam"); Tlw = T("lw"); Tjunk = T("junk"); TattT = T("attT"); Tcst = T("cst")
        Kr = Ring([(sb(f"Kh{i}", [128, SEQ], BF16), T(f"Kh{i}")) for i in range(2)])
        Vr = Ring([(sb(f"Vh{i}", [128, NBLK, 128], BF16), T(f"Vh{i}")) for i in range(2)])
        Qr = Ring([(sb(f"Qh{i}", [128, 2, OWN], BF16), T(f"Qh{i}")) for i in range(2)])
        sbr = Ring([(sb(f"sbs{i}", [128, 512], F32), T(f"sbs{i}")) for i in range(4)])
        pbr = Ring([(sb(f"pb{i}", [128, 512], BF16), T(f"pb{i}")) for i in range(6)])
        rz = [(sb(f"rz{i}", [128, 512], F32), T(f"rz{i}")) for i in range(2)]
        oo = [(sb(f"oo{i}", [128, 512], F32), T(f"oo{i}")) for i in range(2)]
        osq = sb("osq", [128, 512], F32); Tosq = T("osq")
        rst = sb("rst", [128, 512], F32); Trst = T("rst")
        Sr = Ring([(_ps(nc, es, f"S{i}", [128, 512], F32), T(f"S{i}")) for i in range(4)])
        Ab = [(_ps(nc, es, f"A{i}", [128, 512], F32), T(f"A{i}")) for i in range(2)]
        Zb = [(_ps(nc, es, f"Z{i}", [128, 512], F32), T(f"Z{i}")) for i in range(2)]
        colp = c.colp_sb

        S.dma("sync", lambda e: e.dma_start(out=kbt[:].rearrange("p a b -> p (a b)"), in_=c.kbt[:, :]), [], [Tc])
        S.dma("sync", lambda e: e.dma_start(out=dq[:].rearrange("p a b -> p (a b)"), in_=c.dq[:, :]), [], [Tc])
        S.dma("sync", lambda e: e.dma_start(out=mf[:].rearrange("p a b c -> p (a b c)"), in_=c.mfull[:, :]), [], [Tc])
        S.dma("sync", lambda e: e.dma_start(out=lamt[:].rearrange("p a b -> p (a b)"), in_=_bcast_rows(c.lamv, 128, 256)),
              [], [Tlam])
        for i in range(2):
            S.op("dve", lambda e, i=i: e.scalar_tensor_tensor(out=junk[:], in0=lamt[:, 2 * i, :], scalar=1.0,
                                                             in1=lamt[:, 2 * i + 1, :], op0=ALU.mult, op1=ALU.mult,
                                                             accum_out=lw[:, i:i + 1]), [Tlam], [Tjunk, Tlw])
        S.op("act", lambda e: e.activation(out=lw[:, 2:4], in_=lw[:, 0:2], func=AF.Exp), [Tlw], [Tlw])
        S.op("dve", lambda e: e.tensor_tensor(out=lw[:, 4:5], in0=lw[:, 3:4], in1=lw[:, 2:3], op=ALU.subtract), [Tlw], [Tlw])
        S.op("dve", lambda e: e.tensor_scalar(out=lw[:, 5:6], in0=lw[:, 4:5], scalar1=-LAM_INIT, scalar2=None, op0=ALU.add),
             [Tlw], [Tlw])
        S.op("dve", lambda e: e.tensor_scalar(out=lw[:, 6:7], in0=colp[:, 48:49], scalar1=(1.0 - LAM_INIT), scalar2=None,
                                              op0=ALU.mult), [], [Tlw])

        def head(h):
            Kh, TK = Kr.next(); Vh, TV = Vr.next(); Qh, TQ = Qr.next()
            S.dma("sync", lambda e: e.dma_start(out=Kh[:], in_=c.k_scr[h, :, :]), [], [TK])
            S.dma("sync", lambda e: e.dma_start(out=Vh[:], in_=c.v_scr[h, :, :, :]), [], [TV])
            S.dma("sync", lambda e: e.dma_start(out=Qh[:], in_=c.q_scr[h, :, :, :]), [], [TQ])

            def qtile(g):
                nj = 16 * g + 16
                pend = {}
                j0 = max(0, 16 * g - 1 - int(math.ceil(1.0 / SLOPES[h])))

                def scores(j):
                    c0 = 0 if j < 16 * g else 128 * ((j - 16 * g) // 4)
                    n = j - 16 * g + 48
                    ps_l = []
                    for m in range(2):
                        ps, Tps = Sr.next()
                        S.op("pe", lambda e, ps=ps, m=m: e.matmul(ps[:, c0:512], Kh[:, j * 128:(j + 1) * 128],
                                                                   Qh[:, m, g * 512 + c0:(g + 1) * 512], start=True, stop=True),
                             [TK, TQ], [Tps])
                        sbt, Tsb = sbr.next()
                        if j >= 16 * g:
                            jj = (j - 16 * g) % 4
                            S.op("dve", lambda e, ps=ps, sbt=sbt: e.tensor_tensor(out=sbt[:, c0:c0 + 128], in0=ps[:, c0:c0 + 128],
                                                                                  in1=mf[:, h, jj, :], op=ALU.add), [Tps, Tc], [Tsb])
                            if c0 + 128 < 512:
                                S.op("dve", lambda e, ps=ps, sbt=sbt: e.scalar_tensor_tensor(
                                    out=sbt[:, c0 + 128:512], in0=ps[:, c0 + 128:512], scalar=kbt[:, h, n:n + 1],
                                    in1=dq[:, h, c0 + 128:512], op0=ALU.add, op1=ALU.add), [Tps, Tc], [Tsb])
                        else:
                            S.op("dve", lambda e, ps=ps, sbt=sbt: e.scalar_tensor_tensor(
                                out=sbt[:, :], in0=ps[:, :], scalar=kbt[:, h, n:n + 1], in1=dq[:, h, :],
                                op0=ALU.add, op1=ALU.add), [Tps, Tc], [Tsb])
                        pb, Tpb = pbr.next()
                        S.op("act", lambda e, sbt=sbt, pb=pb: e.activation(out=pb[:, c0:512], in_=sbt[:, c0:512], func=AF.Exp),
                             [Tsb], [Tpb])
                        ps_l.append((pb, Tpb))
                    pend[j] = (c0, ps_l)

                def av(j):
                    c0, ps_l = pend.pop(j)
                    for m in range(2):
                        pb, Tpb = ps_l[m]
                        S.op("pe", lambda e, pb=pb, m=m: e.matmul(Ab[m][0][:, c0:512], Vh[:, j, :], pb[:, c0:512],
                                                                   start=(j == j0), stop=(j == nj - 1)), [TV, Tpb], [Ab[m][1]])
                        S.op("pe", lambda e, pb=pb, m=m: e.matmul(Zb[m][0][:, c0:512], c.ones_bf, pb[:, c0:512],
                                                                   start=(j == j0), stop=(j == nj - 1)), [Tcst, Tpb], [Zb[m][1]])

                j0 = max(0, 16 * g - 1 - int(math.ceil(1.0 / SLOPES[h])))
                for st in range(j0, nj + 1):
                    if st < nj:
                        scores(st)
                    if st >= j0 + 1:
                        av(st - 1)
                for m in range(2):
                    S.op("dve", lambda e, m=m: e.reciprocal(out=rz[m][0][:], in_=Zb[m][0][:]), [Zb[m][1]], [rz[m][1]])
                    S.op("dve", lambda e, m=m: e.tensor_tensor(out=oo[m][0][:], in0=Ab[m][0][:], in1=rz[m][0][:], op=ALU.mult),
                         [Ab[m][1], rz[m][1]], [oo[m][1]])
                S.op("dve", lambda e: e.scalar_tensor_tensor(out=oo[0][0][:], in0=oo[1][0][:], scalar=lw[:, 5:6], in1=oo[0][0][:],
                                                             op0=ALU.mult, op1=ALU.add), [oo[0][1], oo[1][1], Tlw], [oo[0][1]])
                S.op("pool", lambda e: e.tensor_tensor(out=osq[:], in0=oo[0][0][:], in1=oo[0][0][:], op=ALU.mult), [oo[0][1]], [Tosq])
                ps, Tps = Sr.next()
                S.op("pe", lambda e, ps=ps: e.matmul(ps[:], c.ones_f, osq[:], start=True, stop=True), [Tosq, Tcst], [Tps])
                S.op("act", lambda e, ps=ps: e.activation(out=rst[:], in_=ps[:], func=AF.Sqrt, bias=c.epsc[:, 0:1],
                                                          scale=1.0 / 128.0), [Tps, Tcst], [Trst])
                S.op("dve", lambda e: e.reciprocal(out=rst[:], in_=rst[:]), [Trst], [Trst])
                S.op("dve", lambda e: e.scalar_tensor_tensor(out=c.attT[:, h, g * 512:(g + 1) * 512], in0=oo[0][0][:],
                                                             scalar=lw[:, 6:7], in1=rst[:], op0=ALU.mult, op1=ALU.mult),
                     [oo[0][1], Trst, Tlw], [TattT])

            for g in range(4):
                qtile(g)

        for h in range(4):
            head(h)
        with nc.Block() as blk:
            S.emit(blk)


class LNbufs:
    def __init__(self, nc, es, tag):
        self.stt = _sb(nc, es, f"ln_stt_{tag}", [128, 12], F32); self.Tstt = T("stt")
        self.mv = _sb(nc, es, f"ln_mv_{tag}", [128, 2], F32); self.Tmv = T("mv")
        self.rs = _sb(nc, es, f"ln_rs_{tag}", [128, 1], F32); self.Trs = T("rs")
        self.nm = _sb(nc, es, f"ln_nm_{tag}", [128, 1], F32); self.Tnm = T("nm")
        self.tmp = _sb(nc, es, f"ln_tmp_{tag}", [128, D], F32); self.Ttmp = T("tmp")


def ln_tm(c, S, B, src, Tsrc, dst, Tdst, grow, brow, Trows):
    for hh in range(2):
        S.op("dve", lambda e, hh=hh: e.bn_stats(out=B.stt[:, hh * 6:(hh + 1) * 6], in_=src[:, hh * 512:(hh + 1) * 512]),
             [Tsrc], [B.Tstt])
    S.op("dve", lambda e: e.bn_aggr(out=B.mv[:], in_=B.stt[:]), [B.Tstt], [B.Tmv])
    S.op("act", lambda e: e.activation(out=B.rs[:], in_=B.mv[:, 1:2], func=AF.Sqrt, bias=c.epsc[:, 0:1], scale=1.0),
         [B.Tmv], [B.Trs])
    S.op("dve", lambda e: e.reciprocal(out=B.rs[:], in_=B.rs[:]), [B.Trs], [B.Trs])
    S.op("dve", lambda e: e.scalar_tensor_tensor(out=B.nm[:], in0=B.mv[:, 0:1], scalar=-1.0, in1=B.rs[:],
                                                 op0=ALU.mult, op1=ALU.mult), [B.Tmv, B.Trs], [B.Tnm])
    S.op("act", lambda e: e.activation(out=B.tmp[:], in_=src[:], func=AF.Identity, scale=B.rs[:, 0:1], bias=B.nm[:, 0:1]),
         [Tsrc, B.Trs, B.Tnm], [B.Ttmp])
    S.op("dve", lambda e: e.tensor_tensor(out=B.tmp[:], in0=B.tmp[:], in1=grow, op=ALU.mult), [B.Ttmp, Trows], [B.Ttmp])
    S.op("pool", lambda e: e.tensor_tensor(out=dst[:], in0=B.tmp[:], in1=brow, op=ALU.add), [B.Ttmp, Trows], [Tdst])


def phase3(c):
    nc = c.nc
    with ExitStack() as es:
        S = Sched(nc, es, "p3")
        rc = {}
        sb = lambda n, s, d: _sb(nc, es, n, s, d)
        wo = sb("wo", [128, 8, D], BF16); Two = [T(f"wo{k}") for k in range(8)]
        wost = Ring([(sb(f"wost{i}", [128, D], F32), T(f"wost{i}")) for i in range(2)])
        wr = sb("wr", [128, 8, NE], F32); Twr = T("wr")
        rowsb = sb("rowsb", [128, 4, D], F32); Trows = T("rows")
        brt = sb("brt", [128, NE], F32); Tbrt = T("brt")
        msk = sb("msk", [128, NOB, NE], BF16); Tmsk = [T(f"msk{i}") for i in range(NOB)]
        xs = Ring([(sb(f"xs3_{i}", [128, D], F32), T(f"xs3_{i}")) for i in range(2)])
        x0_l = [(sb(f"x0_{i}", [128, D], F32), T(f"x0_{i}")) for i in range(2)]
        yy_l = [(sb(f"yy_{i}", [128, D], F32), T(f"yy_{i}")) for i in range(2)]
        x1r = Ring([(sb(f"x1_{i}", [128, D], F32), T(f"x1_{i}")) for i in range(2)])
        x1br = Ring([(sb(f"x1b_{i}", [128, D], BF16), T(f"x1b_{i}")) for i in range(2)])
        x1T_l = [(sb(f"x1T_{i}", [128, 8, 128], F32), T(f"x1T_{i}")) for i in range(2)]
        lg_l = [(sb(f"lg_{i}", [128, NE], F32), T(f"lg_{i}")) for i in range(2)]
        t8_l = [(sb(f"t8_{i}", [128, 8], F32), T(f"t8_{i}")) for i in range(2)]
        sm_l = [(sb(f"sm3_{i}", [128, 16], F32), T(f"sm3_{i}")) for i in range(2)]
        destf_l = [(sb(f"destf_{i}", [128, NE], F32), T(f"destf_{i}")) for i in range(2)]
        junk_l = [(sb(f"junk3_{i}", [128, NE], F32), T(f"junk3_{i}")) for i in range(2)]
        B0_l = [LNbufs(nc, es, f"a{i}") for i in range(2)]; B1_l = [LNbufs(nc, es, f"b{i}") for i in range(2)]
        mixr = Ring([(_ps(nc, es, f"mix{i}", [128, 512], F32), T(f"mix{i}")) for i in range(4)])
        tpf = [(_ps(nc, es, f"tpf{i}", [128, 512], F32), T(f"tpf{i}")) for i in range(2)]
        lgp = _ps(nc, es, "lgp", [128, 512], F32); Tlgp = T("lgp")
        posp = _ps(nc, es, "posp", [128, 512], F32); Tposp = T("posp")
        Tcst = T("cst"); Tatt = T("att"); Trec = T("rec"); Tgts = T("gts"); Tdst = T("dst")
        Txg = T("xg_scr"); Tx1s = T("x1_scr")

        for i in range(4):
            S.dma("sync", lambda e, i=i: e.dma_start(out=rowsb[:, i, :], in_=_bcast_rows(c.rows, 128, D, offset=i * D)),
                  [], [Trows])
        S.dma("sync", lambda e: e.dma_start(out=brt[:], in_=_bcast_rows(c.b_router, 128, NE)), [], [Tbrt])
        for kc in range(8):
            S.dma("sync", lambda e, kc=kc: e.dma_start(out=wr[:, kc, :], in_=c.w_router[kc * 128:(kc + 1) * 128, :]), [], [Twr])
            st, Tst = wost.next()
            S.dma("sync", lambda e, st=st, kc=kc: e.dma_start(out=st[:], in_=c.w_out[kc * 128:(kc + 1) * 128, :]), [], [Tst])
            if kc % 2:
                S.op("act", lambda e, st=st, kc=kc: e.copy(out=wo[:, kc, :], in_=st[:]), [Tst], [Two[kc]])
            else:
                S.op("pool", lambda e, st=st, kc=kc: e.tensor_copy(out=wo[:, kc, :], in_=st[:]), [Tst], [Two[kc]])

        def block(ob):
            cs = slice(ob * 128, (ob + 1) * 128)
            x0, Tx0 = x0_l[ob % 2]; yy, Tyy = yy_l[ob % 2]; x1T, Tx1T = x1T_l[ob % 2]; lg, Tlg = lg_l[ob % 2]
            t8, Tt8 = t8_l[ob % 2]; sm, Tsm = sm_l[ob % 2]; destf, Tdestf = destf_l[ob % 2]; junk, Tjunk = junk_l[ob % 2]
            B0 = B0_l[ob % 2]; B1 = B1_l[ob % 2]
            xt, Txt = xs.next()
            S.dma("sync", lambda e: e.dma_start(out=xt[:], in_=c.x_own[cs, :]), [], [Txt])
            ln_tm(c, S, B0, xt, Txt, x0, Tx0, rowsb[:, 0, :], rowsb[:, 1, :], Trows)
            for hf in range(2):
                ps, Tps = mixr.next()
                for kc in range(8):
                    lhs = c.attT[:, kc, cs] if kc < 4 else c.recT[:, kc - 4, cs]
                    S.op("pe", lambda e, ps=ps, lhs=lhs, kc=kc, hf=hf: e.matmul(ps[:], lhs, wo[:, kc, hf * 512:(hf + 1) * 512],
                                                                              start=(kc == 0), stop=(kc == 7)),
                         [Two[kc], Tatt, Trec], [Tps])
                S.op("dve", lambda e, ps=ps, hf=hf: e.scalar_tensor_tensor(
                    out=yy[:, hf * 512:(hf + 1) * 512], in0=x0[:, hf * 512:(hf + 1) * 512], scalar=float(ALPHA), in1=ps[:],
                    op0=ALU.mult, op1=ALU.add), [Tx0, Tps], [Tyy])
            x1, Tx1 = x1r.next()
            ln_tm(c, S, B1, yy, Tyy, x1, Tx1, rowsb[:, 2, :], rowsb[:, 3, :], Trows)
            S.dma("sync", lambda e: e.dma_start(out=c.x1_scr[cs, :], in_=x1[:]), [Tx1], [Tx1s], sem_tile=Tx1s)
            x1b, Tx1b = x1br.next()
            S.op("act", lambda e: e.copy(out=x1b[:], in_=x1[:]), [Tx1], [Tx1b])
            for kc in range(8):
                pt, Tpt = tpf[kc // 4]
                S.op("pe", lambda e, pt=pt, kc=kc: e.transpose(out=pt[:, (kc % 4) * 128:(kc % 4 + 1) * 128],
                                                               in_=x1[:, kc * 128:(kc + 1) * 128], identity=c.ident_f),
                     [Tx1, Tcst], [Tpt])
            S.op("act", lambda e: e.copy(out=x1T[:, 0:4, :].rearrange("p a b -> p (a b)"), in_=tpf[0][0][:]), [tpf[0][1]], [Tx1T])
            S.op("dve", lambda e: e.tensor_copy(out=x1T[:, 4:8, :].rearrange("p a b -> p (a b)"), in_=tpf[1][0][:]),
                 [tpf[1][1]], [Tx1T])
            for kc in range(8):
                S.op("pe", lambda e, kc=kc: e.matmul(lgp[:, 0:NE], x1T[:, kc, :], wr[:, kc, :], start=(kc == 0), stop=(kc == 7)),
                     [Tx1T, Twr], [Tlgp])
            S.op("dve", lambda e: e.tensor_tensor(out=lg[:], in0=lgp[:, 0:NE], in1=brt[:], op=ALU.add), [Tlgp, Tbrt], [Tlg])
            S.op("dve", lambda e: e.max(out=t8[:], in_=lg[:]), [Tlg], [Tt8])
            S.op("dve", lambda e: e.tensor_scalar(out=sm[:, 0:1], in0=t8[:, 0:1], scalar1=-1.0, scalar2=None, op0=ALU.mult),
                 [Tt8], [Tsm])
            S.op("act", lambda e: e.activation(out=sm[:, 4:8], in_=t8[:, 0:4], func=AF.Exp, bias=sm[:, 0:1], scale=1.0,
                                               accum_out=sm[:, 1:2]), [Tt8, Tsm], [Tsm])
            S.op("dve", lambda e: e.reciprocal(out=sm[:, 1:2], in_=sm[:, 1:2]), [Tsm], [Tsm])
            S.op("dve", lambda e: e.tensor_scalar(out=c.gts[:, ob, :], in0=sm[:, 4:8], scalar1=sm[:, 1:2], scalar2=None,
                                                  op0=ALU.mult), [Tsm], [Tgts])
            S.op("dve", lambda e: e.tensor_scalar(out=msk[:, ob, :], in0=lg[:], scalar1=t8[:, 3:4], scalar2=None, op0=ALU.is_ge),
                 [Tlg, Tt8], [Tmsk[ob]])
            for o2 in range(ob + 1):
                lhs = c.ltri_bf if o2 == ob else c.ones_bf
                S.op("pe", lambda e, lhs=lhs, o2=o2: e.matmul(posp[:, 0:NE], lhs, msk[:, o2, :], start=(o2 == 0), stop=(o2 == ob)),
                     [Tmsk[o2], Tcst], [Tposp])
            S.op("dve", lambda e: e.tensor_tensor(out=destf[:], in0=posp[:, 0:NE], in1=c.ecap, op=ALU.add), [Tposp, Tcst], [Tdestf])
            for k in range(4):
                S.op("dve", lambda e, k=k: e.scalar_tensor_tensor(out=junk[:], in0=lg[:], scalar=t8[:, k:k + 1], in1=destf[:],
                                                                 op0=ALU.is_equal, op1=ALU.mult, accum_out=sm[:, 8 + k:9 + k]),
                     [Tlg, Tt8, Tdestf], [Tjunk, Tsm])
            S.op("dve", lambda e: e.tensor_copy(out=c.dst[:, ob, :], in_=sm[:, 8:12]), [Tsm], [Tdst])
            for k in range(4):
                S.dma("pool", lambda e, k=k: e.indirect_dma_start(
                    out=c.xg_scr[:, :], out_offset=bass.IndirectOffsetOnAxis(ap=c.dst[:, ob, k:k + 1], axis=0),
                    in_=x1b[:, :], in_offset=None, bounds_check=_breg(e, rc), oob_is_err=False), [Tx1b, Tdst], [Txg], sem_tile=Txg)

        for ob in range(NOB):
            block(ob)
        with nc.Block() as blk:
            S.emit(blk)


def phase4(c):
    nc = c.nc
    with ExitStack() as es:
        S = Sched(nc, es, "p4")
        sb = lambda n, s, d: _sb(nc, es, n, s, d)
        wg = [(sb(f"wg{i}", [128, 8, 2048], BF16), [T(f"wg{i}_{k}") for k in range(8)]) for i in range(2)]
        wd = [(sb(f"wd{i}", [128, 8, D], BF16), [T(f"wd{i}_{k}") for k in range(8)]) for i in range(2)]
        stg = Ring([(sb(f"stg{i}", [128, 2048], F32), T(f"stg{i}")) for i in range(4)])
        xgt_l = [(sb(f"xgt{i}", [128, 3, D], BF16), T(f"xgt{i}")) for i in range(2)]
        xgT = [(sb(f"xgT{i}", [128, 8, CAP], BF16), T(f"xgT{i}")) for i in range(2)]
        actT = [(sb(f"actT{i}", [128, 8, CAP], BF16), T(f"actT{i}")) for i in range(2)]
        glu = Ring([(sb(f"glu{i}", [128, CAP], F32), T(f"glu{i}")) for i in range(3)])
        sg = Ring([(sb(f"sg{i}", [128, CAP], F32), T(f"sg{i}")) for i in range(3)])
        lin = Ring([(sb(f"lin{i}", [128, CAP], F32), T(f"lin{i}")) for i in range(3)])
        ysb = Ring([(sb(f"ysb{i}", [128, D], F32), T(f"ysb{i}")) for i in range(2)])
        bdf_l = [(sb(f"bdf{i}", [1, D], F32), T(f"bdf{i}")) for i in range(2)]
        bdb_l = [(sb(f"bdb{i}", [128, D], BF16), T(f"bdb{i}")) for i in range(2)]
        bg = sb("bg", [128, NE * 16], F32); Tbg = T("bg")
        bl1 = sb("bl1", [128, NE * 16], F32); Tbl1 = T("bl1")
        tpl = [_ps(nc, es, f"tp4_{i}", [128, 1024], BF16) for i in range(2)]; Ttp = [T("tp4a"), T("tp4b")]
        psg = Ring([(_ps(nc, es, f"psg{i}", [128, 512], F32), T(f"psg{i}")) for i in range(2)])
        psl = Ring([(_ps(nc, es, f"psl{i}", [128, 512], F32), T(f"psl{i}")) for i in range(2)])
        psy = Ring([(_ps(nc, es, f"psy{i}", [128, 512], F32), T(f"psy{i}")) for i in range(2)])
        Tcst = T("cst"); Tys = T("y_scr")
        S.dma("sync", lambda e: e.dma_start(out=bg[:], in_=c.bgu[:, :]), [], [Tbg])
        for bdb_, Tbdb_ in bdb_l:
            S.op("pool", lambda e, bdb_=bdb_: e.memset(bdb_[:], 0.0), [], [Tbdb_])
        S.op("pool", lambda e: e.tensor_scalar(out=bl1[:], in0=bg[:], scalar1=1.0, scalar2=None, op0=ALU.add), [Tbg], [Tbl1])
        cast_cycle = ["act", "dve", "act", "act", "dve", "act", "act", "dve"]
        cc = [0]

        def cast(out, in_, reads, writes):
            eng = cast_cycle[cc[0] % len(cast_cycle)]
            cc[0] += 1
            if eng == "act":
                S.op("act", lambda e: e.copy(out=out, in_=in_), reads, writes)
            else:
                S.op(eng, lambda e: e.tensor_copy(out=out, in_=in_), reads, writes)

        def weight_chunks(e_):
            wgt, Twg = wg[e_ % 2]
            wdt, Twd = wd[e_ % 2]
            out = []

            def gu(kc):
                st, Tst = stg.next()
                S.dma("sync", lambda e: e.dma_start(out=st[:], in_=c.w_gu[e_, kc * 128:(kc + 1) * 128, :]), [], [Tst])
                v = st[:].rearrange("p (f two) -> p two f", two=2)
                cast(wgt[:, kc, 0:1024], v[:, 0, :], [Tst], [Twg[kc]])
                cast(wgt[:, kc, 1024:2048], v[:, 1, :], [Tst], [Twg[kc]])

            def dn(kc):
                st, Tst = stg.next()
                S.dma("sync", lambda e: e.dma_start(
                    out=st[:].rearrange("p (a d) -> p a d", a=2),
                    in_=c.w_down[e_, kc * 128:(kc + 2) * 128, :].rearrange("(a p) d -> p a d", p=128)), [], [Tst])
                cast(wdt[:, kc, :], st[:, 0:D], [Tst], [Twd[kc]])
                cast(wdt[:, kc + 1, :], st[:, D:2 * D], [Tst], [Twd[kc + 1]])

            for kc in range(8):
                out.append(lambda kc=kc: gu(kc))
            for kc in range(0, 8, 2):
                out.append(lambda kc=kc: dn(kc))
            return out

        def load_acts(e_):
            xgt, Txgt = xgt_l[e_ % 2]
            bdf, Tbdf = bdf_l[e_ % 2]
            bdb, Tbdb = bdb_l[e_ % 2]
            S.dma("sync", lambda e: e.dma_start(out=xgt[:], in_=c.xg_scr[e_ * CAP:(e_ + 1) * CAP, :].rearrange("(s p) d -> p s d", p=128)),
                  [], [Txgt])
            S.dma("sync", lambda e: e.dma_start(out=bdf[:], in_=c.b_down[e_:e_ + 1, :]), [], [Tbdf])
            S.op("act", lambda e: e.copy(out=bdb[0:1, :], in_=bdf[:]), [Tbdf], [Tbdb])

        def load_weights(e_):
            for f in weight_chunks(e_):
                f()

        hooks = []

        def step():
            if hooks:
                hooks.pop(0)()

        def expert(e_):
            wgt, Twg = wg[e_ % 2]
            wdt, Twd = wd[e_ % 2]
            xT_, TxT_ = xgT[e_ % 2]
            aT, TaT = actT[e_ % 2]
            xgt, Txgt = xgt_l[e_ % 2]
            bdb, Tbdb = bdb_l[e_ % 2]
            for kc in range(8 if c.p4mask & 2 else 0):
                hf = kc % 2
                for sc in range(3):
                    S.op("pe", lambda e, kc=kc, sc=sc, hf=hf: e.transpose(
                        out=tpl[hf][:, sc * 128:(sc + 1) * 128], in_=xgt[:, sc, kc * 128:(kc + 1) * 128],
                        identity=c.ident_bf), [Txgt, Tcst], [Ttp[hf]])
                if kc % 2:
                    S.op("act", lambda e, kc=kc, hf=hf: e.copy(out=xT_[:, kc, :], in_=tpl[hf][:, 0:CAP]), [Ttp[hf]], [TxT_])
                else:
                    S.op("dve", lambda e, kc=kc, hf=hf: e.tensor_copy(out=xT_[:, kc, :], in_=tpl[hf][:, 0:CAP]),
                         [Ttp[hf]], [TxT_])
            for fc in range(8 if c.p4mask & 4 else 0):
                pg, Tpg = psg.next()
                pl, Tpl = psl.next()
                for kc in range(8):
                    S.op("pe", lambda e, pg=pg, kc=kc, fc=fc: e.matmul(pg[:, 0:CAP], wgt[:, kc, fc * 128:(fc + 1) * 128], xT_[:, kc, :],
                                                                     start=(kc == 0), stop=(kc == 7)), [Twg[kc], TxT_], [Tpg])
                for kc in range(8):
                    S.op("pe", lambda e, pl=pl, kc=kc, fc=fc: e.matmul(pl[:, 0:CAP], wgt[:, kc, 1024 + fc * 128:1024 + (fc + 1) * 128],
                                                                     xT_[:, kc, :], start=(kc == 0), stop=(kc == 7)),
                         [Twg[kc], TxT_], [Tpl])
                gl_, Tgl = glu.next(); sg_, Tsg = sg.next(); ln_, Tln = lin.next()
                col = e_ * 16 + fc
                S.op("dve", lambda e, pg=pg, gl_=gl_, col=col: e.tensor_scalar(out=gl_[:], in0=pg[:, 0:CAP], scalar1=bg[:, col:col + 1],
                                                                              scalar2=7.0, op0=ALU.add, op1=ALU.min), [Tpg, Tbg], [Tgl])
                S.op("act", lambda e, gl_=gl_, sg_=sg_: e.activation(out=sg_[:], in_=gl_[:], func=AF.Sigmoid, scale=1.702), [Tgl], [Tsg])
                S.op("dve", lambda e, pl=pl, ln_=ln_, col=col: e.tensor_scalar(out=ln_[:], in0=pl[:, 0:CAP], scalar1=bl1[:, col + 8:col + 9],
                                                                              scalar2=-6.0, op0=ALU.add, op1=ALU.max), [Tpl, Tbl1], [Tln])
                S.op("dve", lambda e, gl_=gl_, sg_=sg_: e.tensor_tensor(out=sg_[:], in0=gl_[:], in1=sg_[:], op=ALU.mult), [Tgl, Tsg], [Tsg])
                S.op("dve", lambda e, ln_=ln_, sg_=sg_, fc=fc: e.scalar_tensor_tensor(out=aT[:, fc, :], in0=ln_[:], scalar=8.0, in1=sg_[:],
                                                                                     op0=ALU.min, op1=ALU.mult), [Tln, Tsg], [TaT])
                step()
            for sc in range(3 if c.p4mask & 16 else 0):
                yt, Tyt = ysb.next()
                for hf in range(2):
                    py, Tpy = psy.next()
                    for fc in range(8):
                        S.op("pe", lambda e, py=py, fc=fc, hf=hf, sc=sc: e.matmul(py[:], aT[:, fc, sc * 128:(sc + 1) * 128],
                                                                                wdt[:, fc, hf * 512:(hf + 1) * 512],
                                                                                start=(fc == 0), stop=False), [TaT, Twd[fc]], [Tpy])
                    S.op("pe", lambda e, py=py, hf=hf: e.matmul(py[:], c.row0_bf, bdb[:, hf * 512:(hf + 1) * 512],
                                                               start=False, stop=True), [Tcst, Tbdb], [Tpy])
                    if hf:
                        S.op("act", lambda e, py=py, yt=yt, hf=hf: e.copy(out=yt[:, hf * 512:(hf + 1) * 512], in_=py[:]), [Tpy], [Tyt])
                    else:
                        S.op("dve", lambda e, py=py, yt=yt, hf=hf: e.tensor_copy(out=yt[:, hf * 512:(hf + 1) * 512], in_=py[:]), [Tpy], [Tyt])
                S.dma("sync", lambda e, yt=yt, sc=sc: e.dma_start(out=c.y_scr[e_ * CAP + sc * 128:e_ * CAP + (sc + 1) * 128, :], in_=yt[:]),
                      [Tyt], [Tys], sem_tile=Tys)
                step()
            while hooks:
                step()

        load_acts(0)
        load_weights(0)
        for e_ in range(c.nexp):
            if e_ + 1 < c.nexp:
                load_acts(e_ + 1)
                hooks.extend(weight_chunks(e_ + 1))
                step()
            expert(e_)
        with nc.Block() as blk:
            S.emit(blk)


def phase5(c):
    nc = c.nc
    with ExitStack() as es:
        S = Sched(nc, es, "p5")
        rc = {}
        sb = lambda n, s, d: _sb(nc, es, n, s, d)
        rowsb = sb("rows5", [128, 2, D], F32); Trows = T("rows5")
        yk = [Ring([(sb(f"yk{k}_{i}", [128, D], F32), T(f"yk{k}_{i}")) for i in range(2)]) for k in range(4)]
        x1r = Ring([(sb(f"x15_{i}", [128, D], F32), T(f"x15_{i}")) for i in range(2)])
        acc_l = [(sb(f"acc5_{i}", [128, D], F32), T(f"acc5_{i}")) for i in range(2)]
        outr = Ring([(sb(f"o5_{i}", [128, D], F32), T(f"o5_{i}")) for i in range(2)])
        B_l = [LNbufs(nc, es, f"c{i}") for i in range(2)]
        Tgts = T("gts"); Tdst = T("dst"); Tout = T("out")
        for i in range(2):
            S.dma("sync", lambda e, i=i: e.dma_start(out=rowsb[:, i, :], in_=_bcast_rows(c.rows, 128, D, offset=(4 + i) * D)),
                  [], [Trows])

        def block(ob):
            cs = slice(ob * 128, (ob + 1) * 128)
            acc, Tacc = acc_l[ob % 2]; B = B_l[ob % 2]
            ys = []
            for k in range(4):
                yt, Tyt = yk[k].next()
                ys.append((yt, Tyt))
                S.dma("pool", lambda e, yt=yt, k=k: e.indirect_dma_start(
                    out=yt[:, :], out_offset=None, in_=c.y_scr[:, :],
                    in_offset=bass.IndirectOffsetOnAxis(ap=c.dst[:, ob, k:k + 1], axis=0),
                    bounds_check=_breg(e, rc), oob_is_err=False), [Tdst], [Tyt])
            x1, Tx1 = x1r.next()
            S.dma("sync", lambda e: e.dma_start(out=x1[:], in_=c.x1_scr[cs, :]), [], [Tx1])
            S.op("dve", lambda e: e.tensor_scalar(out=acc[:], in0=ys[0][0][:], scalar1=c.gts[:, ob, 0:1], scalar2=None, op0=ALU.mult),
                 [ys[0][1], Tgts], [Tacc])
            for k in range(1, 4):
                S.op("dve", lambda e, k=k: e.scalar_tensor_tensor(out=acc[:], in0=ys[k][0][:], scalar=c.gts[:, ob, k:k + 1], in1=acc[:],
                                                                 op0=ALU.mult, op1=ALU.add), [ys[k][1], Tgts, Tacc], [Tacc])
            S.op("dve", lambda e: e.scalar_tensor_tensor(out=acc[:], in0=x1[:], scalar=float(ALPHA), in1=acc[:],
                                                         op0=ALU.mult, op1=ALU.add), [Tx1, Tacc], [Tacc])
            ot, Tot = outr.next()
            ln_tm(c, S, B, acc, Tacc, ot, Tot, rowsb[:, 0, :], rowsb[:, 1, :], Trows)
            S.dma("sync", lambda e: e.dma_start(out=c.out[cs, :], in_=ot[:]), [Tot], [Tout], sem_tile=Tout)

        for ob in range(NOB):
            block(ob)
        with nc.Block() as blk:
            S.emit(blk)


def _bf(a):
    return np.ascontiguousarray(a).astype(ml_dtypes.bfloat16)


def make_core_consts(r):
    f = np.float32
    p = np.arange(128, dtype=np.float64)
    kbt = np.zeros((128, 4, 64), f)
    dq = np.zeros((128, 4, 512), f)
    mfull = np.zeros((128, 4, 4, 128), f)
    q = np.arange(512)
    for h, sl in enumerate(SLOPES):
        for n in range(64):
            kbt[:, h, n] = sl * (128.0 * (n - 48 - r) + p)
        dq[:, h, :] = (-sl * (512.0 * (q // 128) + (q % 128)))[None, :]
        for jj in range(4):
            kpos = 128 * jj + np.arange(128)[:, None]
            qpos = 128 * r + np.arange(128)[None, :]
            allowed = (kpos // 64) <= (qpos // 64)
            mfull[:, h, jj, :] = np.where(allowed, -sl * np.abs(qpos - kpos), NEG)
    ident = np.eye(128, dtype=f)
    ones = np.ones((128, 128), f)
    ltri = (np.arange(128)[:, None] < np.arange(128)[None, :]).astype(f)
    row0 = np.zeros((128, 128), f)
    row0[0, :] = 1.0
    cst_bf = _bf(np.concatenate([ident, ones, ltri, row0], axis=1))
    cst_f = np.zeros((128, NCF), f)
    cst_f[:, 0:128] = ident
    cst_f[:, 128:256] = 1.0
    cst_f[:, 256:256 + NE] = (np.arange(NE) * CAP)[None, :]
    cst_f[:, 256 + NE + r] = 1.0
    cst_f[:, 292] = LN_EPS
    cst_f[:, 293] = 1.0
    cst_f[:, 294] = -0.5
    return {"kbt": kbt.reshape(128, -1), "dq": dq.reshape(128, -1), "mfull": mfull.reshape(128, -1),
            "cst_bf": cst_bf, "cst_f": cst_f}


def make_in_maps(inp, ne_w=NE):
    f = np.float32
    g = lambda k: np.asarray(inp[k], dtype=f)
    x = g("x")
    colp = np.zeros((128, 64), f)
    chunk = lambda v, n: np.asarray(v, f).reshape(n, 128).T
    colp[:, 0:8] = chunk(g("ln0_g"), 8)
    colp[:, 8:16] = chunk(g("ln0_b"), 8)
    cw = g("conv_w")[0]
    for ch in range(4):
        for w in range(4):
            colp[:, 16 + ch * 4 + w] = cw[w, ch * 128:(ch + 1) * 128]
    colp[:, 32:36] = chunk(g("conv_b")[0], 4)
    colp[:, 36:40] = chunk(g("b_rg_a")[0].reshape(512), 4)
    colp[:, 40:44] = chunk(g("b_rg_x")[0].reshape(512), 4)
    colp[:, 44:48] = chunk(g("lru_lambda")[0], 4)
    colp[:, 48] = g("subln_g")[0]
    rows = np.zeros((8, D), f)
    rows[0], rows[1] = g("ln0_g"), g("ln0_b")
    rows[2], rows[3] = g("ln1_g")[0], g("ln1_b")[0]
    rows[4], rows[5] = g("ln2_g")[0], g("ln2_b")[0]
    bgu = g("b_gu")[0]
    bg = bgu[:, 0::2].reshape(NE, 8, 128)
    bl = bgu[:, 1::2].reshape(NE, 8, 128)
    bgu_l = np.concatenate([bg, bl], axis=1).transpose(2, 0, 1).reshape(128, NE * 16)
    shared = {
        "w_in": np.ascontiguousarray(g("w_in")[0]), "w_out": np.ascontiguousarray(g("w_out")[0]),
        "w_router": np.ascontiguousarray(g("w_router")[0]), "w_gu": np.ascontiguousarray(g("w_gu")[0][:ne_w]),
        "w_down": np.ascontiguousarray(g("w_down")[0][:ne_w]), "b_down": np.ascontiguousarray(g("b_down")[0]),
        "rows": rows, "colp": colp,
        "w_rg": np.ascontiguousarray(np.stack([g("w_rg_a")[0], g("w_rg_x")[0]])),
        "lamv": np.ascontiguousarray(np.stack([g("lam_q1")[0], g("lam_k1")[0], g("lam_q2")[0], g("lam_k2")[0]])),
        "b_router": np.ascontiguousarray(g("b_router")), "bgu": np.ascontiguousarray(bgu_l),
    }
    maps = []
    for core in range(NCORES):
        b, r = core // 4, core % 4
        m = dict(shared)
        m["x_full"] = np.ascontiguousarray(x[b])
        m["x_own"] = np.ascontiguousarray(x[b].reshape(NOB, 4, 128, D)[:, r].reshape(OWN, D))
        m.update(make_core_consts(r))
        maps.append(m)
    return maps


def assemble(results):
    out = np.zeros((2, SEQ, D), np.float32)
    for core in range(NCORES):
        b, r = core // 4, core % 4
        o = np.asarray(results[core]["out"], np.float32).reshape(NOB, 128, D)
        out[b].reshape(NOB, 4, 128, D)[:, r] = o
    return out


_NC_CACHE = {}


def kernel(**inputs):
    if "nc" not in _NC_CACHE:
        _NC_CACHE["nc"] = build_program()
    nc = _NC_CACHE["nc"]
    maps = make_in_maps(inputs)
    res = run_bass_kernel_spmd(nc, maps, core_ids=list(range(NCORES)))
    return assemble(res.results)
```

```python
import math
from contextlib import ExitStack

import numpy as np
import ml_dtypes
import concourse.bass as bass
import concourse.mybir as mybir
from concourse.bass_utils import run_bass_kernel_spmd

F32 = mybir.dt.float32
BF16 = mybir.dt.bfloat16
U32 = mybir.dt.uint32
AF = mybir.ActivationFunctionType
ALU = mybir.AluOpType

NCORES = 8
D = 1024
SEQ = 8192
NBLK = SEQ // 128
OWN = 2048
NOB = OWN // 128
NE = 32
CAP = 384
DEPTH = 1
ALPHA = (2.0 * DEPTH) ** 0.25
LN_EPS = 1e-5
LAM_INIT = 0.8 - 0.6 * math.exp(-0.3 * 0)
SLOPES = [2.0 ** (-8.0 * (i + 1) / 4) for i in range(4)]
NEG = -30000.0
NCF = 128 + 128 + NE + 4 + 4


class T:
    __slots__ = ("name", "w", "r", "dsem", "dcnt")

    def __init__(self, name):
        self.name = name
        self.w = None
        self.r = {}
        self.dsem = None
        self.dcnt = 0


ENGS = ("sync", "pe", "act", "dve", "pool")


SEM_STACK = [None]


class Sched:
    def __init__(self, nc, es, tag):
        self.nc = nc
        self.es = SEM_STACK[0]
        self.tag = tag
        self.q = {e: [] for e in ENGS}
        self.sem = {}
        self.cnt = {}
        for e in ("pe", "act", "dve", "pool"):
            self.sem[e] = self.es.enter_context(nc.semaphore(f"{tag}_{e}"))
            self.cnt[e] = 0
        self.seen = {e: {} for e in ENGS}
        self.semobj = {e: self.sem[e] for e in self.sem}
        self.dma_tiles = []
        self.nsem = 4

    def _waits(self, eng, reads, writes, strict=False):
        deps = {}

        def add(m, same_ok):
            if m is None:
                return
            k, v = m
            if k == eng and not (same_ok or strict):
                return
            if deps.get(k, 0) < v:
                deps[k] = v

        for t in reads:
            add(t.w, eng != "pe")
        for t in writes:
            add(t.w, False)
            for k, v in t.r.items():
                add((k, v), False)
        out = []
        for k, v in deps.items():
            if self.seen[eng].get(k, 0) >= v:
                continue
            self.seen[eng][k] = v
            out.append((self.semobj[k], v))
        return out

    def _mark(self, mark, reads, writes):
        k, v = mark
        for t in reads:
            if t.r.get(k, 0) < v:
                t.r[k] = v
        for t in writes:
            t.w = mark
            t.r = {}

    def op(self, eng, fn, reads=(), writes=()):
        waits = self._waits(eng, reads, writes)
        self.cnt[eng] += 1
        mark = (eng, self.cnt[eng])
        self.q[eng].append((waits, fn, (self.sem[eng], 1)))
        self._mark(mark, reads, writes)

    def dma(self, queue, fn, reads, writes, sem_tile=None):
        st = sem_tile if sem_tile is not None else writes[0]
        if st.dsem is None:
            st.dsem = self.es.enter_context(self.nc.semaphore(f"{self.tag}_d{self.nsem}"))
            self.nsem += 1
            self.semobj[id(st)] = st.dsem
            self.dma_tiles.append(st)
        waits = self._waits(queue, reads, writes, strict=True)
        st.dcnt += 16
        mark = (id(st), st.dcnt)
        self.q[queue].append((waits, fn, (st.dsem, 16)))
        if queue == "pool":
            pass
        self._mark(mark, reads, writes)

    def emit(self, block):
        final = [(t.dsem, t.dcnt) for t in self.dma_tiles]
        self.q["sync"].append((final, None, None))
        table = (("sync", block.sync), ("pe", block.tensor), ("act", block.scalar),
                 ("dve", block.vector), ("pool", block.gpsimd))
        for name, deco in table:
            ops = self.q[name]

            def body(eng, ops=ops):
                for waits, fn, inc in ops:
                    for s, v in waits:
                        eng.wait_ge(s, v)
                    if fn is not None:
                        ins = fn(eng)
                        if inc is not None:
                            ins.then_inc(inc[0], inc[1])

            deco(body)


class Ring:
    def __init__(self, bufs):
        self.bufs = bufs
        self.i = 0

    def next(self):
        b = self.bufs[self.i % len(self.bufs)]
        self.i += 1
        return b


class Ctx:
    pass


def _sb(nc, es, name, shape, dt):
    return es.enter_context(nc.sbuf_tensor(name, shape, dt))


def _ps(nc, es, name, shape, dt):
    return es.enter_context(nc.psum_tensor(name, shape, dt))


def _breg(e, cache):
    if "r" not in cache:
        cache["r"] = e.to_reg(NE * CAP - 1)
    return cache["r"]


def _bcast_rows(handle, nrows, ncols, offset=0):
    return bass.AP(handle, offset, [[0, nrows], [1, ncols]])


def build_program(debug=False, upto=5, nexp=NE, p4mask=31, only=None):
    nc = bass.Bass("TRN2", target_bir_lowering=False)
    c = Ctx()
    c.nc = nc
    dk = "ExternalOutput" if debug else "Internal"

    def din(name, shape, dt=F32):
        return nc.dram_tensor(name, list(shape), dt, kind="ExternalInput")

    c.x_full = din("x_full", [SEQ, D])
    c.x_own = din("x_own", [OWN, D])
    c.w_in = din("w_in", [D, 2560])
    c.w_out = din("w_out", [D, D])
    c.w_router = din("w_router", [D, NE])
    ne_w = nexp if upto >= 4 else 1
    c.nexp = nexp
    c.p4mask = p4mask
    c.w_gu = din("w_gu", [ne_w, D, 2048])
    c.w_down = din("w_down", [ne_w, D, D])
    c.b_down = din("b_down", [NE, D])
    c.rows = din("rows", [8, D])
    c.colp = din("colp", [128, 64])
    c.w_rg = din("w_rg", [2, 8, 64, 64])
    c.lamv = din("lamv", [4, 64])
    c.b_router = din("b_router", [1, NE])
    c.bgu = din("bgu", [128, NE * 16])
    c.kbt = din("kbt", [128, 4 * 64])
    c.dq = din("dq", [128, 4 * 512])
    c.mfull = din("mfull", [128, 16 * 128])
    c.cst_bf = din("cst_bf", [128, 4 * 128], BF16)
    c.cst_f = din("cst_f", [128, NCF])
    c.out = nc.dram_tensor("out", [OWN, D], F32, kind="ExternalOutput")
    c.k_scr = nc.dram_tensor("k_scr", [4, 128, SEQ], BF16, kind=dk)
    c.v_scr = nc.dram_tensor("v_scr", [4, 128, NBLK, 128], BF16, kind=dk)
    c.q_scr = nc.dram_tensor("q_scr", [4, 128, 2, OWN], BF16, kind=dk)
    c.xg_scr = nc.dram_tensor("xg_scr", [NE * CAP, D], BF16, kind=dk)
    c.y_scr = nc.dram_tensor("y_scr", [NE * CAP, D], F32, kind=dk)
    c.x1_scr = nc.dram_tensor("x1_scr", [OWN, D], F32, kind=dk)
    c.dbg = nc.dram_tensor("dbg", [128, 4 * OWN * 2], BF16, kind=dk)
    c.dbg_gts = nc.dram_tensor("dbg_gts", [128, NOB * 4], F32, kind=dk)
    c.dbg_dst = nc.dram_tensor("dbg_dst", [128, NOB * 4], U32, kind=dk)

    with ExitStack() as es:
        SEM_STACK[0] = es
        c.cbf = _sb(nc, es, "cbf", [128, 4 * 128], BF16)
        c.cf = _sb(nc, es, "cf", [128, NCF], F32)
        c.colp_sb = _sb(nc, es, "colp_sb", [128, 64], F32)
        c.gts = _sb(nc, es, "gts", [128, NOB, 4], F32)
        c.dst = _sb(nc, es, "dst", [128, NOB, 4], U32)
        c.ident_bf = c.cbf[:, 0:128]
        c.ones_bf = c.cbf[:, 128:256]
        c.ltri_bf = c.cbf[:, 256:384]
        c.row0_bf = c.cbf[:, 384:512]
        c.ident_f = c.cf[:, 0:128]
        c.ones_f = c.cf[:, 128:256]
        c.ecap = c.cf[:, 256:256 + NE]
        c.sel = c.cf[:, 256 + NE:256 + NE + 4]
        c.epsc = c.cf[:, 292:293]
        c.onec = c.cf[:, 293:294]

        if only is not None:
            S0 = Sched(nc, es, "p0")
            S0.dma("sync", lambda e: e.dma_start(out=c.cbf[:], in_=c.cst_bf[:, :]), [], [T("a")])
            S0.dma("sync", lambda e: e.dma_start(out=c.cf[:], in_=c.cst_f[:, :]), [], [T("b")])
            with nc.Block() as blk:
                S0.emit(blk)
            {4: phase4, 5: phase5}[only](c)
            return nc
        with ExitStack() as es2:
            c.recT = _sb(nc, es2, "recT", [128, 4, OWN], BF16)
            phase1(c)
            c.attT = _sb(nc, es2, "attT", [128, 4, OWN], BF16)
            if upto >= 2:
                phase2(c)
            if upto >= 3:
                phase3(c)
            if debug:
                phase_dbg(c)
        if upto >= 4:
            phase4(c)
        if upto >= 5:
            phase5(c)
    return nc


def phase_dbg(c):
    nc = c.nc
    with ExitStack() as es:
        S = Sched(nc, es, "pd")
        td = T("dbg")
        S.dma("sync", lambda e: e.dma_start(out=c.dbg[:, 0:4 * OWN], in_=c.recT[:].rearrange("p a b -> p (a b)")), [], [td])
        S.dma("sync", lambda e: e.dma_start(out=c.dbg[:, 4 * OWN:8 * OWN], in_=c.attT[:].rearrange("p a b -> p (a b)")), [], [td])
        S.dma("sync", lambda e: e.dma_start(out=c.dbg_gts[:, :], in_=c.gts[:].rearrange("p a b -> p (a b)")), [], [td])
        S.dma("sync", lambda e: e.dma_start(out=c.dbg_dst[:, :], in_=c.dst[:].rearrange("p a b -> p (a b)")), [], [td])
        with nc.Block() as blk:
            S.emit(blk)


def phase1(c):
    nc = c.nc
    with ExitStack() as es:
        S = Sched(nc, es, "p1")
        sb = lambda n, s, d: _sb(nc, es, n, s, d)
        c.Tk_scr, c.Tv_scr, c.Tq_scr = T("k_scr"), T("v_scr"), T("q_scr")
        win = sb("win", [128, 8, 2560], BF16)
        Twin = [T(f"win{k}") for k in range(8)]
        wst = Ring([(sb(f"wst{i}", [128, 1280], F32), T(f"wst{i}")) for i in range(2)])
        xs = Ring([(sb(f"xs{i}", [128, D], F32), T(f"xs{i}")) for i in range(3)])
        stt = sb("stt", [128, 4, 12], F32); Tstt_l = [T(f"stt{i}") for i in range(4)]
        mv = sb("mv", [128, 4, 2], F32); Tmv_l = [T(f"mv{i}") for i in range(4)]
        rstd = sb("rstd", [128, 4], F32); Trstd_l = [T(f"rstd{i}") for i in range(4)]
        nmr = sb("nmr", [128, 4], F32); Tnmr_l = [T(f"nmr{i}") for i in range(4)]
        xn = sb("xn", [128, 4, D], BF16); Txn = [T(f"xn{b}") for b in range(4)]
        xT = sb("xT", [128, 8, 512], BF16); TxT = [T(f"xT{k}") for k in range(8)]
        kst = sb("kst", [128, 4, 512], BF16); Tkst = T("kst")
        vst = sb("vst", [128, 4, 512], BF16); Tvst = T("vst")
        qst = sb("qst", [128, 4, 2, 512], BF16); Tqst = T("qst")
        xlb = [(sb(f"xlb{i}", [128, 4, 515], F32), T(f"xlb{i}")) for i in range(2)]
        xc = sb("xc", [128, 4, 512], F32); Txc = T("xc")
        xcb = sb("xcb", [128, 4, 512], BF16); Txcb = T("xcb")
        rg = sb("rg", [128, 4, 512], F32); Trg = T("rg")
        ig = sb("ig", [128, 4, 512], F32); Tig = T("ig")
        aa = sb("aa", [128, 4, 512], F32); Taa = T("aa")
        sq = sb("sq", [128, 4, 512], F32); Tsq = T("sq")
        hb = [(sb(f"hb{i}", [128, 4, 512], F32), T(f"hb{i}")) for i in range(2)]
        hsel = sb("hsel", [128, 4, 128], F32); Thsel = T("hsel")
        ge = sb("ge", [128, 512], F32); Tge = T("ge")
        bdst = sb("bdst", [128, 2, 4, 128], F32); Tbdst = T("bdst")
        bd = sb("bd", [128, 2, 4, 128], BF16); Tbd = T("bd")
        lrp = sb("lrp", [128, 12], F32); Tlrp = T("lrp")
        Tcst = T("cst"); Tcolp = T("colp"); TrecT = T("recT")
        pst = [(_ps(nc, es, f"pst{i}", [128, 1024], BF16), T(f"pst{i}")) for i in range(2)]
        psr = Ring([(_ps(nc, es, f"psm{i}", [128, 512], F32), T(f"psm{i}")) for i in range(6)])
        colp = c.colp_sb

        S.dma("sync", lambda e: e.dma_start(out=c.cbf[:], in_=c.cst_bf[:, :]), [], [Tcst])
        S.dma("sync", lambda e: e.dma_start(out=c.cf[:], in_=c.cst_f[:, :]), [], [Tcst])
        S.dma("sync", lambda e: e.dma_start(out=colp[:], in_=c.colp[:, :]), [], [Tcolp])
        Tparts = [T(f"bdp{i}") for i in range(16)]
        S.op("pool", lambda e: e.memset(bdst[:].rearrange("p a b c -> p (a b c)"), 0.0), [], [Tbdst] + Tparts)
        for m in range(2):
            for ch in range(4):
                for u in range(2):
                    S.dma("sync", lambda e, m=m, ch=ch, u=u: e.dma_start(
                        out=bdst[u * 64:(u + 1) * 64, m, ch, u * 64:(u + 1) * 64],
                        in_=c.w_rg[m, 2 * ch + u, :, :]), [], [Tparts[m * 8 + ch * 2 + u]])
        S.op("act", lambda e: e.copy(out=bd[:].rearrange("p a b c -> p (a b c)"),
                                     in_=bdst[:].rearrange("p a b c -> p (a b c)")), [Tbdst] + Tparts, [Tbd])
        S.op("act", lambda e: e.activation(out=lrp[:, 8:12], in_=colp[:, 44:48], func=AF.Exp, scale=-1.0),
             [Tcolp], [Tlrp])
        S.op("act", lambda e: e.activation(out=lrp[:, 8:12], in_=lrp[:, 8:12], func=AF.Ln, bias=1.0),
             [Tlrp], [Tlrp])
        S.op("dve", lambda e: e.tensor_scalar(out=lrp[:, 0:4], in0=lrp[:, 8:12], scalar1=-8.0, scalar2=None,
                                              op0=ALU.mult), [Tlrp], [Tlrp])
        S.op("dve", lambda e: e.tensor_scalar(out=lrp[:, 4:8], in0=lrp[:, 8:12], scalar1=-16.0, scalar2=None,
                                              op0=ALU.mult), [Tlrp], [Tlrp])
        S.op("pool", lambda e: e.memset(qst[:].rearrange("p a b c -> p (a b c)"), 0.0), [], [Tqst])
        S.op("pool", lambda e: e.memset(xlb[0][0][:, :, 0:3], 0.0), [], [xlb[0][1]])
        cast_engs = ("act", "pool")
        for kc in range(8):
            for hf in range(2):
                st, Tst = wst.next()
                S.dma("sync", lambda e, st=st, kc=kc, hf=hf: e.dma_start(
                    out=st[:], in_=c.w_in[kc * 128:(kc + 1) * 128, hf * 1280:(hf + 1) * 1280]), [], [Tst])
                eng = cast_engs[(kc * 2 + hf) % 2]
                if eng == "act":
                    S.op("act", lambda e, st=st, kc=kc, hf=hf: e.copy(out=win[:, kc, hf * 1280:(hf + 1) * 1280], in_=st[:]),
                         [Tst], [Twin[kc]])
                else:
                    S.op("pool", lambda e, st=st, kc=kc, hf=hf: e.tensor_copy(out=win[:, kc, hf * 1280:(hf + 1) * 1280], in_=st[:]),
                         [Tst], [Twin[kc]])

        def mm_group(ps, Tps, lhs_fn, rhs_fn, reads_fn):
            for kc in range(8):
                S.op("pe", lambda e, kc=kc: e.matmul(ps, lhs_fn(kc), rhs_fn(kc), start=(kc == 0), stop=(kc == 7)),
                     reads_fn(kc), [Tps])

        def do_tile(it):
            own = it >= 16
            src = c.x_own if own else c.x_full
            t0 = (it - 16) * 512 if own else it * 512
            xsl = []
            for b in range(4):
                xt, Txt = xs.next()
                Tstt, Tmv, Trstd, Tnmr = Tstt_l[b], Tmv_l[b], Trstd_l[b], Tnmr_l[b]
                xsl.append((xt, Txt))
                S.dma("sync", lambda e, xt=xt, b=b: e.dma_start(out=xt[:], in_=src[t0 + b * 128:t0 + (b + 1) * 128, :]),
                      [], [Txt])
                for hh in range(2):
                    S.op("dve", lambda e, xt=xt, b=b, hh=hh: e.bn_stats(out=stt[:, b, hh * 6:(hh + 1) * 6],
                                                                     in_=xt[:, hh * 512:(hh + 1) * 512]), [Txt], [Tstt])
                S.op("dve", lambda e, b=b: e.bn_aggr(out=mv[:, b, :], in_=stt[:, b, :]), [Tstt], [Tmv])
                if b == 2 or b == 3:
                    pass
                S.op("act", lambda e, b=b: e.activation(out=rstd[:, b:b + 1], in_=mv[:, b, 1:2], func=AF.Sqrt,
                                                        bias=c.epsc[:, 0:1], scale=1.0), [Tmv], [Trstd])
                S.op("dve", lambda e, b=b: e.reciprocal(out=rstd[:, b:b + 1], in_=rstd[:, b:b + 1]), [Trstd], [Trstd])
                S.op("dve", lambda e, b=b: e.scalar_tensor_tensor(out=nmr[:, b:b + 1], in0=mv[:, b, 0:1], scalar=-1.0,
                                                                  in1=rstd[:, b:b + 1], op0=ALU.mult, op1=ALU.mult),
                     [Tmv, Trstd], [Tnmr])
                S.op("act", lambda e, xt=xt, b=b: e.activation(out=xn[:, b, :], in_=xt[:], func=AF.Identity,
                                                                scale=rstd[:, b:b + 1], bias=nmr[:, b:b + 1]),
                     [Txt, Trstd, Tnmr], [Txn[b]])
            for kc in range(8):
                pt, Tpt = pst[(kc // 2) % 2]
                off = (kc % 2) * 512
                for b in range(4):
                    S.op("pe", lambda e, pt=pt, off=off, b=b, kc=kc: e.transpose(
                        out=pt[:, off + b * 128:off + (b + 1) * 128], in_=xn[:, b, kc * 128:(kc + 1) * 128],
                        identity=c.ident_bf), [Txn[b], Tcst], [Tpt])
                S.op("act", lambda e, pt=pt, off=off, kc=kc: e.activation(
                    out=xT[:, kc, :], in_=pt[:, off:off + 512], func=AF.Identity,
                    scale=colp[:, kc:kc + 1], bias=colp[:, 8 + kc:9 + kc]), [Tpt, Tcolp], [TxT[kc]])
            if not own:
                for h in range(4):
                    ps, Tps = psr.next()
                    mm_group(ps[:], Tps, lambda kc, h=h: win[:, kc, 512 + h * 128:512 + (h + 1) * 128],
                             lambda kc: xT[:, kc, :], lambda kc: [Twin[kc], TxT[kc]])
                    S.op("dve" if h % 2 else "act",
                         (lambda e, ps=ps, h=h: e.tensor_copy(out=kst[:, h, :], in_=ps[:])) if h % 2 else
                         (lambda e, ps=ps, h=h: e.copy(out=kst[:, h, :], in_=ps[:])), [Tps], [Tkst])
                for h in range(4):
                    S.dma("pool", lambda e, h=h: e.dma_start(out=c.k_scr[h, :, t0:t0 + 512], in_=kst[:, h, :]),
                          [Tkst], [c.Tk_scr], sem_tile=c.Tk_scr)
                for b in range(4):
                    ps, Tps = psr.next()
                    mm_group(ps[:], Tps, lambda kc, b=b: xT[:, kc, b * 128:(b + 1) * 128],
                             lambda kc: win[:, kc, 1024:1536], lambda kc: [Twin[kc], TxT[kc]])
                    S.op("dve" if b % 2 else "act",
                         (lambda e, ps=ps, b=b: e.tensor_copy(out=vst[:, b, :], in_=ps[:])) if b % 2 else
                         (lambda e, ps=ps, b=b: e.copy(out=vst[:, b, :], in_=ps[:])), [Tps], [Tvst])
                for h in range(4):
                    S.dma("pool", lambda e, h=h: e.dma_start(out=c.v_scr[h, :, it * 4:(it + 1) * 4, :],
                                                              in_=vst[:, :, h * 128:(h + 1) * 128]),
                          [Tvst], [c.Tv_scr], sem_tile=c.Tv_scr)
                xl, Txl = xlb[it % 2]
                xl2, Txl2 = xlb[(it + 1) % 2]
                hcur, Thcur = hb[it % 2]
                hprev, Thprev = hb[(it + 1) % 2]
                for ch in range(4):
                    ps, Tps = psr.next()
                    mm_group(ps[:], Tps, lambda kc, ch=ch: win[:, kc, 1536 + ch * 128:1536 + (ch + 1) * 128],
                             lambda kc: xT[:, kc, :], lambda kc: [Twin[kc], TxT[kc]])
                    S.op("dve", lambda e, ps=ps, ch=ch, xl=xl: e.tensor_copy(out=xl[:, ch, 3:515], in_=ps[:]), [Tps], [Txl])
                S.op("pool", lambda e, xl=xl, xl2=xl2: e.tensor_copy(out=xl2[:, :, 0:3], in_=xl[:, :, 512:515]),
                     [Txl], [Txl2])
                for ch in range(4):
                    S.op("dve", lambda e, ch=ch, xl=xl: e.tensor_scalar(
                        out=xc[:, ch, :], in0=xl[:, ch, 3:515], scalar1=colp[:, 16 + ch * 4 + 3:16 + ch * 4 + 4],
                        scalar2=colp[:, 32 + ch:33 + ch], op0=ALU.mult, op1=ALU.add), [Txl, Tcolp], [Txc])
                    for w in (2, 1, 0):
                        S.op("dve", lambda e, ch=ch, w=w, xl=xl: e.scalar_tensor_tensor(
                            out=xc[:, ch, :], in0=xl[:, ch, w:w + 512], scalar=colp[:, 16 + ch * 4 + w:16 + ch * 4 + w + 1],
                            in1=xc[:, ch, :], op0=ALU.mult, op1=ALU.add), [Txl, Tcolp, Txc], [Txc])
                S.op("act", lambda e: e.copy(out=xcb[:].rearrange("p a b -> p (a b)"),
                                             in_=xc[:].rearrange("p a b -> p (a b)")), [Txc], [Txcb])
                for ch in range(4):
                    for m, (dstt, Tdst, bo) in enumerate(((rg, Trg, 36), (ig, Tig, 40))):
                        ps, Tps = psr.next()
                        S.op("pe", lambda e, ps=ps, m=m, ch=ch: e.matmul(ps[:], bd[:, m, ch, :], xcb[:, ch, :],
                                                                         start=True, stop=True), [Tbd, Txcb], [Tps])
                        S.op("act", lambda e, ps=ps, ch=ch, dstt=dstt, bo=bo: e.activation(
                            out=dstt[:, ch, :], in_=ps[:], func=AF.Sigmoid, bias=colp[:, bo + ch:bo + ch + 1], scale=1.0),
                            [Tps, Tcolp], [Tdst])
                for ch in range(4):
                    S.op("act", lambda e, ch=ch: e.activation(out=aa[:, ch, :], in_=rg[:, ch, :], func=AF.Exp,
                                                              scale=lrp[:, ch:ch + 1]), [Trg, Tlrp], [Taa])
                    S.op("act", lambda e, ch=ch: e.activation(out=sq[:, ch, :], in_=rg[:, ch, :], func=AF.Exp,
                                                              scale=lrp[:, 4 + ch:5 + ch]), [Trg, Tlrp], [Tsq])
                S.op("act", lambda e: e.activation(out=sq[:].rearrange("p a b -> p (a b)"),
                                                   in_=sq[:].rearrange("p a b -> p (a b)"), func=AF.Sqrt,
                                                   bias=c.onec[:, 0:1], scale=-1.0), [Tsq], [Tsq])
                S.op("pool", lambda e: e.tensor_tensor(out=ig[:].rearrange("p a b -> p (a b)"),
                                                       in0=ig[:].rearrange("p a b -> p (a b)"),
                                                       in1=xc[:].rearrange("p a b -> p (a b)"), op=ALU.mult),
                     [Tig, Txc], [Tig])
                S.op("dve", lambda e: e.tensor_tensor(out=ig[:].rearrange("p a b -> p (a b)"),
                                                      in0=ig[:].rearrange("p a b -> p (a b)"),
                                                      in1=sq[:].rearrange("p a b -> p (a b)"), op=ALU.mult),
                     [Tig, Tsq], [Tig])
                for ch in range(4):
                    init = 0.0 if it == 0 else hprev[:, ch, 511:512]
                    S.op("dve", lambda e, ch=ch, init=init, hcur=hcur: e.tensor_tensor_scan(
                        out=hcur[:, ch, :], data0=aa[:, ch, :], data1=ig[:, ch, :], initial=init,
                        op0=ALU.mult, op1=ALU.add), [Taa, Tig] + ([] if it == 0 else [Thprev]), [Thcur])
                S.op("dve", lambda e, hcur=hcur: e.tensor_scalar(out=hsel[:], in0=hcur[:, :, 0:128], scalar1=c.sel[:, 0:1],
                                                                 scalar2=None, op0=ALU.mult), [Thcur, Tcst], [Thsel])
                for t in (1, 2):
                    S.op("dve", lambda e, t=t, hcur=hcur: e.scalar_tensor_tensor(
                        out=hsel[:], in0=hcur[:, :, t * 128:(t + 1) * 128], scalar=c.sel[:, t:t + 1], in1=hsel[:],
                        op0=ALU.mult, op1=ALU.add), [Thcur, Tcst, Thsel], [Thsel])
                S.op("dve", lambda e, hcur=hcur, it=it: e.scalar_tensor_tensor(
                    out=c.recT[:, :, it * 128:(it + 1) * 128], in0=hcur[:, :, 384:512], scalar=c.sel[:, 3:4], in1=hsel[:],
                    op0=ALU.mult, op1=ALU.add), [Thcur, Tcst, Thsel], [TrecT])
            else:
                ot = it - 16
                for h in range(4):
                    ps, Tps = psr.next()
                    mm_group(ps[:], Tps, lambda kc, h=h: win[:, kc, h * 128:(h + 1) * 128],
                             lambda kc: xT[:, kc, :], lambda kc: [Twin[kc], TxT[kc]])
                    S.op("act", lambda e, ps=ps, h=h: e.mul(out=qst[0:64, h, 0, :], in_=ps[0:64, :], mul=0.125), [Tps], [Tqst])
                    S.op("dve", lambda e, ps=ps, h=h: e.tensor_scalar(out=qst[64:128, h, 1, :], in0=ps[64:128, :], scalar1=0.125,
                                                                      scalar2=None, op0=ALU.mult), [Tps], [Tqst])
                for h in range(4):
                    S.dma("pool", lambda e, h=h, ot=ot: e.dma_start(out=c.q_scr[h, :, :, ot * 512:(ot + 1) * 512],
                                                                     in_=qst[:, h, :, :]), [Tqst], [c.Tq_scr], sem_tile=c.Tq_scr)
                for ch in range(4):
                    ps, Tps = psr.next()
                    mm_group(ps[:], Tps, lambda kc, ch=ch: win[:, kc, 2048 + ch * 128:2048 + (ch + 1) * 128],
                             lambda kc: xT[:, kc, :], lambda kc: [Twin[kc], TxT[kc]])
                    S.op("act", lambda e, ps=ps: e.activation(out=ge[:], in_=ps[:], func=AF.Gelu), [Tps], [Tge])
                    S.op("dve", lambda e, ch=ch, ot=ot: e.tensor_tensor(
                        out=c.recT[:, ch, ot * 512:(ot + 1) * 512], in0=c.recT[:, ch, ot * 512:(ot + 1) * 512], in1=ge[:],
                        op=ALU.mult), [Tge, TrecT], [TrecT])

        for it in range(20):
            do_tile(it)
        with nc.Block() as blk:
            S.emit(blk)


def phase2(c):
    nc = c.nc
    with ExitStack() as es:
        S = Sched(nc, es, "p2")
        sb = lambda n, s, d: _sb(nc, es, n, s, d)
        kbt = sb("kbt_s", [128, 4, 64], F32); dq = sb("dq_s", [128, 4, 512], F32); mf = sb("mf_s", [128, 4, 4, 128], F32)
        lamt = sb("lamt", [128, 4, 64], F32); lw = sb("lw", [128, 8], F32); junk = sb("junk2", [128, 64], F32)
        Tc = T("c2"); Tlam = T("lam"); Tlw = T("lw"); Tjunk = T("junk"); TattT = T("attT"); Tcst = T("cst")
        Kr = Ring([(sb(f"Kh{i}", [128, SEQ], BF16), T(f"Kh{i}")) for i in range(2)])
        Vr = Ring([(sb(f"Vh{i}", [128, NBLK, 128], BF16), T(f"Vh{i}")) for i in range(2)])
        Qr = Ring([(sb(f"Qh{i}", [128, 2, OWN], BF16), T(f"Qh{i}")) for i in range(2)])
        sbr = Ring([(sb(f"sbs{i}", [128, 512], F32), T(f"sbs{i}")) for i in range(4)])
        pbr = Ring([(sb(f"pb{i}", [128, 512], BF16), T(f"pb{i}")) for i in range(6)])
        rz = [(sb(f"rz{i}", [128, 512], F32), T(f"rz{i}")) for i in range(2)]
        oo = [(sb(f"oo{i}", [128, 512], F32), T(f"oo{i}")) for i in range(2)]
        osq = sb("osq", [128, 512], F32); Tosq = T("osq")
        rst = sb("rst", [128, 512], F32); Trst = T("rst")
        Sr = Ring([(_ps(nc, es, f"S{i}", [128, 512], F32), T(f"S{i}")) for i in range(4)])
        Ab = [(_ps(nc, es, f"A{i}", [128, 512], F32), T(f"A{i}")) for i in range(2)]
        Zb = [(_ps(nc, es, f"Z{i}", [128, 512], F32), T(f"Z{i}")) for i in range(2)]
        colp = c.colp_sb

        S.dma("sync", lambda e: e.dma_start(out=kbt[:].rearrange("p a b -> p (a b)"), in_=c.kbt[:, :]), [], [Tc])
        S.dma("sync", lambda e: e.dma_start(out=dq[:].rearrange("p a b -> p (a b)"), in_=c.dq[:, :]), [], [Tc])
        S.dma("sync", lambda e: e.dma_start(out=mf[:].rearrange("p a b c -> p (a b c)"), in_=c.mfull[:, :]), [], [Tc])
        S.dma("sync", lambda e: e.dma_start(out=lamt[:].rearrange("p a b -> p (a b)"), in_=_bcast_rows(c.lamv, 128, 256)),
              [], [Tlam])
        for i in range(2):
            S.op("dve", lambda e, i=i: e.scalar_tensor_tensor(out=junk[:], in0=lamt[:, 2 * i, :], scalar=1.0,
                                                             in1=lamt[:, 2 * i + 1, :], op0=ALU.mult, op1=ALU.mult,
                                                             accum_out=lw[:, i:i + 1]), [Tlam], [Tjunk, Tlw])
        S.op("act", lambda e: e.activation(out=lw[:, 2:4], in_=lw[:, 0:2], func=AF.Exp), [Tlw], [Tlw])
        S.op("dve", lambda e: e.tensor_tensor(out=lw[:, 4:5], in0=lw[:, 3:4], in1=lw[:, 2:3], op=ALU.subtract), [Tlw], [Tlw])
        S.op("dve", lambda e: e.tensor_scalar(out=lw[:, 5:6], in0=lw[:, 4:5], scalar1=-LAM_INIT, scalar2=None, op0=ALU.add),
             [Tlw], [Tlw])
        S.op("dve", lambda e: e.tensor_scalar(out=lw[:, 6:7], in0=colp[:, 48:49], scalar1=(1.0 - LAM_INIT), scalar2=None,
                                              op0=ALU.mult), [], [Tlw])

        def head(h):
            Kh, TK = Kr.next(); Vh, TV = Vr.next(); Qh, TQ = Qr.next()
            S.dma("sync", lambda e: e.dma_start(out=Kh[:], in_=c.k_scr[h, :, :]), [], [TK])
            S.dma("sync", lambda e: e.dma_start(out=Vh[:], in_=c.v_scr[h, :, :, :]), [], [TV])
            S.dma("sync", lambda e: e.dma_start(out=Qh[:], in_=c.q_scr[h, :, :, :]), [], [TQ])

            def qtile(g):
                nj = 16 * g + 16
                pend = {}
                j0 = max(0, 16 * g - 1 - int(math.ceil(1.0 / SLOPES[h])))

                def scores(j):
                    c0 = 0 if j < 16 * g else 128 * ((j - 16 * g) // 4)
                    n = j - 16 * g + 48
                    ps_l = []
                    for m in range(2):
                        ps, Tps = Sr.next()
                        S.op("pe", lambda e, ps=ps, m=m: e.matmul(ps[:, c0:512], Kh[:, j * 128:(j + 1) * 128],
                                                                   Qh[:, m, g * 512 + c0:(g + 1) * 512], start=True, stop=True),
                             [TK, TQ], [Tps])
                        sbt, Tsb = sbr.next()
                        if j >= 16 * g:
                            jj = (j - 16 * g) % 4
                            S.op("dve", lambda e, ps=ps, sbt=sbt: e.tensor_tensor(out=sbt[:, c0:c0 + 128], in0=ps[:, c0:c0 + 128],
                                                                                  in1=mf[:, h, jj, :], op=ALU.add), [Tps, Tc], [Tsb])
                            if c0 + 128 < 512:
                                S.op("dve", lambda e, ps=ps, sbt=sbt: e.scalar_tensor_tensor(
                                    out=sbt[:, c0 + 128:512], in0=ps[:, c0 + 128:512], scalar=kbt[:, h, n:n + 1],
                                    in1=dq[:, h, c0 + 128:512], op0=ALU.add, op1=ALU.add), [Tps, Tc], [Tsb])
                        else:
                            S.op("dve", lambda e, ps=ps, sbt=sbt: e.scalar_tensor_tensor(
                                out=sbt[:, :], in0=ps[:, :], scalar=kbt[:, h, n:n + 1], in1=dq[:, h, :],
                                op0=ALU.add, op1=ALU.add), [Tps, Tc], [Tsb])
                        pb, Tpb = pbr.next()
                        S.op("act", lambda e, sbt=sbt, pb=pb: e.activation(out=pb[:, c0:512], in_=sbt[:, c0:512], func=AF.Exp),
                             [Tsb], [Tpb])
                        ps_l.append((pb, Tpb))
                    pend[j] = (c0, ps_l)

                def av(j):
                    c0, ps_l = pend.pop(j)
                    for m in range(2):
                        pb, Tpb = ps_l[m]
                        S.op("pe", lambda e, pb=pb, m=m: e.matmul(Ab[m][0][:, c0:512], Vh[:, j, :], pb[:, c0:512],
                                                                   start=(j == j0), stop=(j == nj - 1)), [TV, Tpb], [Ab[m][1]])
                        S.op("pe", lambda e, pb=pb, m=m: e.matmul(Zb[m][0][:, c0:512], c.ones_bf, pb[:, c0:512],
                                                                   start=(j == j0), stop=(j == nj - 1)), [Tcst, Tpb], [Zb[m][1]])

                j0 = max(0, 16 * g - 1 - int(math.ceil(1.0 / SLOPES[h])))
                for st in range(j0, nj + 1):
                    if st < nj:
                        scores(st)
                    if st >= j0 + 1:
                        av(st - 1)
                for m in range(2):
                    S.op("dve", lambda e, m=m: e.reciprocal(out=rz[m][0][:], in_=Zb[m][0][:]), [Zb[m][1]], [rz[m][1]])
                    S.op("dve", lambda e, m=m: e.tensor_tensor(out=oo[m][0][:], in0=Ab[m][0][:], in1=rz[m][0][:], op=ALU.mult),
                         [Ab[m][1], rz[m][1]], [oo[m][1]])
                S.op("dve", lambda e: e.scalar_tensor_tensor(out=oo[0][0][:], in0=oo[1][0][:], scalar=lw[:, 5:6], in1=oo[0][0][:],
                                                             op0=ALU.mult, op1=ALU.add), [oo[0][1], oo[1][1], Tlw], [oo[0][1]])
                S.op("pool", lambda e: e.tensor_tensor(out=osq[:], in0=oo[0][0][:], in1=oo[0][0][:], op=ALU.mult), [oo[0][1]], [Tosq])
                ps, Tps = Sr.next()
                S.op("pe", lambda e, ps=ps: e.matmul(ps[:], c.ones_f, osq[:], start=True, stop=True), [Tosq, Tcst], [Tps])
                S.op("act", lambda e, ps=ps: e.activation(out=rst[:], in_=ps[:], func=AF.Sqrt, bias=c.epsc[:, 0:1],
                                                          scale=1.0 / 128.0), [Tps, Tcst], [Trst])
                S.op("dve", lambda e: e.reciprocal(out=rst[:], in_=rst[:]), [Trst], [Trst])
                S.op("dve", lambda e: e.scalar_tensor_tensor(out=c.attT[:, h, g * 512:(g + 1) * 512], in0=oo[0][0][:],
                                                             scalar=lw[:, 6:7], in1=rst[:], op0=ALU.mult, op1=ALU.mult),
                     [oo[0][1], Trst, Tlw], [TattT])

            for g in range(4):
                qtile(g)

        for h in range(4):
            head(h)
        with nc.Block() as blk:
            S.emit(blk)


class LNbufs:
    def __init__(self, nc, es, tag):
        self.stt = _sb(nc, es, f"ln_stt_{tag}", [128, 12], F32); self.Tstt = T("stt")
        self.mv = _sb(nc, es, f"ln_mv_{tag}", [128, 2], F32); self.Tmv = T("mv")
        self.rs = _sb(nc, es, f"ln_rs_{tag}", [128, 1], F32); self.Trs = T("rs")
        self.nm = _sb(nc, es, f"ln_nm_{tag}", [128, 1], F32); self.Tnm = T("nm")
        self.tmp = _sb(nc, es, f"ln_tmp_{tag}", [128, D], F32); self.Ttmp = T("tmp")


def ln_tm(c, S, B, src, Tsrc, dst, Tdst, grow, brow, Trows):
    for hh in range(2):
        S.op("dve", lambda e, hh=hh: e.bn_stats(out=B.stt[:, hh * 6:(hh + 1) * 6], in_=src[:, hh * 512:(hh + 1) * 512]),
             [Tsrc], [B.Tstt])
    S.op("dve", lambda e: e.bn_aggr(out=B.mv[:], in_=B.stt[:]), [B.Tstt], [B.Tmv])
    S.op("act", lambda e: e.activation(out=B.rs[:], in_=B.mv[:, 1:2], func=AF.Sqrt, bias=c.epsc[:, 0:1], scale=1.0),
         [B.Tmv], [B.Trs])
    S.op("dve", lambda e: e.reciprocal(out=B.rs[:], in_=B.rs[:]), [B.Trs], [B.Trs])
    S.op("dve", lambda e: e.scalar_tensor_tensor(out=B.nm[:], in0=B.mv[:, 0:1], scalar=-1.0, in1=B.rs[:],
                                                 op0=ALU.mult, op1=ALU.mult), [B.Tmv, B.Trs], [B.Tnm])
    S.op("act", lambda e: e.activation(out=B.tmp[:], in_=src[:], func=AF.Identity, scale=B.rs[:, 0:1], bias=B.nm[:, 0:1]),
         [Tsrc, B.Trs, B.Tnm], [B.Ttmp])
    S.op("dve", lambda e: e.tensor_tensor(out=B.tmp[:], in0=B.tmp[:], in1=grow, op=ALU.mult), [B.Ttmp, Trows], [B.Ttmp])
    S.op("pool", lambda e: e.tensor_tensor(out=dst[:], in0=B.tmp[:], in1=brow, op=ALU.add), [B.Ttmp, Trows], [Tdst])


def phase3(c):
    nc = c.nc
    with ExitStack() as es:
        S = Sched(nc, es, "p3")
        rc = {}
        sb = lambda n, s, d: _sb(nc, es, n, s, d)
        wo = sb("wo", [128, 8, D], BF16); Two = [T(f"wo{k}") for k in range(8)]
        wost = Ring([(sb(f"wost{i}", [128, D], F32), T(f"wost{i}")) for i in range(2)])
        wr = sb("wr", [128, 8, NE], F32); Twr = T("wr")
        rowsb = sb("rowsb", [128, 4, D], F32); Trows = T("rows")
        brt = sb("brt", [128, NE], F32); Tbrt = T("brt")
        msk = sb("msk", [128, NOB, NE], BF16); Tmsk = [T(f"msk{i}") for i in range(NOB)]
        xs = Ring([(sb(f"xs3_{i}", [128, D], F32), T(f"xs3_{i}")) for i in range(2)])
        x0_l = [(sb(f"x0_{i}", [128, D], F32), T(f"x0_{i}")) for i in range(2)]
        yy_l = [(sb(f"yy_{i}", [128, D], F32), T(f"yy_{i}")) for i in range(2)]
        x1r = Ring([(sb(f"x1_{i}", [128, D], F32), T(f"x1_{i}")) for i in range(2)])
        x1br = Ring([(sb(f"x1b_{i}", [128, D], BF16), T(f"x1b_{i}")) for i in range(2)])
        x1T_l = [(sb(f"x1T_{i}", [128, 8, 128], F32), T(f"x1T_{i}")) for i in range(2)]
        lg_l = [(sb(f"lg_{i}", [128, NE], F32), T(f"lg_{i}")) for i in range(2)]
        t8_l = [(sb(f"t8_{i}", [128, 8], F32), T(f"t8_{i}")) for i in range(2)]
        sm_l = [(sb(f"sm3_{i}", [128, 16], F32), T(f"sm3_{i}")) for i in range(2)]
        destf_l = [(sb(f"destf_{i}", [128, NE], F32), T(f"destf_{i}")) for i in range(2)]
        junk_l = [(sb(f"junk3_{i}", [128, NE], F32), T(f"junk3_{i}")) for i in range(2)]
        B0_l = [LNbufs(nc, es, f"a{i}") for i in range(2)]; B1_l = [LNbufs(nc, es, f"b{i}") for i in range(2)]
        mixr = Ring([(_ps(nc, es, f"mix{i}", [128, 512], F32), T(f"mix{i}")) for i in range(4)])
        tpf = [(_ps(nc, es, f"tpf{i}", [128, 512], F32), T(f"tpf{i}")) for i in range(2)]
        lgp = _ps(nc, es, "lgp", [128, 512], F32); Tlgp = T("lgp")
        posp = _ps(nc, es, "posp", [128, 512], F32); Tposp = T("posp")
        Tcst = T("cst"); Tatt = T("att"); Trec = T("rec"); Tgts = T("gts"); Tdst = T("dst")
        Txg = T("xg_scr"); Tx1s = T("x1_scr")

        for i in range(4):
            S.dma("sync", lambda e, i=i: e.dma_start(out=rowsb[:, i, :], in_=_bcast_rows(c.rows, 128, D, offset=i * D)),
                  [], [Trows])
        S.dma("sync", lambda e: e.dma_start(out=brt[:], in_=_bcast_rows(c.b_router, 128, NE)), [], [Tbrt])
        for kc in range(8):
            S.dma("sync", lambda e, kc=kc: e.dma_start(out=wr[:, kc, :], in_=c.w_router[kc * 128:(kc + 1) * 128, :]), [], [Twr])
            st, Tst = wost.next()
            S.dma("sync", lambda e, st=st, kc=kc: e.dma_start(out=st[:], in_=c.w_out[kc * 128:(kc + 1) * 128, :]), [], [Tst])
            if kc % 2:
                S.op("act", lambda e, st=st, kc=kc: e.copy(out=wo[:, kc, :], in_=st[:]), [Tst], [Two[kc]])
            else:
                S.op("pool", lambda e, st=st, kc=kc: e.tensor_copy(out=wo[:, kc, :], in_=st[:]), [Tst], [Two[kc]])

        def block(ob):
            cs = slice(ob * 128, (ob + 1) * 128)
            x0, Tx0 = x0_l[ob % 2]; yy, Tyy = yy_l[ob % 2]; x1T, Tx1T = x1T_l[ob % 2]; lg, Tlg = lg_l[ob % 2]
            t8, Tt8 = t8_l[ob % 2]; sm, Tsm = sm_l[ob % 2]; destf, Tdestf = destf_l[ob % 2]; junk, Tjunk = junk_l[ob % 2]
            B0 = B0_l[ob % 2]; B1 = B1_l[ob % 2]
            xt, Txt = xs.next()
            S.dma("sync", lambda e: e.dma_start(out=xt[:], in_=c.x_own[cs, :]), [], [Txt])
            ln_tm(c, S, B0, xt, Txt, x0, Tx0, rowsb[:, 0, :], rowsb[:, 1, :], Trows)
            for hf in range(2):
                ps, Tps = mixr.next()
                for kc in range(8):
                    lhs = c.attT[:, kc, cs] if kc < 4 else c.recT[:, kc - 4, cs]
                    S.op("pe", lambda e, ps=ps, lhs=lhs, kc=kc, hf=hf: e.matmul(ps[:], lhs, wo[:, kc, hf * 512:(hf + 1) * 512],
                                                                              start=(kc == 0), stop=(kc == 7)),
                         [Two[kc], Tatt, Trec], [Tps])
                S.op("dve", lambda e, ps=ps, hf=hf: e.scalar_tensor_tensor(
                    out=yy[:, hf * 512:(hf + 1) * 512], in0=x0[:, hf * 512:(hf + 1) * 512], scalar=float(ALPHA), in1=ps[:],
                    op0=ALU.mult, op1=ALU.add), [Tx0, Tps], [Tyy])
            x1, Tx1 = x1r.next()
            ln_tm(c, S, B1, yy, Tyy, x1, Tx1, rowsb[:, 2, :], rowsb[:, 3, :], Trows)
            S.dma("pool", lambda e: e.dma_start(out=c.x1_scr[cs, :], in_=x1[:]), [Tx1], [Tx1s], sem_tile=Tx1s)
            x1b, Tx1b = x1br.next()
            S.op("act", lambda e: e.copy(out=x1b[:], in_=x1[:]), [Tx1], [Tx1b])
            for kc in range(8):
                pt, Tpt = tpf[kc // 4]
                S.op("pe", lambda e, pt=pt, kc=kc: e.transpose(out=pt[:, (kc % 4) * 128:(kc % 4 + 1) * 128],
                                                               in_=x1[:, kc * 128:(kc + 1) * 128], identity=c.ident_f),
                     [Tx1, Tcst], [Tpt])
            S.op("act", lambda e: e.copy(out=x1T[:, 0:4, :].rearrange("p a b -> p (a b)"), in_=tpf[0][0][:]), [tpf[0][1]], [Tx1T])
            S.op("dve", lambda e: e.tensor_copy(out=x1T[:, 4:8, :].rearrange("p a b -> p (a b)"), in_=tpf[1][0][:]),
                 [tpf[1][1]], [Tx1T])
            for kc in range(8):
                S.op("pe", lambda e, kc=kc: e.matmul(lgp[:, 0:NE], x1T[:, kc, :], wr[:, kc, :], start=(kc == 0), stop=(kc == 7)),
                     [Tx1T, Twr], [Tlgp])
            S.op("dve", lambda e: e.tensor_tensor(out=lg[:], in0=lgp[:, 0:NE], in1=brt[:], op=ALU.add), [Tlgp, Tbrt], [Tlg])
            S.op("dve", lambda e: e.max(out=t8[:], in_=lg[:]), [Tlg], [Tt8])
            S.op("dve", lambda e: e.tensor_scalar(out=sm[:, 0:1], in0=t8[:, 0:1], scalar1=-1.0, scalar2=None, op0=ALU.mult),
                 [Tt8], [Tsm])
            S.op("act", lambda e: e.activation(out=sm[:, 4:8], in_=t8[:, 0:4], func=AF.Exp, bias=sm[:, 0:1], scale=1.0,
                                               accum_out=sm[:, 1:2]), [Tt8, Tsm], [Tsm])
            S.op("dve", lambda e: e.reciprocal(out=sm[:, 1:2], in_=sm[:, 1:2]), [Tsm], [Tsm])
            S.op("dve", lambda e: e.tensor_scalar(out=c.gts[:, ob, :], in0=sm[:, 4:8], scalar1=sm[:, 1:2], scalar2=None,
                                                  op0=ALU.mult), [Tsm], [Tgts])
            S.op("dve", lambda e: e.tensor_scalar(out=msk[:, ob, :], in0=lg[:], scalar1=t8[:, 3:4], scalar2=None, op0=ALU.is_ge),
                 [Tlg, Tt8], [Tmsk[ob]])
            for o2 in range(ob + 1):
                lhs = c.ltri_bf if o2 == ob else c.ones_bf
                S.op("pe", lambda e, lhs=lhs, o2=o2: e.matmul(posp[:, 0:NE], lhs, msk[:, o2, :], start=(o2 == 0), stop=(o2 == ob)),
                     [Tmsk[o2], Tcst], [Tposp])
            S.op("dve", lambda e: e.tensor_tensor(out=destf[:], in0=posp[:, 0:NE], in1=c.ecap, op=ALU.add), [Tposp, Tcst], [Tdestf])
            for k in range(4):
                S.op("dve", lambda e, k=k: e.scalar_tensor_tensor(out=junk[:], in0=lg[:], scalar=t8[:, k:k + 1], in1=destf[:],
                                                                 op0=ALU.is_equal, op1=ALU.mult, accum_out=sm[:, 8 + k:9 + k]),
                     [Tlg, Tt8, Tdestf], [Tjunk, Tsm])
            S.op("dve", lambda e: e.tensor_copy(out=c.dst[:, ob, :], in_=sm[:, 8:12]), [Tsm], [Tdst])
            for k in range(4):
                S.dma("pool", lambda e, k=k: e.indirect_dma_start(
                    out=c.xg_scr[:, :], out_offset=bass.IndirectOffsetOnAxis(ap=c.dst[:, ob, k:k + 1], axis=0),
                    in_=x1b[:, :], in_offset=None, bounds_check=_breg(e, rc), oob_is_err=False), [Tx1b, Tdst], [Txg], sem_tile=Txg)

        for ob in range(NOB):
            block(ob)
        with nc.Block() as blk:
            S.emit(blk)


def phase4(c):
    nc = c.nc
    with ExitStack() as es:
        S = Sched(nc, es, "p4")
        sb = lambda n, s, d: _sb(nc, es, n, s, d)
        wg = [(sb(f"wg{i}", [128, 8, 2048], BF16), [T(f"wg{i}_{k}") for k in range(8)]) for i in range(2)]
        wd = [(sb(f"wd{i}", [128, 8, D], BF16), [T(f"wd{i}_{k}") for k in range(8)]) for i in range(2)]
        stg = Ring([(sb(f"stg{i}", [128, 2048], F32), T(f"stg{i}")) for i in range(4)])
        xgt_l = [(sb(f"xgt{i}", [128, 3, D], BF16), T(f"xgt{i}")) for i in range(2)]
        xgT = [(sb(f"xgT{i}", [128, 8, CAP], BF16), T(f"xgT{i}")) for i in range(2)]
        actT = [(sb(f"actT{i}", [128, 8, CAP], BF16), T(f"actT{i}")) for i in range(2)]
        glu = Ring([(sb(f"glu{i}", [128, CAP], F32), T(f"glu{i}")) for i in range(3)])
        sg = Ring([(sb(f"sg{i}", [128, CAP], F32), T(f"sg{i}")) for i in range(3)])
        lin = Ring([(sb(f"lin{i}", [128, CAP], F32), T(f"lin{i}")) for i in range(3)])
        ysb = Ring([(sb(f"ysb{i}", [128, D], F32), T(f"ysb{i}")) for i in range(2)])
        bdf_l = [(sb(f"bdf{i}", [1, D], F32), T(f"bdf{i}")) for i in range(2)]
        bdb_l = [(sb(f"bdb{i}", [128, D], BF16), T(f"bdb{i}")) for i in range(2)]
        bg = sb("bg", [128, NE * 16], F32); Tbg = T("bg")
        bl1 = sb("bl1", [128, NE * 16], F32); Tbl1 = T("bl1")
        tpl = [_ps(nc, es, f"tp4_{i}", [128, 1024], BF16) for i in range(2)]; Ttp = [T("tp4a"), T("tp4b")]
        psg = Ring([(_ps(nc, es, f"psg{i}", [128, 512], F32), T(f"psg{i}")) for i in range(2)])
        psl = Ring([(_ps(nc, es, f"psl{i}", [128, 512], F32), T(f"psl{i}")) for i in range(2)])
        psy = Ring([(_ps(nc, es, f"psy{i}", [128, 512], F32), T(f"psy{i}")) for i in range(2)])
        Tcst = T("cst"); Tys = T("y_scr")
        S.dma("sync", lambda e: e.dma_start(out=bg[:], in_=c.bgu[:, :]), [], [Tbg])
        for bdb_, Tbdb_ in bdb_l:
            S.op("pool", lambda e, bdb_=bdb_: e.memset(bdb_[:], 0.0), [], [Tbdb_])
        S.op("pool", lambda e: e.tensor_scalar(out=bl1[:], in0=bg[:], scalar1=1.0, scalar2=None, op0=ALU.add), [Tbg], [Tbl1])
        cast_cycle = ["act", "dve", "act", "act", "dve", "act", "act", "dve"]
        cc = [0]

        def cast(out, in_, reads, writes):
            eng = cast_cycle[cc[0] % len(cast_cycle)]
            cc[0] += 1
            if eng == "act":
                S.op("act", lambda e: e.copy(out=out, in_=in_), reads, writes)
            else:
                S.op(eng, lambda e: e.tensor_copy(out=out, in_=in_), reads, writes)

        def weight_chunks(e_):
            wgt, Twg = wg[e_ % 2]
            wdt, Twd = wd[e_ % 2]
            out = []

            def gu(kc):
                st, Tst = stg.next()
                S.dma("sync", lambda e: e.dma_start(out=st[:], in_=c.w_gu[e_, kc * 128:(kc + 1) * 128, :]), [], [Tst])
                v = st[:].rearrange("p (f two) -> p two f", two=2)
                cast(wgt[:, kc, 0:1024], v[:, 0, :], [Tst], [Twg[kc]])
                cast(wgt[:, kc, 1024:2048], v[:, 1, :], [Tst], [Twg[kc]])

            def dn(kc):
                st, Tst = stg.next()
                S.dma("sync", lambda e: e.dma_start(
                    out=st[:].rearrange("p (a d) -> p a d", a=2),
                    in_=c.w_down[e_, kc * 128:(kc + 2) * 128, :].rearrange("(a p) d -> p a d", p=128)), [], [Tst])
                cast(wdt[:, kc, :], st[:, 0:D], [Tst], [Twd[kc]])
                cast(wdt[:, kc + 1, :], st[:, D:2 * D], [Tst], [Twd[kc + 1]])

            for kc in range(8):
                out.append(lambda kc=kc: gu(kc))
            for kc in range(0, 8, 2):
                out.append(lambda kc=kc: dn(kc))
            return out

        def load_acts(e_):
            xgt, Txgt = xgt_l[e_ % 2]
            bdf, Tbdf = bdf_l[e_ % 2]
            bdb, Tbdb = bdb_l[e_ % 2]
            S.dma("pool", lambda e: e.dma_start(out=xgt[:], in_=c.xg_scr[e_ * CAP:(e_ + 1) * CAP, :].rearrange("(s p) d -> p s d", p=128)),
                  [], [Txgt])
            S.dma("pool", lambda e: e.dma_start(out=bdf[:], in_=c.b_down[e_:e_ + 1, :]), [], [Tbdf])
            S.op("pool", lambda e: e.tensor_copy(out=bdb[0:1, :], in_=bdf[:]), [Tbdf], [Tbdb])

        def load_weights(e_):
            for f in weight_chunks(e_):
                f()

        hooks = []

        def step():
            if hooks:
                hooks.pop(0)()

        def expert(e_):
            wgt, Twg = wg[e_ % 2]
            wdt, Twd = wd[e_ % 2]
            xT_, TxT_ = xgT[e_ % 2]
            aT, TaT = actT[e_ % 2]
            xgt, Txgt = xgt_l[e_ % 2]
            bdb, Tbdb = bdb_l[e_ % 2]
            for kc in range(8 if c.p4mask & 2 else 0):
                hf = kc % 2
                for sc in range(3):
                    S.op("pe", lambda e, kc=kc, sc=sc, hf=hf: e.transpose(
                        out=tpl[hf][:, sc * 128:(sc + 1) * 128], in_=xgt[:, sc, kc * 128:(kc + 1) * 128],
                        identity=c.ident_bf), [Txgt, Tcst], [Ttp[hf]])
                if kc % 2:
                    S.op("act", lambda e, kc=kc, hf=hf: e.copy(out=xT_[:, kc, :], in_=tpl[hf][:, 0:CAP]), [Ttp[hf]], [TxT_])
                else:
                    S.op("dve", lambda e, kc=kc, hf=hf: e.tensor_copy(out=xT_[:, kc, :], in_=tpl[hf][:, 0:CAP]),
                         [Ttp[hf]], [TxT_])
            for fc in range(8 if c.p4mask & 4 else 0):
                pg, Tpg = psg.next()
                pl, Tpl = psl.next()
                for kc in range(8):
                    S.op("pe", lambda e, pg=pg, kc=kc, fc=fc: e.matmul(pg[:, 0:CAP], wgt[:, kc, fc * 128:(fc + 1) * 128], xT_[:, kc, :],
                                                                     start=(kc == 0), stop=(kc == 7)), [Twg[kc], TxT_], [Tpg])
                for kc in range(8):
                    S.op("pe", lambda e, pl=pl, kc=kc, fc=fc: e.matmul(pl[:, 0:CAP], wgt[:, kc, 1024 + fc * 128:1024 + (fc + 1) * 128],
                                                                     xT_[:, kc, :], start=(kc == 0), stop=(kc == 7)),
                         [Twg[kc], TxT_], [Tpl])
                gl_, Tgl = glu.next(); sg_, Tsg = sg.next(); ln_, Tln = lin.next()
                col = e_ * 16 + fc
                S.op("dve", lambda e, pg=pg, gl_=gl_, col=col: e.tensor_scalar(out=gl_[:], in0=pg[:, 0:CAP], scalar1=bg[:, col:col + 1],
                                                                              scalar2=7.0, op0=ALU.add, op1=ALU.min), [Tpg, Tbg], [Tgl])
                S.op("act", lambda e, gl_=gl_, sg_=sg_: e.activation(out=sg_[:], in_=gl_[:], func=AF.Sigmoid, scale=1.702), [Tgl], [Tsg])
                S.op("dve", lambda e, pl=pl, ln_=ln_, col=col: e.tensor_scalar(out=ln_[:], in0=pl[:, 0:CAP], scalar1=bl1[:, col + 8:col + 9],
                                                                              scalar2=-6.0, op0=ALU.add, op1=ALU.max), [Tpl, Tbl1], [Tln])
                S.op("dve", lambda e, gl_=gl_, sg_=sg_: e.tensor_tensor(out=sg_[:], in0=gl_[:], in1=sg_[:], op=ALU.mult), [Tgl, Tsg], [Tsg])
                S.op("dve", lambda e, ln_=ln_, sg_=sg_, fc=fc: e.scalar_tensor_tensor(out=aT[:, fc, :], in0=ln_[:], scalar=8.0, in1=sg_[:],
                                                                                     op0=ALU.min, op1=ALU.mult), [Tln, Tsg], [TaT])
                step()
            for sc in range(3 if c.p4mask & 16 else 0):
                yt, Tyt = ysb.next()
                for hf in range(2):
                    py, Tpy = psy.next()
                    for fc in range(8):
                        S.op("pe", lambda e, py=py, fc=fc, hf=hf, sc=sc: e.matmul(py[:], aT[:, fc, sc * 128:(sc + 1) * 128],
                                                                                wdt[:, fc, hf * 512:(hf + 1) * 512],
                                                                                start=(fc == 0), stop=False), [TaT, Twd[fc]], [Tpy])
                    S.op("pe", lambda e, py=py, hf=hf: e.matmul(py[:], c.row0_bf, bdb[:, hf * 512:(hf + 1) * 512],
                                                               start=False, stop=True), [Tcst, Tbdb], [Tpy])
                    if hf:
                        S.op("act", lambda e, py=py, yt=yt, hf=hf: e.copy(out=yt[:, hf * 512:(hf + 1) * 512], in_=py[:]), [Tpy], [Tyt])
                    else:
                        S.op("dve", lambda e, py=py, yt=yt, hf=hf: e.tensor_copy(out=yt[:, hf * 512:(hf + 1) * 512], in_=py[:]), [Tpy], [Tyt])
                S.dma("pool", lambda e, yt=yt, sc=sc: e.dma_start(out=c.y_scr[e_ * CAP + sc * 128:e_ * CAP + (sc + 1) * 128, :], in_=yt[:]),
                      [Tyt], [Tys], sem_tile=Tys)
                step()
            while hooks:
                step()

        load_acts(0)
        load_weights(0)
        for e_ in range(c.nexp):
            if e_ + 1 < c.nexp:
                load_acts(e_ + 1)
                hooks.extend(weight_chunks(e_ + 1))
                step()
            expert(e_)
        with nc.Block() as blk:
            S.emit(blk)


def phase5(c):
    nc = c.nc
    with ExitStack() as es:
        S = Sched(nc, es, "p5")
        rc = {}
        sb = lambda n, s, d: _sb(nc, es, n, s, d)
        rowsb = sb("rows5", [128, 2, D], F32); Trows = T("rows5")
        yk = [Ring([(sb(f"yk{k}_{i}", [128, D], F32), T(f"yk{k}_{i}")) for i in range(2)]) for k in range(4)]
        x1r = Ring([(sb(f"x15_{i}", [128, D], F32), T(f"x15_{i}")) for i in range(2)])
        acc_l = [(sb(f"acc5_{i}", [128, D], F32), T(f"acc5_{i}")) for i in range(2)]
        outr = Ring([(sb(f"o5_{i}", [128, D], F32), T(f"o5_{i}")) for i in range(2)])
        B_l = [LNbufs(nc, es, f"c{i}") for i in range(2)]
        Tgts = T("gts"); Tdst = T("dst"); Tout = T("out")
        for i in range(2):
            S.dma("sync", lambda e, i=i: e.dma_start(out=rowsb[:, i, :], in_=_bcast_rows(c.rows, 128, D, offset=(4 + i) * D)),
                  [], [Trows])

        loaded = {}

        def loads(ob):
            cs = slice(ob * 128, (ob + 1) * 128)
            ys = []
            for k in range(4):
                yt, Tyt = yk[k].next()
                ys.append((yt, Tyt))
                S.dma("pool", lambda e, yt=yt, k=k: e.indirect_dma_start(
                    out=yt[:, :], out_offset=None, in_=c.y_scr[:, :],
                    in_offset=bass.IndirectOffsetOnAxis(ap=c.dst[:, ob, k:k + 1], axis=0),
                    bounds_check=_breg(e, rc), oob_is_err=False), [Tdst], [Tyt])
            x1, Tx1 = x1r.next()
            S.dma("sync", lambda e: e.dma_start(out=x1[:], in_=c.x1_scr[cs, :]), [], [Tx1])
            loaded[ob] = (ys, x1, Tx1)

        def block(ob):
            cs = slice(ob * 128, (ob + 1) * 128)
            acc, Tacc = acc_l[ob % 2]; B = B_l[ob % 2]
            ys, x1, Tx1 = loaded.pop(ob)
            if ob + 1 < NOB:
                loads(ob + 1)
            S.op("dve", lambda e: e.tensor_scalar(out=acc[:], in0=ys[0][0][:], scalar1=c.gts[:, ob, 0:1], scalar2=None, op0=ALU.mult),
                 [ys[0][1], Tgts], [Tacc])
            for k in range(1, 4):
                S.op("dve", lambda e, k=k: e.scalar_tensor_tensor(out=acc[:], in0=ys[k][0][:], scalar=c.gts[:, ob, k:k + 1], in1=acc[:],
                                                                 op0=ALU.mult, op1=ALU.add), [ys[k][1], Tgts, Tacc], [Tacc])
            S.op("dve", lambda e: e.scalar_tensor_tensor(out=acc[:], in0=x1[:], scalar=float(ALPHA), in1=acc[:],
                                                         op0=ALU.mult, op1=ALU.add), [Tx1, Tacc], [Tacc])
            ot, Tot = outr.next()
            ln_tm(c, S, B, acc, Tacc, ot, Tot, rowsb[:, 0, :], rowsb[:, 1, :], Trows)
            S.dma("sync", lambda e: e.dma_start(out=c.out[cs, :], in_=ot[:]), [Tot], [Tout], sem_tile=Tout)

        loads(0)
        for ob in range(NOB):
            block(ob)
        with nc.Block() as blk:
            S.emit(blk)


def _bf(a):
    return np.ascontiguousarray(a).astype(ml_dtypes.bfloat16)


def make_core_consts(r):
    f = np.float32
    p = np.arange(128, dtype=np.float64)
    kbt = np.zeros((128, 4, 64), f)
    dq = np.zeros((128, 4, 512), f)
    mfull = np.zeros((128, 4, 4, 128), f)
    q = np.arange(512)
    for h, sl in enumerate(SLOPES):
        for n in range(64):
            kbt[:, h, n] = sl * (128.0 * (n - 48 - r) + p)
        dq[:, h, :] = (-sl * (512.0 * (q // 128) + (q % 128)))[None, :]
        for jj in range(4):
            kpos = 128 * jj + np.arange(128)[:, None]
            qpos = 128 * r + np.arange(128)[None, :]
            allowed = (kpos // 64) <= (qpos // 64)
            mfull[:, h, jj, :] = np.where(allowed, -sl * np.abs(qpos - kpos), NEG)
    ident = np.eye(128, dtype=f)
    ones = np.ones((128, 128), f)
    ltri = (np.arange(128)[:, None] < np.arange(128)[None, :]).astype(f)
    row0 = np.zeros((128, 128), f)
    row0[0, :] = 1.0
    cst_bf = _bf(np.concatenate([ident, ones, ltri, row0], axis=1))
    cst_f = np.zeros((128, NCF), f)
    cst_f[:, 0:128] = ident
    cst_f[:, 128:256] = 1.0
    cst_f[:, 256:256 + NE] = (np.arange(NE) * CAP)[None, :]
    cst_f[:, 256 + NE + r] = 1.0
    cst_f[:, 292] = LN_EPS
    cst_f[:, 293] = 1.0
    cst_f[:, 294] = -0.5
    return {"kbt": kbt.reshape(128, -1), "dq": dq.reshape(128, -1), "mfull": mfull.reshape(128, -1),
            "cst_bf": cst_bf, "cst_f": cst_f}


def make_in_maps(inp, ne_w=NE):
    f = np.float32
    g = lambda k: np.asarray(inp[k], dtype=f)
    x = g("x")
    colp = np.zeros((128, 64), f)
    chunk = lambda v, n: np.asarray(v, f).reshape(n, 128).T
    colp[:, 0:8] = chunk(g("ln0_g"), 8)
    colp[:, 8:16] = chunk(g("ln0_b"), 8)
    cw = g("conv_w")[0]
    for ch in range(4):
        for w in range(4):
            colp[:, 16 + ch * 4 + w] = cw[w, ch * 128:(ch + 1) * 128]
    colp[:, 32:36] = chunk(g("conv_b")[0], 4)
    colp[:, 36:40] = chunk(g("b_rg_a")[0].reshape(512), 4)
    colp[:, 40:44] = chunk(g("b_rg_x")[0].reshape(512), 4)
    colp[:, 44:48] = chunk(g("lru_lambda")[0], 4)
    colp[:, 48] = g("subln_g")[0]
    rows = np.zeros((8, D), f)
    rows[0], rows[1] = g("ln0_g"), g("ln0_b")
    rows[2], rows[3] = g("ln1_g")[0], g("ln1_b")[0]
    rows[4], rows[5] = g("ln2_g")[0], g("ln2_b")[0]
    bgu = g("b_gu")[0]
    bg = bgu[:, 0::2].reshape(NE, 8, 128)
    bl = bgu[:, 1::2].reshape(NE, 8, 128)
    bgu_l = np.concatenate([bg, bl], axis=1).transpose(2, 0, 1).reshape(128, NE * 16)
    shared = {
        "w_in": np.ascontiguousarray(g("w_in")[0]), "w_out": np.ascontiguousarray(g("w_out")[0]),
        "w_router": np.ascontiguousarray(g("w_router")[0]), "w_gu": np.ascontiguousarray(g("w_gu")[0][:ne_w]),
        "w_down": np.ascontiguousarray(g("w_down")[0][:ne_w]), "b_down": np.ascontiguousarray(g("b_down")[0]),
        "rows": rows, "colp": colp,
        "w_rg": np.ascontiguousarray(np.stack([g("w_rg_a")[0], g("w_rg_x")[0]])),
        "lamv": np.ascontiguousarray(np.stack([g("lam_q1")[0], g("lam_k1")[0], g("lam_q2")[0], g("lam_k2")[0]])),
        "b_router": np.ascontiguousarray(g("b_router")), "bgu": np.ascontiguousarray(bgu_l),
    }
    maps = []
    for core in range(NCORES):
        b, r = core // 4, core % 4
        m = dict(shared)
        m["x_full"] = np.ascontiguousarray(x[b])
        m["x_own"] = np.ascontiguousarray(x[b].reshape(NOB, 4, 128, D)[:, r].reshape(OWN, D))
        m.update(make_core_consts(r))
        maps.append(m)
    return maps


def assemble(results):
    out = np.zeros((2, SEQ, D), np.float32)
    for core in range(NCORES):
        b, r = core // 4, core % 4
        o = np.asarray(results[core]["out"], np.float32).reshape(NOB, 128, D)
        out[b].reshape(NOB, 4, 128, D)[:, r] = o
    return out


_NC_CACHE = {}


def kernel(**inputs):
    if "nc" not in _NC_CACHE:
        _NC_CACHE["nc"] = build_program()
    nc = _NC_CACHE["nc"]
    maps = make_in_maps(inputs)
    res = run_bass_kernel_spmd(nc, maps, core_ids=list(range(NCORES)))
    return assemble(res.results)
```
